# Optimizing a Trainium2 kernel written in Bass

```python
import jax, jax.numpy as jnp
from jax import lax
import numpy as np

D_MODEL = 1024
BATCH = 8
SEQ = 4096
DEPTH = 2
DEC_BATCH = 32
DEC_SEQ = 64
PAST_LEN = 1024

CHUNK = 64
GMLP_CHUNK = 128
GMLP_GROUPS = 8
D_V = 3 * D_MODEL
GMLP_GROUP_DIM = D_V // GMLP_GROUPS
CONV_WIDTH = 31
CONV_STATE = CONV_WIDTH - 1
D_FF = 7 * D_MODEL // 2
N_EXPERTS = 8
TOP_K = 2
N_GMLP_LAYERS = (DEPTH + 1) // 2
N_CONV_LAYERS = DEPTH // 2
LN_EPS = 1e-5
DEEPNORM_ALPHA = (2.0 * DEPTH) ** 0.25
DEEPNORM_BETA = (8.0 * DEPTH) ** -0.25

kernel_name = 'streaming_gmlp_conformer_hybrid_step'


def layer_norm(x, g, b):
    xf = x.astype(jnp.float32)
    mu = jnp.mean(xf, axis=-1, keepdims=True)
    xc = xf - mu
    var = jnp.mean(xc * xc, axis=-1, keepdims=True)
    y = xc * lax.rsqrt(var + LN_EPS) * g.astype(jnp.float32) + b.astype(jnp.float32)
    return y.astype(x.dtype)


def gmlp_mixer(x, w_in, b_in, lnv_g, lnv_b, w_s, b_s, w_out, b_out, block, offset):
    bsz, seq, _ = x.shape
    h = jax.nn.gelu(x @ w_in + b_in, approximate=False)
    u, v = jnp.split(h, 2, axis=-1)
    v = layer_norm(v, lnv_g, lnv_b)
    pos = np.arange(offset, offset + block) // CHUNK
    mask = pos[None, :] <= pos[:, None]
    w = jnp.where(mask, w_s[:, offset:offset + block, offset:offset + block], 0)
    vb = v.reshape(bsz, seq // block, block, GMLP_GROUPS, GMLP_GROUP_DIM)
    bias = jnp.transpose(b_s[:, offset:offset + block])[:, :, None]
    mixed = jnp.einsum('gij,bnjgc->bnigc', w, vb) + bias
    out = u * mixed.reshape(bsz, seq, D_V)
    return out @ w_out + b_out, v


def conv_glu(x, w_pw1, b_pw1):
    a, gate = jnp.split(x @ w_pw1 + b_pw1, 2, axis=-1)
    return a * jax.nn.sigmoid(gate)


def conv_tail(rows, w_dw, b_dw, ln_g, ln_b, w_pw2, b_pw2):
    y = lax.conv_general_dilated(
        rows, w_dw[:, None, :], window_strides=(1,), padding='VALID',
        dimension_numbers=('NWC', 'WIO', 'NWC'), feature_group_count=D_MODEL) + b_dw
    y = jax.nn.silu(layer_norm(y, ln_g, ln_b))
    return y @ w_pw2 + b_pw2


def swiglu(x, wg, wu, wd):
    return (jax.nn.silu(x @ wg) * (x @ wu)) @ wd


def moe_swiglu(x, w_router, b_router, wg, wu, wd):
    shape = x.shape
    t = x.reshape(-1, shape[-1])
    logits = (t @ w_router).astype(jnp.float32) + b_router.astype(jnp.float32)
    top_val, top_idx = lax.top_k(logits, TOP_K)
    top_w = jax.nn.softmax(top_val, axis=-1)
    gates = jnp.einsum('tk,tke->te', top_w,
                       jax.nn.one_hot(top_idx, N_EXPERTS, dtype=jnp.float32)).astype(t.dtype)
    out = jnp.zeros_like(t)
    for e in range(N_EXPERTS):
        out = out + gates[:, e:e + 1] * swiglu(t, wg[e], wu[e], wd[e])
    return out.reshape(shape)


def setup_inputs(seed: int = 0) -> dict:
    key = jax.random.key(seed)
    ks = iter(jax.random.split(key, 40))

    def nrm(shape, scale):
        return jax.random.normal(next(ks), shape, jnp.float32) * scale

    def gain(shape):
        return 1.0 + nrm(shape, 0.01)

    nA, nC, D, F, E = N_GMLP_LAYERS, N_CONV_LAYERS, D_MODEL, D_FF, N_EXPERTS
    return {
        'x_prompt': nrm((BATCH, SEQ, D), 1.0),
        'x_sample': nrm((DEC_BATCH, DEC_SEQ, D), 1.0),
        'cache_conv': nrm((nC, DEC_BATCH, CONV_STATE, D), 0.5),
        'gm_w_in': nrm((nA, D, 2 * D_V), D ** -0.5),
        'gm_b_in': nrm((nA, 2 * D_V), 0.01),
        'gm_lnv_g': gain((nA, D_V)),
        'gm_lnv_b': nrm((nA, D_V), 0.01),
        'gm_w_s': nrm((nA, GMLP_GROUPS, GMLP_CHUNK, GMLP_CHUNK), GMLP_CHUNK ** -0.5),
        'gm_b_s': gain((nA, GMLP_GROUPS, GMLP_CHUNK)),
        'gm_w_out': nrm((nA, D_V, D), D_V ** -0.5 * DEEPNORM_BETA),
        'gm_b_out': nrm((nA, D), 0.01),
        'cv_w_pw1': nrm((nC, D, 2 * D), D ** -0.5),
        'cv_b_pw1': nrm((nC, 2 * D), 0.01),
        'cv_w_dw': nrm((nC, CONV_WIDTH, D), CONV_WIDTH ** -0.5),
        'cv_b_dw': nrm((nC, D), 0.01),
        'cv_ln_g': gain((nC, D)),
        'cv_ln_b': nrm((nC, D), 0.01),
        'cv_w_pw2': nrm((nC, D, D), D ** -0.5 * DEEPNORM_BETA),
        'cv_b_pw2': nrm((nC, D), 0.01),
        'ff_w_gate': nrm((nA, D, F), D ** -0.5),
        'ff_w_up': nrm((nA, D, F), D ** -0.5),
        'ff_w_down': nrm((nA, F, D), F ** -0.5 * DEEPNORM_BETA),
        'moe_w_router': nrm((nC, D, E), D ** -0.5),
        'moe_b_router': nrm((nC, E), 0.01),
        'moe_w_gate': nrm((nC, E, D, F), D ** -0.5),
        'moe_w_up': nrm((nC, E, D, F), D ** -0.5),
        'moe_w_down': nrm((nC, E, F, D), F ** -0.5 * DEEPNORM_BETA),
        'ln_g': gain((DEPTH, 2, D)),
        'ln_b': nrm((DEPTH, 2, D), 0.01),
    }


def reference(x_prompt, x_sample, cache_conv,
              gm_w_in, gm_b_in, gm_lnv_g, gm_lnv_b, gm_w_s, gm_b_s, gm_w_out, gm_b_out,
              cv_w_pw1, cv_b_pw1, cv_w_dw, cv_b_dw, cv_ln_g, cv_ln_b, cv_w_pw2, cv_b_pw2,
              ff_w_gate, ff_w_up, ff_w_down,
              moe_w_router, moe_b_router, moe_w_gate, moe_w_up, moe_w_down,
              ln_g, ln_b):
    xp, xs = x_prompt, x_sample
    gmlp_offset = PAST_LEN % GMLP_CHUNK
    gm_state_p, gm_state_s, conv_state_p, conv_state_s = [], [], [], []
    for i in range(DEPTH):
        j = i // 2
        if i % 2 == 0:
            gm = (gm_w_in[j], gm_b_in[j], gm_lnv_g[j], gm_lnv_b[j], gm_w_s[j], gm_b_s[j],
                  gm_w_out[j], gm_b_out[j])
            mp, vp = gmlp_mixer(xp, *gm, GMLP_CHUNK, 0)
            ms, vs = gmlp_mixer(xs, *gm, xs.shape[1], gmlp_offset)
            gm_state_p.append(vp[:, -GMLP_CHUNK:])
            gm_state_s.append(vs)
        else:
            gp = conv_glu(xp, cv_w_pw1[j], cv_b_pw1[j])
            gs = conv_glu(xs, cv_w_pw1[j], cv_b_pw1[j])
            rows_p = jnp.concatenate(
                [jnp.zeros((gp.shape[0], CONV_STATE, D_MODEL), gp.dtype), gp], axis=1)
            rows_s = jnp.concatenate([cache_conv[j].astype(gs.dtype), gs], axis=1)
            tail = (cv_w_dw[j], cv_b_dw[j], cv_ln_g[j], cv_ln_b[j], cv_w_pw2[j], cv_b_pw2[j])
            mp = conv_tail(rows_p, *tail)
            ms = conv_tail(rows_s, *tail)
            conv_state_p.append(rows_p[:, -CONV_STATE:])
            conv_state_s.append(rows_s[:, -CONV_STATE:])
        xp = layer_norm(DEEPNORM_ALPHA * xp + mp, ln_g[i, 0], ln_b[i, 0])
        xs = layer_norm(DEEPNORM_ALPHA * xs + ms, ln_g[i, 0], ln_b[i, 0])
        if i % 2 == 0:
            fp = swiglu(xp, ff_w_gate[j], ff_w_up[j], ff_w_down[j])
            fs = swiglu(xs, ff_w_gate[j], ff_w_up[j], ff_w_down[j])
        else:
            moe = (moe_w_router[j], moe_b_router[j], moe_w_gate[j], moe_w_up[j], moe_w_down[j])
            fp = moe_swiglu(xp, *moe)
            fs = moe_swiglu(xs, *moe)
        xp = layer_norm(DEEPNORM_ALPHA * xp + fp, ln_g[i, 1], ln_b[i, 1])
        xs = layer_norm(DEEPNORM_ALPHA * xs + fs, ln_g[i, 1], ln_b[i, 1])
    return (xp, xs, jnp.stack(gm_state_p), jnp.stack(gm_state_s),
            jnp.stack(conv_state_p), jnp.stack(conv_state_s))
```

```python
import numpy as np
from contextlib import ExitStack
import concourse.bass as bass
import concourse.mybir as mybir
from concourse.bass_utils import run_bass_kernel_spmd

F32 = mybir.dt.float32
BF16 = mybir.dt.bfloat16
AF = mybir.ActivationFunctionType
ALU = mybir.AluOpType
AX = mybir.AxisListType

D = 1024
KD = 8
DV = 3072
CV = 24
FF = 3584
NE = 8
SEQ = 4096
NCORE = 8
NSAMP = 4
LS = 64
HW_ = 30
NTAP = 31
ALPHA = float((2.0 * 2) ** 0.25)
EPS = 1e-5
TOK = SEQ + NSAMP * LS
WT = 512
NPT = SEQ // WT
SLOT = 7168
NSLOT = 5
import os as _os
NOSELF = bool(_os.environ.get("NOSELF"))
POOLENG = "dve" if _os.environ.get("NOPOOL") else "pool"


class Res:
    __slots__ = ("w", "r", "const", "excl")

    def __init__(self, const=False, excl=False):
        self.w = None
        self.r = {}
        self.const = const
        self.excl = excl


class Builder:
    def __init__(self, nc, es):
        self.nc = nc
        self.es = es
        self.engs = {"pe": nc.tensor, "act": nc.scalar, "dve": nc.vector, "pool": nc.gpsimd, "sp": nc.sync}
        self.prog = {k: [] for k in self.engs}
        self.cnt = {k: 0 for k in self.engs}
        self.waited = {k: {} for k in self.engs}
        self.S = {k: es.enter_context(nc.semaphore("S_" + k)) for k in ("pe", "act", "dve", "pool")}
        self.dsem = {}
        self.regs = {"pe": es.enter_context(nc.tensor.register("cnt_pe")),
                     "act": es.enter_context(nc.scalar.register("cnt_act")),
                     "dve": es.enter_context(nc.vector.register("cnt_dve"))}
        self.semobj = {}
        for k, s in self.S.items():
            self.semobj[id(s)] = s

    def sb(self, name, shape, dt):
        return self.es.enter_context(self.nc.sbuf_tensor(name, shape, dt))

    def dma_sem(self, name):
        if name not in self.dsem:
            s = self.es.enter_context(self.nc.semaphore("D_" + name))
            self.dsem[name] = [s, 0]
            self.semobj[id(s)] = s
        return self.dsem[name]

    def _collect(self, eng, reads, writes):
        waits = {}
        own = id(self.S[eng]) if eng in self.S else None

        def need(ev):
            if ev is None:
                return
            sid, val = ev
            if sid == own and (eng == "pe" or NOSELF):
                return
            if self.waited[eng].get(sid, 0) >= val:
                return
            if waits.get(sid, 0) < val:
                waits[sid] = val

        for r in reads:
            need(r.w)
        for w in writes:
            need(w.w)
            for sid, val in w.r.items():
                need((sid, val))
        for sid, val in waits.items():
            self.waited[eng][sid] = val
        return [(self.semobj[sid], val) for sid, val in waits.items()]

    def _commit(self, ev, reads, writes):
        for r in reads:
            if not r.const:
                if r.r.get(ev[0], 0) < ev[1]:
                    r.r[ev[0]] = ev[1]
        for w in writes:
            w.w = ev
            w.r = {}

    def op(self, eng, fn, reads=(), writes=(), signal=True):
        if eng != "pe":
            ex = [r for r in reads if r.excl]
            if ex:
                reads = [r for r in reads if not r.excl]
                writes = list(writes) + ex
        waits = self._collect(eng, reads, writes)
        if signal:
            self.cnt[eng] += 1
            ev = (id(self.S[eng]), self.cnt[eng])
        else:
            ev = (id(self.S[eng]), self.cnt[eng] + 1)
        self._commit(ev, reads, writes)
        self.prog[eng].append((waits, fn, (self.S[eng], 1) if signal else None))

    def dma(self, q, out, in_, reads, writes, semname, **kw):
        waits = self._collect(q, reads, writes)
        ds = self.dma_sem(semname)
        ds[1] += 16
        ev = (id(ds[0]), ds[1])
        self._commit(ev, reads, writes)
        self.prog[q].append((waits, lambda e: e.dma_start(out=out, in_=in_, **kw), (ds[0], 16)))

    def if_begin(self, engs, ei, thresh):
        self._if = {}
        for eng in engs:
            entry = ["IF", ei, thresh, 0]
            self.prog[eng].append(entry)
            self._if[eng] = (entry, self.cnt[eng], dict(self.waited[eng]))

    def if_end(self):
        for eng, (entry, c0, w0) in self._if.items():
            entry[3] = self.cnt[eng] - c0
            self.prog[eng].append(["ENDIF"])
            self.waited[eng] = w0
        self._if = {}

    def barrier(self, engs):
        for eng in engs:
            waits = []
            for k, sem in self.S.items():
                if self.cnt[k] > 0 and not (eng == "pe" and k == "pe"):
                    if self.waited[eng].get(id(sem), 0) < self.cnt[k]:
                        self.waited[eng][id(sem)] = self.cnt[k]
                        waits.append((sem, self.cnt[k]))
            for name, (sem, tot) in self.dsem.items():
                if tot > 0 and self.waited[eng].get(id(sem), 0) < tot:
                    self.waited[eng][id(sem)] = tot
                    waits.append((sem, tot))
            self.prog[eng].append((waits, None, None))

    def final_wait(self, q):
        waits = []
        for name, (s, tot) in self.dsem.items():
            if tot > 0:
                waits.append((s, tot))
        self.prog[q].append((waits, None, None))

    def emit(self):
        nc = self.nc
        block = self.es.enter_context(nc.Block())

        def replay(key, e):
            prog = self.prog[key]
            i = 0
            n = len(prog)

            def run(entry):
                waits, fn, inc = entry
                for s_, v in waits:
                    e.wait_ge(s_, v)
                if fn is None:
                    return
                ins = fn(e)
                if inc is not None:
                    ins.then_inc(inc[0], inc[1])

            while i < n:
                entry = prog[i]
                if entry[0] == "IF":
                    _, ei, thresh, k = entry
                    j = i + 1
                    while prog[j][0] != "ENDIF":
                        j += 1
                    reg = self.regs[key]
                    with e.If_lt(reg, thresh + 1):
                        if k > 0:
                            e.drain().then_inc(self.S[key], k)
                    with e.Else():
                        for ent in prog[i + 1:j]:
                            run(ent)
                    i = j + 1
                    continue
                run(entry)
                i += 1

        @block.tensor
        def _(e):
            replay("pe", e)

        @block.scalar
        def _(e):
            replay("act", e)

        @block.vector
        def _(e):
            replay("dve", e)

        @block.gpsimd
        def _(e):
            replay("pool", e)

        @block.sync
        def _(e):
            replay("sp", e)


def build_program(dbg=None):
    nc = bass.Bass("TRN2", target_bir_lowering=False)
    es = ExitStack()
    with es:
        _build(nc, es, dbg or {})
    return nc


def _build(nc, es, dbg):
    B = Builder(nc, es)
    op, dma, sb = B.op, B.dma, B.sb

    def din(name, shape):
        return nc.dram_tensor(name, shape, F32, kind="ExternalInput").ap()

    def dout(name, shape):
        return nc.dram_tensor(name, shape, F32, kind="ExternalOutput").ap()

    xin = din("xin", [TOK, D])
    cache = din("cache", [NSAMP, HW_, D])
    w_in = din("gm_w_in", [D, 2 * DV])
    b_in = din("gm_b_in", [1, 2 * DV])
    lnv_g = din("gm_lnv_g", [1, DV])
    lnv_b = din("gm_lnv_b", [1, DV])
    w_s = din("gm_w_s", [8, 128, 128])
    b_s = din("gm_b_s", [8, 128])
    w_out = din("gm_w_out", [DV, D])
    b_out = din("gm_b_out", [1, D])
    w_pw1 = din("cv_w_pw1", [D, 2 * D])
    b_pw1 = din("cv_b_pw1", [1, 2 * D])
    w_dw = din("cv_w_dw", [NTAP, D])
    b_dw = din("cv_b_dw", [1, D])
    cln_g = din("cv_ln_g", [1, D])
    cln_b = din("cv_ln_b", [1, D])
    w_pw2 = din("cv_w_pw2", [D, D])
    b_pw2 = din("cv_b_pw2", [1, D])
    ffg = din("ff_w_gate", [D, FF])
    ffu = din("ff_w_up", [D, FF])
    ffd = din("ff_w_down", [FF, D])
    w_r = din("moe_w_router", [D, NE])
    b_r = din("moe_b_router", [1, NE])
    mog = din("moe_w_gate", [NE, D, FF])
    mou = din("moe_w_up", [NE, D, FF])
    mod = din("moe_w_down", [NE, FF, D])
    ln_g = din("ln_g", [1, 4 * D])
    ln_b = din("ln_b", [1, 4 * D])
    y_o = dout("y_o", [TOK, D])
    gv_o = dout("gv_o", [128 + NSAMP * LS, DV])
    cs_o = dout("cs_o", [HW_ * (1 + NSAMP), D])

    XF = sb("XF", [128, KD, WT], F32)
    XB = sb("XB", [128, KD, WT], BF16)
    RING = [sb("ring%d" % i, [128, SLOT], BF16) for i in range(NSLOT)]
    PH = sb("PH", [128, 24576], BF16)
    XS0 = sb("XS0", [128, D], F32)
    XS = [XS0, XS0]
    BROWF = PH[0:1, 0:4096].bitcast(F32).rearrange("o (a n) -> o a n", a=2)
    BROWT = PH[0:1, 4096:8192].bitcast(F32).rearrange("o (a n) -> o a n", a=2)
    WSA = PH[:, 8192:10240].bitcast(F32).rearrange("p (g j) -> p g j", g=8)
    IOTA_I = PH[:, 10240:11264].bitcast(mybir.dt.int32)
    UTRI_F = PH[:, 11264:11520].bitcast(F32)
    LNS = sb("LNS", [128, 13312], BF16)

    def lnv(off, n, f32=True):
        v = LNS[:, off:off + n]
        return v.bitcast(F32) if f32 else v
    MEAN, VAR, RSTD = lnv(0, 1024), lnv(1024, 1024), lnv(2048, 1024)
    T1 = [lnv(3072 + i * 1024, 1024) for i in range(2)]
    T2 = [lnv(5120 + i * 1024, 1024) for i in range(2)]
    SQ = [lnv(7168 + i * 512, 512, False) for i in range(2)]
    ZB = [lnv(8192 + i * 512, 512, False) for i in range(2)]
    identF = sb("identF", [128, 128], F32)
    identB = sb("identB", [128, 128], BF16)
    onesB = sb("onesB", [128, 128], BF16)
    WST = [sb("WST%d" % i, [128, 8, 128], BF16) for i in range(2)]
    BROW = sb("BROW", [1, 4, 1024], BF16)
    SEL = sb("SEL", [8, NE, 128], F32)
    WR = sb("WR", [128, KD, NE], F32)
    BR = sb("BR", [128, NE], F32)
    GTS = sb("GTS", [8, WT], F32)
    VB_IN = sb("VB_IN", [128, 48], F32)
    VLNVG = sb("VLNVG", [128, CV], F32)
    VLNVB = sb("VLNVB", [128, CV], F32)
    VBOUT = sb("VBOUT", [128, KD], F32)
    VBPW1 = sb("VBPW1", [128, 16], F32)
    WDW = sb("WDW", [128, KD, NTAP], F32)
    VBDW = sb("VBDW", [128, KD], F32)
    VCLG = sb("VCLG", [128, KD], F32)
    VCLB = sb("VCLB", [128, KD], F32)
    VBPW2 = sb("VBPW2", [128, KD], F32)
    VLNG = sb("VLNG", [128, 32], F32)
    VLNB = sb("VLNB", [128, 32], F32)
    VLNGA = sb("VLNGA", [128, 32], F32)
    VLNBA = sb("VLNBA", [128, 32], F32)
    EPST = sb("EPST", [128, 1], F32)
    HIST = sb("HIST", [128, KD, HW_], BF16)
    GL = sb("GL", [128, KD, NSAMP, HW_], F32)
    RT = {n: sb("RT_" + n, [128, NE], F32) for n in ("L", "M1", "L2", "M2", "G")}
    RS = {n: sb("RS_" + n, [128, 1], F32) for n in ("m1", "m2", "d", "e", "den", "w1", "w2")}
    I32 = mybir.dt.int32
    CAP = int(dbg.get("cap", 256))
    PM = LNS
    IOTA_F = sb("IOTA_F", [128, WT], F32)
    SLOT_I = sb("SLOT_I", [128, 4], I32)
    SLOTID = sb("SLOTID", [128, 4], F32)
    UTRI = sb("UTRI", [128, 128], BF16)
    MSKF = sb("MSKF", [128, 4, NE], F32)
    MSK = sb("MSK", [128, 4, NE], BF16)
    POS = sb("POS", [128, 4, NE], F32)
    POST = sb("POST", [8, WT], F32)
    CNT = sb("CNT", [1, NE], I32)
    PS = [es.enter_context(nc.psum_tensor("ps%d" % i, [128, 512], F32)) for i in range(8)]

    R = {}

    def res(name, const=False):
        if name not in R:
            R[name] = Res(const)
        return R[name]

    rPS = [res("ps%d" % i) for i in range(8)]
    for r_ in rPS:
        r_.excl = True
    rRING = [res("ring%d" % i) for i in range(NSLOT)]
    rXFm = [res("XF%d" % m) for m in range(KD)]
    rXB = res("XB")
    rXS = [res("XS0"), res("XS0")]
    rC = res("consts")
    rCd, rWSA, rCp, rCv = res("Cd"), res("WSA"), res("Cp"), res("Cv")

    def phv(off, n):
        return PH[:, off:off + n]

    VNT = phv(0, 12288).rearrange("p (c t) -> p c t", c=CV)
    OT = VNT
    VN = phv(12288, 12288).rearrange("p (b c) -> p b c", b=4)
    GTB = phv(0, 4336)
    DG = [phv(4336 + i * 3968, 3968).rearrange("p (k c) -> p k c", k=NTAP) for i in range(2)]
    ST = phv(12272, 4096).rearrange("p (m t) -> p m t", m=KD)
    YC = phv(16384, 8192).bitcast(F32).rearrange("p (m t) -> p m t", m=KD)
    HT = [phv(i * 3584, 3584).rearrange("p (f t) -> p f t", f=7) for i in range(2)]
    SS = [phv(7168 + i * 512, 512) for i in range(2)]
    TT = [phv(8192 + i * 512, 512) for i in range(2)]
    GBC = [phv(9216 + i * 1024, 1024).bitcast(F32) for i in range(2)]
    rVNT, rVN, rGTB, rST, rYC = res("VNT"), res("VN"), res("GTB"), res("ST"), res("YC")
    rVNTc = [res("VNTc%d" % c) for c in range(CV)]
    rDG = [res("DG0"), res("DG1")]
    rHT = [res("HT0"), res("HT1")]
    rSS = [res("SS0"), res("SS1")]
    rTT = [res("TT0"), res("TT1")]
    rGBC = [res("GBC0"), res("GBC1")]
    rT1 = [res("T1_0"), res("T1_1")]
    rT2 = [res("T2_0"), res("T2_1")]
    rSQ = [res("SQ0"), res("SQ1")]
    rZB = [res("ZB0"), res("ZB1")]
    rSTAT = res("STAT")
    rHIST, rGL, rGTS, rRT = res("HIST"), res("GL"), res("GTS"), res("RT")
    WA, WB = CAP, WT - CAP
    A_SB, B_SB = list(range(CAP // 128)), list(range(CAP // 128, 4))
    HTA = [phv(i * 1792, 7 * WA).rearrange("p (f t) -> p f t", f=7) for i in range(2)] if WA <= 256 else None
    HTBv = phv(3584, 7 * WB).rearrange("p (f t) -> p f t", f=7)
    SSA = [phv(6272 + i * 384, 384) for i in range(2)]
    XTOK = phv(7040, 4096).rearrange("p (b c) -> p b c", b=4)
    PE_ = [phv(11136 + i * 512, 512) for i in range(4)]
    Q_ = [phv(13184 + i * 512, 512) for i in range(4)]
    XG = [phv(15232 + i * 4096, 4096).rearrange("p (k t) -> p k t", k=KD) for i in range(2)]
    GBC1 = phv(23424, 1024).bitcast(F32)
    ACC = [PM[:, i * 2048:(i + 1) * 2048].bitcast(F32) for i in range(4)]
    ACCB = [PM[:, 8192 + i * 1024: 8192 + (i + 1) * 1024] for i in range(4)]
    POSBC = PM[:, 12288:13312].bitcast(F32)
    rXTOK, rPE_, rGBC1, rPOSBC, rHTB = res("XTOK"), res("PE_"), res("GBC1"), res("POSBC"), res("HTB")
    rQ = [res("Q%d" % i) for i in range(4)]
    rXG = [res("XG0"), res("XG1")]
    rHTA = [res("HTA0"), res("HTA1")]
    rSSA = [res("SSA0"), res("SSA1")]
    rACC = [res("ACC%d" % i) for i in range(4)]
    rACCB = [res("ACCB%d" % i) for i in range(4)]
    rMSK, rPOS, rCNT, rPOST = res("MSK"), res("POS"), res("CNT"), res("POST")

    def vec_load(dst, src_row, n):
        dma("sp", dst, src_row.rearrange("o (c p) -> p (o c)", p=128), [], [rCd], "const",
            allow_slow_non_contiguous=True)

    skip = dbg.get("skip", ())
    if 'vec' not in skip:
        vec_load(VB_IN[:], b_in, 48)
        vec_load(VLNVG[:], lnv_g, CV)
        vec_load(VLNVB[:], lnv_b, CV)
        vec_load(VBOUT[:], b_out, KD)
        vec_load(VBPW1[:], b_pw1, 16)
        vec_load(VBDW[:], b_dw, KD)
        vec_load(VCLG[:], cln_g, KD)
        vec_load(VCLB[:], cln_b, KD)
        vec_load(VBPW2[:], b_pw2, KD)
        vec_load(VLNG[:], ln_g, 32)
        vec_load(VLNB[:], ln_b, 32)
    if 'wdw' not in skip:
        for k in range(NTAP):
            dma("sp", WDW[:, :, k], w_dw[k:k + 1, :].rearrange("o (c p) -> p (o c)", p=128), [], [rCd], "const",
                allow_slow_non_contiguous=True)
    if 'wr' not in skip:
        dma("sp", WR[:], w_r.rearrange("(k p) e -> p k e", p=128), [], [rCd], "const")
    if 'br' not in skip:
        dma("sp", BR[:], b_r.partition_broadcast(128), [], [rCd], "const")
    if 'wsa' not in skip:
        dma("sp", WSA[:], w_s.rearrange("g i j -> i g j"), [], [rWSA], "wsa")
    if 'browf' not in skip:
        dma("sp", BROWF[:, 0, :], b_s.rearrange("(o g) i -> o (g i)", o=1), [], [rCd], "const")
        for h in range(2):
            dma("sp", BROWF[:, 1, :].rearrange("o (g i) -> o g i", g=8)[:, :, h * 64:(h + 1) * 64],
                b_s[:, 0:64].rearrange("(o g) i -> o g i", o=1), [], [rCd], "const")
    if 'pool' not in skip:
        op("pool", lambda e: e.memset(identF[:], 0.0), [], [rCp])
        op("pool", lambda e: e.affine_select(out=identF[:], in_=identF[:], compare_op=ALU.not_equal, fill=1.0,
                                             base=0, pattern=[[-1, 128]], channel_multiplier=1), [], [rCp])
    if 'sel' not in skip:
        op("pool", lambda e: e.memset(SEL[:], 0.0), [], [rCp])
        for ee in range(NE):
            op("pool", lambda e, ee=ee: e.affine_select(out=SEL[:, ee, :], in_=SEL[:, ee, :], compare_op=ALU.not_equal,
                                                        fill=1.0, base=-ee, pattern=[[0, 128]], channel_multiplier=1),
               [], [rCp])
    if 'pool2' not in skip:
        op("pool", lambda e: e.memset(EPST[:], EPS), [], [rCp])
        op("pool", lambda e: e.memset(onesB[:], 1.0), [], [rCp])
        op("pool", lambda e: e.memset(HIST[:], 0.0), [], [rHIST])
        op("pool", lambda e: e.iota(out=IOTA_I[:], pattern=[[1, WT]], base=0, channel_multiplier=0), [], [rCp])
        op("pool", lambda e: e.iota(out=SLOT_I[:], pattern=[[128, 4]], base=0, channel_multiplier=1), [], [rCp])
        op("pool", lambda e: e.memset(UTRI_F[:], 1.0), [], [rCp])
        op("pool", lambda e: e.affine_select(out=UTRI_F[:], in_=UTRI_F[:], compare_op=ALU.is_gt, fill=0.0, base=0,
                                             pattern=[[1, 128]], channel_multiplier=-1), [], [rCp])
        op("dve", lambda e: e.tensor_copy(out=IOTA_F[:], in_=IOTA_I[:]), [rCp], [rCv])
        op("dve", lambda e: e.tensor_copy(out=SLOTID[:], in_=SLOT_I[:]), [rCp], [rCv])
        op("dve", lambda e: e.tensor_copy(out=UTRI[:], in_=UTRI_F[:]), [rCp], [rCv])
    if 'identb' not in skip:
        op("dve", lambda e: e.tensor_copy(out=identB[:], in_=identF[:]), [rCp], [rCv])
    if 'lnga' not in skip:
        op("dve", lambda e: e.tensor_scalar(out=VLNGA[:], in0=VLNG[:], scalar1=ALPHA, scalar2=None, op0=ALU.mult), [rCd], [rCv])
        op("dve", lambda e: e.tensor_scalar(out=VLNBA[:], in0=VLNB[:], scalar1=ALPHA, scalar2=None, op0=ALU.mult), [rCd], [rCv])
    if 'brow' not in skip:
        op("dve", lambda e: e.tensor_copy(out=BROW[:, 0, :], in_=BROWF[:, 0, :]), [rCd, rCv], [rCv])
        op("dve", lambda e: e.tensor_copy(out=BROW[:, 2, :], in_=BROWF[:, 1, :]), [rCd, rCv], [rCv])
        op("dve", lambda e: e.tensor_tensor(out=BROWT[:, 0, :], in0=BROWF[:, 0, :], in1=BROW[:, 0, :], op=ALU.subtract), [rCd, rCv], [rCv])
        op("dve", lambda e: e.tensor_tensor(out=BROWT[:, 1, :], in0=BROWF[:, 1, :], in1=BROW[:, 2, :], op=ALU.subtract), [rCd, rCv], [rCv])
        op("dve", lambda e: e.tensor_copy(out=BROW[:, 1, :], in_=BROWT[:, 0, :]), [rCd, rCv], [rCv])
        op("dve", lambda e: e.tensor_copy(out=BROW[:, 3, :], in_=BROWT[:, 1, :]), [rCd, rCv], [rCv])
    if 'wstp' not in skip:
        op("dve", lambda e: e.memset(WST[0][:], 0.0), [], [rCv])
        op("dve", lambda e: e.memset(WST[1][:], 0.0), [], [rCv])
        for g in range(8):
            bank = g % 2
            op("pe", lambda e, g=g, bank=bank: e.transpose(out=PS[bank][:, 0:128], in_=WSA[:, g, :], identity=identF[:]),
               [rWSA, rCp], [rPS[bank]])
            op("dve", lambda e, g=g, bank=bank: e.tensor_copy(out=WST[0][0:64, g, :], in_=PS[bank][0:64, 0:128]),
               [rPS[bank]], [rCv])
            op("dve", lambda e, g=g, bank=bank: e.tensor_copy(out=WST[0][64:128, g, 64:128], in_=PS[bank][64:128, 64:128]),
               [rPS[bank]], [rCv])
    if 'wsts' not in skip:
        op("dve", lambda e: e.memset(WSA[:], 0.0), [], [rWSA])
        dma("sp", WSA[0:64, :, 0:64], w_s[:, 0:64, 0:64].rearrange("g i j -> i g j"), [], [rWSA], "wsa")
        dma("sp", WSA[64:128, :, 64:128], w_s[:, 0:64, 0:64].rearrange("g i j -> i g j"), [], [rWSA], "wsa")
        for g in range(8):
            bank = g % 2
            op("pe", lambda e, g=g, bank=bank: e.transpose(out=PS[bank][:, 0:128], in_=WSA[:, g, :], identity=identF[:]),
               [rWSA, rCp], [rPS[bank]])
            op("dve", lambda e, g=g, bank=bank: e.tensor_copy(out=WST[1][:, g, :], in_=PS[bank][:, 0:128]),
               [rPS[bank]], [rCv])
    B.barrier(["pe", "act", "dve", "pool"])
    rC.w = None
    rC.r = {}
    rC.const = True

    ring_state = {"i": 0, "piece": 0, "first": True}
    NPIECE = 126
    WSCR = nc.dram_tensor("wscr", [NPIECE, 128, SLOT], BF16, kind="Internal").ap()
    rSCR = [Res() for _ in range(NPIECE)]

    def wload(src3, shape):
        i = ring_state["i"] % NSLOT
        ring_state["i"] += 1
        j = ring_state["piece"]
        ring_state["piece"] += 1
        a, b = shape
        flat = RING[i][:, 0:a * b]
        view = flat.rearrange("p (a b) -> p a b", a=a)
        if ring_state["first"] or dbg.get("noscr"):
            dma("pool", view, src3, [], [rRING[i]], "rgs%d" % i)
            if not dbg.get("noscr"):
                dma("sp", WSCR[j, :, 0:a * b], flat, [rRING[i]], [rSCR[j]], "wst%d" % i)
        else:
            dma("sp", flat, WSCR[j, :, 0:a * b], [rSCR[j]], [rRING[i]], "rgh%d" % i)
        return view, rRING[i]

    w_in_v = w_in.rearrange("(k p) n -> p k n", p=128)
    w_out_v = w_out.rearrange("(c p) n -> p c n", p=128)
    w_pw1_v = w_pw1.rearrange("(k p) n -> p k n", p=128)
    w_pw2_v = w_pw2.rearrange("(k p) n -> p k n", p=128)

    def layer_norm(zs, W, nch, srcs_bf=None, outs=None, fence=()):
        inv = 1.0 / (nch * 128)
        for m in range(nch):
            z, rz = zs[m]
            s = m % 2
            fz = list(fence) if m == 0 else []
            op("act", lambda e, z=z, s=s: e.activation(out=SQ[s][:, 0:W], in_=z, func=AF.Square), [rz], [rSQ[s]] + fz)
            if srcs_bf is None:
                op("dve", lambda e, z=z, s=s: e.tensor_copy(out=ZB[s][:, 0:W], in_=z), [rz], [rZB[s]] + fz)
                zb, rzb = ZB[s][:, 0:W], rZB[s]
            else:
                zb, rzb = srcs_bf[m]
            op("pe", lambda e, zb=zb, m=m: e.matmul(PS[6][:, 0:W], lhsT=onesB[:], rhs=zb, start=(m == 0), stop=(m == nch - 1)),
               [rzb, rC], [rPS[6]], signal=(m == nch - 1))
            op("pe", lambda e, s=s, m=m: e.matmul(PS[7][:, 0:W], lhsT=onesB[:], rhs=SQ[s][:, 0:W], start=(m == 0), stop=(m == nch - 1)),
               [rSQ[s], rC], [rPS[7]], signal=True)
        op("dve", lambda e: e.tensor_scalar(out=MEAN[:, 0:W], in0=PS[6][:, 0:W], scalar1=inv, scalar2=None, op0=ALU.mult),
           [rPS[6]], [rSTAT])
        op("dve", lambda e: e.tensor_tensor(out=VAR[:, 0:W], in0=MEAN[:, 0:W], in1=MEAN[:, 0:W], op=ALU.mult), [rSTAT], [rSTAT])
        op("dve", lambda e: e.scalar_tensor_tensor(out=VAR[:, 0:W], in0=PS[7][:, 0:W], scalar=inv, in1=VAR[:, 0:W],
                                                   op0=ALU.mult, op1=ALU.subtract), [rPS[7], rSTAT], [rSTAT])
        op("act", lambda e: e.activation(out=RSTD[:, 0:W], in_=VAR[:, 0:W], func=AF.Sqrt, bias=EPST[:], scale=1.0),
           [rSTAT, rC], [rSTAT])
        op("dve", lambda e: e.reciprocal(out=RSTD[:, 0:W], in_=RSTD[:, 0:W]), [rSTAT], [rSTAT])
        for m in range(nch):
            z, rz = zs[m]
            s = m % 2
            op("dve", lambda e, z=z, s=s: e.tensor_tensor(out=T1[s][:, 0:W], in0=z, in1=MEAN[:, 0:W], op=ALU.subtract),
               [rz, rSTAT], [rT1[s]])
            op(POOLENG, lambda e, s=s: e.tensor_tensor(out=T2[s][:, 0:W], in0=T1[s][:, 0:W], in1=RSTD[:, 0:W], op=ALU.mult),
               [rT1[s], rSTAT], [rT2[s]])
            outs(m, T2[s][:, 0:W], rT2[s])

    def deepnorm_ln(W, lnidx, scaled, make_xb=True):
        gsrc, bsrc = (VLNGA, VLNBA) if scaled else (VLNG, VLNB)

        def outs(m, t2, rt2):
            col = lnidx * 8 + m
            op("act", lambda e: e.activation(out=XF[:, m, 0:W], in_=t2, func=AF.Identity,
                                             bias=bsrc[:, col:col + 1], scale=gsrc[:, col:col + 1]), [rt2, rC], [rXFm[m]])
            if make_xb:
                if POOLENG == "pool":
                    op("pool", lambda e: e.tensor_scalar(out=XB[:, m, 0:W], in0=t2, scalar1=VLNG[:, col:col + 1],
                                                         scalar2=VLNB[:, col:col + 1], op0=ALU.mult, op1=ALU.add), [rt2, rC], [rXB])
                else:
                    op("act", lambda e: e.activation(out=XB[:, m, 0:W], in_=t2, func=AF.Identity,
                                                     bias=VLNB[:, col:col + 1], scale=VLNG[:, col:col + 1]), [rt2, rC], [rXB])
        layer_norm([(XF[:, m, 0:W], rXFm[m]) for m in range(KD)], W, KD, None, outs,
                   fence=(rACC + rACCB + [rPOSBC]) if lnidx == 3 else ())

    def load_tile(tok0, W):
        nb = W // 128
        LT = _os.environ.get("LT", "dpav")
        for b in range(nb):
            s = b % 2
            if "d" in LT:
                dma("sp", XS[s][:], xin[tok0 + b * 128: tok0 + (b + 1) * 128, :], [], [rXS[s]], "xs%d" % s)
            if "p" in LT:
                for m in range(KD):
                    bank = m // 4
                    op("pe", lambda e, m=m, s=s, bank=bank: e.transpose(out=PS[bank][:, (m % 4) * 128:(m % 4 + 1) * 128],
                                                                        in_=XS[s][:, m * 128:(m + 1) * 128], identity=identF[:]),
                       [rXS[s], rC], [rPS[bank]], signal=(m % 4 == 3))
            for h in range(2):
                src = PS[h][:].rearrange("p (a t) -> p a t", a=4)
                if "a" in LT:
                    op("act", lambda e, h=h, b=b, src=src: e.mul(out=XF[:, 4 * h:4 * h + 4, b * 128:(b + 1) * 128], in_=src, mul=ALPHA),
                       [rPS[h]], rXFm[4 * h:4 * h + 4])
                if "v" in LT:
                    op("dve", lambda e, h=h, b=b, src=src: e.tensor_copy(out=XB[:, 4 * h:4 * h + 4, b * 128:(b + 1) * 128], in_=src),
                       [rPS[h]] + ([rXFm[4 * h]] if _os.environ.get("SER") else []), [rXB])

    def store_tile(tok0, W):
        nb = W // 128
        for b in range(nb):
            s = b % 2
            for m in range(KD):
                bank = 2 + m // 4
                op("pe", lambda e, m=m, b=b, bank=bank: e.transpose(out=PS[bank][:, (m % 4) * 128:(m % 4 + 1) * 128],
                                                                    in_=XF[:, m, b * 128:(b + 1) * 128], identity=identF[:]),
                   [rXFm[m], rC], [rPS[bank]], signal=(m % 4 == 3))
            op("act", lambda e, s=s: e.copy(out=XS[s][:, 0:512], in_=PS[2][:]), [rPS[2]], [rXS[s]])
            op("dve", lambda e, s=s: e.tensor_copy(out=XS[s][:, 512:1024], in_=PS[3][:]), [rPS[3]], [rXS[s]])
            dma("sp", y_o[tok0 + b * 128: tok0 + (b + 1) * 128, :], XS[s][:], [rXS[s]], [], "ys%d" % s)

    def gmlp(W, sample, gv_rows):
        nb = W // 128
        wst = WST[1 if sample else 0]
        bro = 2 if sample else 0
        vps = 0
        for pc in range(4):
            wv, rw = wload(w_in_v[:, :, DV + pc * 768: DV + (pc + 1) * 768], (KD, 768))
            for cc in range(6):
                c = pc * 6 + cc
                bank = vps % 2
                vps += 1
                for k in range(KD):
                    op("pe", lambda e, k=k, cc=cc, bank=bank, wv=wv: e.matmul(PS[bank][:, 0:W], lhsT=wv[:, k, cc * 128:(cc + 1) * 128],
                                                                               rhs=XB[:, k, 0:W], start=(k == 0), stop=(k == KD - 1)),
                       [rw, rXB], [rPS[bank]], signal=(k == KD - 1))
                op("act", lambda e, c=c, bank=bank: e.activation(out=VNT[:, c, 0:W], in_=PS[bank][:, 0:W], func=AF.Gelu,
                                                                 bias=VB_IN[:, 24 + c:25 + c], scale=1.0),
                   [rPS[bank], rC], [rVNTc[c]])

        def outs(c, t2, rt2):
            op("act", lambda e: e.activation(out=VNT[:, c, 0:W], in_=t2, func=AF.Identity,
                                             bias=VLNVB[:, c:c + 1], scale=VLNVG[:, c:c + 1]), [rt2, rC], [rVNTc[c]])
        zs = [(VNT[:, c, 0:W], rVNTc[c]) for c in range(CV)]
        layer_norm(zs, W, CV, zs, outs)
        tb = 0
        for b in range(nb):
            for c8 in range(3):
                bank = 2 + tb % 2
                tb += 1
                psb = PS[bank][:].bitcast(BF16)
                for cc in range(8):
                    c = c8 * 8 + cc
                    op("pe", lambda e, c=c, cc=cc, b=b, psb=psb: e.transpose(out=psb[:, cc * 128:(cc + 1) * 128],
                                                                               in_=VNT[:, c, b * 128:(b + 1) * 128], identity=identB[:]),
                       [rVNTc[c], rC], [rPS[bank]], signal=(cc == 7))
                eng = "act" if c8 == 1 else "dve"
                if eng == "act":
                    op("act", lambda e, b=b, c8=c8, psb=psb: e.copy(out=VN[:, b, c8 * 1024:(c8 + 1) * 1024], in_=psb), [rPS[bank]], [rVN])
                else:
                    op("dve", lambda e, b=b, c8=c8, psb=psb: e.tensor_copy(out=VN[:, b, c8 * 1024:(c8 + 1) * 1024], in_=psb), [rPS[bank]], [rVN])
            if b in gv_rows:
                r0 = gv_rows[b]
                dma("pool", gv_o[r0:r0 + 128, :], VN[:, b, :], [rVN], [], "gv")
        ups = 0
        for pc in range(4):
            wu, rw = wload(w_in_v[:, :, pc * 768:(pc + 1) * 768], (KD, 768))
            for cc in range(6):
                c = pc * 6 + cc
                g = c // 3
                bank = ups % 2
                mb = 2 + ups % 2
                s = ups % 2
                ups += 1
                for k in range(KD):
                    op("pe", lambda e, k=k, cc=cc, bank=bank, wu=wu: e.matmul(PS[bank][:, 0:W], lhsT=wu[:, k, cc * 128:(cc + 1) * 128],
                                                                               rhs=XB[:, k, 0:W], start=(k == 0), stop=(k == KD - 1)),
                       [rw, rXB], [rPS[bank]], signal=(k == KD - 1))
                op("act", lambda e, c=c, bank=bank, s=s: e.activation(out=ZB[s][:, 0:W], in_=PS[bank][:, 0:W], func=AF.Gelu,
                                                                      bias=VB_IN[:, c:c + 1], scale=1.0),
                   [rPS[bank], rC], [rZB[s]])
                for b in range(nb):
                    cols = slice(b * 128, (b + 1) * 128)
                    op("pe", lambda e, b=b, c=c, g=g, mb=mb, cols=cols: e.matmul(PS[mb][:, cols], lhsT=VN[:, b, c * 128:(c + 1) * 128],
                                                                                  rhs=wst[:, g, :], start=True, stop=False),
                       [rVN, rC], [rPS[mb]], signal=False)
                    op("pe", lambda e, g=g, mb=mb, cols=cols: e.matmul(PS[mb][:, cols], lhsT=onesB[0:1, :],
                                                                       rhs=BROW[0:1, bro, g * 128:(g + 1) * 128], start=False, stop=False),
                       [rC], [rPS[mb]], signal=False)
                    op("pe", lambda e, g=g, mb=mb, cols=cols: e.matmul(PS[mb][:, cols], lhsT=onesB[0:1, :],
                                                                       rhs=BROW[0:1, bro + 1, g * 128:(g + 1) * 128], start=False, stop=True),
                       [rC], [rPS[mb]], signal=(b == nb - 1))
                op("dve", lambda e, c=c, mb=mb, s=s: e.tensor_tensor(out=OT[:, c, 0:W], in0=ZB[s][:, 0:W], in1=PS[mb][:, 0:W], op=ALU.mult),
                   [rZB[s], rPS[mb]], [rVNTc[c]])
        for pc in range(4):
            wo, rw = wload(w_out_v[:, :, pc * 256:(pc + 1) * 256], (CV, 256))
            for mm in range(2):
                m = pc * 2 + mm
                bank = 4 + m % 2
                for c in range(CV):
                    op("pe", lambda e, c=c, mm=mm, bank=bank, wo=wo: e.matmul(PS[bank][:, 0:W], lhsT=wo[:, c, mm * 128:(mm + 1) * 128],
                                                                               rhs=OT[:, c, 0:W], start=(c == 0), stop=(c == CV - 1)),
                       [rw, rVNTc[c]], [rPS[bank]], signal=(c == CV - 1))
                op("dve", lambda e, m=m, bank=bank: e.scalar_tensor_tensor(out=XF[:, m, 0:W], in0=PS[bank][:, 0:W], scalar=VBOUT[:, m:m + 1],
                                                                           in1=XF[:, m, 0:W], op0=ALU.add, op1=ALU.add),
                   [rPS[bank], rC, rXFm[m]], [rXFm[m]])

    def experts(W, wg_list, gated):
        cnt = 0
        for ei, (gv, uv, dv) in enumerate(wg_list):
            if gated:
                gs = ei % 2
                op("pe", lambda e, ei=ei: e.matmul(PS[6][:, 0:W], lhsT=SEL[:, ei, :], rhs=GTS[0:8, 0:W], start=True, stop=True),
                   [rC, rGTS], [rPS[6]])
                op("act", lambda e, gs=gs: e.copy(out=GBC[gs][:, 0:W], in_=PS[6][:, 0:W]), [rPS[6]], [rGBC[gs]])
            for gi in range(4):
                wgp, rwg = wload(gv[:, :, gi * 896:(gi + 1) * 896], (KD, 896))
                wup, rwu = wload(uv[:, :, gi * 896:(gi + 1) * 896], (KD, 896))
                wdp, rwd = wload(dv[:, gi * 7:(gi + 1) * 7, :], (7, D))
                hs = cnt % 2
                cnt += 1
                for fc in range(7):
                    s = fc % 2
                    gb, ub = fc % 2, 2 + fc % 2
                    for k in range(KD):
                        op("pe", lambda e, k=k, fc=fc, gb=gb, wgp=wgp: e.matmul(PS[gb][:, 0:W], lhsT=wgp[:, k, fc * 128:(fc + 1) * 128],
                                                                                 rhs=XB[:, k, 0:W], start=(k == 0), stop=(k == KD - 1)),
                           [rwg, rXB], [rPS[gb]], signal=(k == KD - 1))
                    for k in range(KD):
                        op("pe", lambda e, k=k, fc=fc, ub=ub, wup=wup: e.matmul(PS[ub][:, 0:W], lhsT=wup[:, k, fc * 128:(fc + 1) * 128],
                                                                                 rhs=XB[:, k, 0:W], start=(k == 0), stop=(k == KD - 1)),
                           [rwu, rXB], [rPS[ub]], signal=(k == KD - 1))
                    op("act", lambda e, gb=gb, s=s: e.activation(out=SS[s][:, 0:W], in_=PS[gb][:, 0:W], func=AF.Silu), [rPS[gb]], [rSS[s]])
                    if gated:
                        op("dve", lambda e, ub=ub, s=s: e.tensor_tensor(out=TT[s][:, 0:W], in0=SS[s][:, 0:W], in1=PS[ub][:, 0:W], op=ALU.mult),
                           [rSS[s], rPS[ub]], [rTT[s]])
                        op("dve", lambda e, s=s, hs=hs, fc=fc, gs=gs: e.tensor_tensor(out=HT[hs][:, fc, 0:W], in0=TT[s][:, 0:W],
                                                                                      in1=GBC[gs][:, 0:W], op=ALU.mult),
                           [rTT[s], rGBC[gs]], [rHT[hs]])
                    else:
                        op("dve", lambda e, ub=ub, s=s, hs=hs, fc=fc: e.tensor_tensor(out=HT[hs][:, fc, 0:W], in0=SS[s][:, 0:W],
                                                                                      in1=PS[ub][:, 0:W], op=ALU.mult),
                           [rSS[s], rPS[ub]], [rHT[hs]])
                for m in range(KD):
                    yb = 4 + m % 2
                    for fc in range(7):
                        op("pe", lambda e, fc=fc, m=m, yb=yb, hs=hs, wdp=wdp: e.matmul(PS[yb][:, 0:W], lhsT=wdp[:, fc, m * 128:(m + 1) * 128],
                                                                                        rhs=HT[hs][:, fc, 0:W], start=(fc == 0), stop=(fc == 6)),
                           [rwd, rHT[hs]], [rPS[yb]], signal=(fc == 6))
                    op("dve", lambda e, m=m, yb=yb: e.tensor_tensor(out=XF[:, m, 0:W], in0=XF[:, m, 0:W], in1=PS[yb][:, 0:W], op=ALU.add),
                       [rPS[yb], rXFm[m]], [rXFm[m]])

    def experts_sparse(wg_list):
        IFE = ["pe", "act", "dve"]
        cnt = 0

        def gather(ei, xg, rxg, c0, width):
            kper = max(1, 512 // width)
            gi_ = 0
            for k0 in range(0, KD, kper):
                bank = 6 + gi_ % 2
                gi_ += 1
                ks = list(range(k0, min(KD, k0 + kper)))
                for kk, k in enumerate(ks):
                    for tb in range(4):
                        op("pe", lambda e, k=k, kk=kk, tb=tb, bank=bank: e.matmul(PS[bank][:, kk * width:(kk + 1) * width],
                                                                                  lhsT=XTOK[:, tb, k * 128:(k + 1) * 128],
                                                                                  rhs=PE_[tb][:, c0:c0 + width], start=(tb == 0), stop=(tb == 3)),
                           [rXTOK, rPE_], [rPS[bank]], signal=(tb == 3 and kk == len(ks) - 1))
                src = PS[bank][:, 0:len(ks) * width].rearrange("p (a t) -> p a t", a=len(ks))
                dst = xg[:, ks[0]:ks[-1] + 1, c0:c0 + width]
                if gi_ % 2 == 0:
                    op("act", lambda e, src=src, dst=dst: e.copy(out=dst, in_=src), [rPS[bank]], [rxg])
                else:
                    op("dve", lambda e, src=src, dst=dst: e.tensor_copy(out=dst, in_=src), [rPS[bank]], [rxg])

        def ffn_block(gi, xg, rxg, c0, width, ht, rht, sbs, wgp, rwg, wup, rwu, wdp, rwd):
            for fc in range(7):
                s_ = fc % 2
                gb, ub = fc % 2, 2 + fc % 2
                for k in range(KD):
                    op("pe", lambda e, k=k, fc=fc, gb=gb: e.matmul(PS[gb][:, 0:width], lhsT=wgp[:, k, fc * 128:(fc + 1) * 128],
                                                                    rhs=xg[:, k, c0:c0 + width], start=(k == 0), stop=(k == KD - 1)),
                       [rwg, rxg], [rPS[gb]], signal=(k == KD - 1))
                for k in range(KD):
                    op("pe", lambda e, k=k, fc=fc, ub=ub: e.matmul(PS[ub][:, 0:width], lhsT=wup[:, k, fc * 128:(fc + 1) * 128],
                                                                    rhs=xg[:, k, c0:c0 + width], start=(k == 0), stop=(k == KD - 1)),
                       [rwu, rxg], [rPS[ub]], signal=(k == KD - 1))
                op("act", lambda e, gb=gb, s_=s_: e.activation(out=SSA[s_][:, 0:width], in_=PS[gb][:, 0:width], func=AF.Silu),
                   [rPS[gb]], [rSSA[s_]])
                op("dve", lambda e, ub=ub, s_=s_, fc=fc: e.tensor_tensor(out=ht[:, fc, 0:width], in0=SSA[s_][:, 0:width], in1=PS[ub][:, 0:width],
                                                                         op=ALU.mult), [rSSA[s_], rPS[ub]], [rht])
            yi = 0
            for j, sbk in enumerate(sbs):
                for half in range(2):
                    yb = 4 + yi % 2
                    yi += 1
                    for fc in range(7):
                        op("pe", lambda e, fc=fc, j=j, half=half, yb=yb: e.matmul(PS[yb][:, 0:512], lhsT=ht[:, fc, j * 128:(j + 1) * 128],
                                                                                  rhs=wdp[:, fc, half * 512:(half + 1) * 512],
                                                                                  start=(fc == 0), stop=(fc == 6)),
                           [rwd, rht], [rPS[yb]], signal=(fc == 6))
                    dst = ACC[sbk][:, half * 512:(half + 1) * 512]
                    if gi == 0:
                        op("dve", lambda e, dst=dst, yb=yb: e.tensor_copy(out=dst, in_=PS[yb][:, 0:512]), [rPS[yb]], [rACC[sbk]])
                    else:
                        op("dve", lambda e, dst=dst, yb=yb: e.tensor_tensor(out=dst, in0=dst, in1=PS[yb][:, 0:512], op=ALU.add),
                           [rPS[yb], rACC[sbk]], [rACC[sbk]])

        def scatter(sbs):
            for sbk in sbs:
                op("act", lambda e, sbk=sbk: e.copy(out=ACCB[sbk][:], in_=ACC[sbk][:]), [rACC[sbk]], [rACCB[sbk]])
            for m in range(KD):
                bank = 6 + m % 2
                for j, sbk in enumerate(sbs):
                    op("pe", lambda e, m=m, j=j, sbk=sbk, bank=bank, n=len(sbs): e.matmul(PS[bank][:, 0:512], lhsT=ACCB[sbk][:, m * 128:(m + 1) * 128],
                                                                                          rhs=Q_[sbk][:], start=(j == 0), stop=(j == n - 1)),
                       [rACCB[sbk], rQ[sbk]], [rPS[bank]], signal=(j == len(sbs) - 1))
                op("dve", lambda e, m=m, bank=bank: e.tensor_tensor(out=XF[:, m, 0:512], in0=XF[:, m, 0:512], in1=PS[bank][:, 0:512], op=ALU.add),
                   [rPS[bank], rXFm[m]], [rXFm[m]])

        def build_q(sbs):
            for sbk in sbs:
                op("dve", lambda e, sbk=sbk: e.scalar_tensor_tensor(out=Q_[sbk][:], in0=POSBC[:], scalar=SLOTID[:, sbk:sbk + 1], in1=GBC1[:],
                                                                    op0=ALU.is_equal, op1=ALU.mult), [rPOSBC, rGBC1, rC], [rQ[sbk]])

        for ei, (gv, uv, dv) in enumerate(wg_list):
            xg, rxg = XG[ei % 2], rXG[ei % 2]
            for eng in IFE:
                op(eng, lambda e, eng=eng, ei=ei: e.reg_load(B.regs[eng], CNT[0:1, ei:ei + 1]), [rCNT], [], signal=False)
            lnf = (rT1 + rT2 + rSQ + rZB + [rSTAT]) if ei == 0 else []
            for tb in range(4):
                op("dve", lambda e, tb=tb, ei=ei: e.tensor_scalar(out=PE_[tb][:], in0=IOTA_F[:], scalar1=POS[:, tb, ei:ei + 1],
                                                                  scalar2=MSKF[:, tb, ei:ei + 1], op0=ALU.is_equal, op1=ALU.mult),
                   [rPOS, rMSK, rC], [rPE_] + (lnf if tb == 0 else []))
            op("pe", lambda e, ei=ei: e.matmul(PS[6][:, 0:512], lhsT=SEL[:, ei, :], rhs=GTS[0:8, 0:512], start=True, stop=True),
               [rC, rGTS], [rPS[6]])
            op("pe", lambda e, ei=ei: e.matmul(PS[7][:, 0:512], lhsT=SEL[:, ei, :], rhs=POST[0:8, 0:512], start=True, stop=True),
               [rC, rPOST], [rPS[7]])
            op("act", lambda e: e.copy(out=GBC1[:], in_=PS[6][:, 0:512]), [rPS[6]], [rGBC1] + lnf)
            op("act", lambda e: e.copy(out=POSBC[:], in_=PS[7][:, 0:512]), [rPS[7]], [rPOSBC])
            build_q(A_SB)
            gather(ei, xg, rxg, 0, WA)
            if B_SB:
                B.if_begin(IFE, ei, CAP)
                build_q(B_SB)
                gather(ei, xg, rxg, WA, WB)
                B.if_end()
            for gi in range(4):
                wgp, rwg = wload(gv[:, :, gi * 896:(gi + 1) * 896], (KD, 896))
                wup, rwu = wload(uv[:, :, gi * 896:(gi + 1) * 896], (KD, 896))
                wdp, rwd = wload(dv[:, gi * 7:(gi + 1) * 7, :], (7, D))
                hs = cnt % 2
                cnt += 1
                ffn_block(gi, xg, rxg, 0, WA, HTA[hs], rHTA[hs], A_SB, wgp, rwg, wup, rwu, wdp, rwd)
                if B_SB:
                    B.if_begin(IFE, ei, CAP)
                    ffn_block(gi, xg, rxg, WA, WB, HTBv, rHTB, B_SB, wgp, rwg, wup, rwu, wdp, rwd)
                    B.if_end()
            scatter(A_SB)
            if B_SB:
                B.if_begin(IFE, ei, CAP)
                scatter(B_SB)
                B.if_end()

    def conv_module(W, nseq, L, sample, last_prompt):
        PADL = HW_ + L
        gtb = GTB[:, 0:KD * nseq * PADL].rearrange("p (m s t) -> p m s t", m=KD, s=nseq)
        if sample:
            for s in range(nseq):
                xs = s % 2
                dma("sp", XS[xs][0:HW_, :], cache[s], [], [rXS[xs]], "xs%d" % xs)
                bank = s % 2
                for m in range(KD):
                    op("pe", lambda e, m=m, xs=xs, bank=bank: e.transpose(out=PS[bank][:, m * HW_:(m + 1) * HW_],
                                                                          in_=XS[xs][0:HW_, m * 128:(m + 1) * 128], identity=identF[0:HW_, 0:HW_]),
                       [rXS[xs], rC], [rPS[bank]], signal=(m == KD - 1))
                op("dve", lambda e, s=s, bank=bank: e.tensor_copy(out=gtb[:, :, s, 0:HW_],
                                                                  in_=PS[bank][:, 0:KD * HW_].rearrange("p (m t) -> p m t", m=KD)),
                   [rPS[bank]], [rGTB])
        else:
            op("dve", lambda e: e.tensor_copy(out=gtb[:, :, 0, 0:HW_], in_=HIST[:]), [rHIST], [rGTB])
        pieces = {}
        for half in range(2):
            wa, rwa = wload(w_pw1_v[:, :, half * 512:(half + 1) * 512], (KD, 512))
            wgt, rwgt = wload(w_pw1_v[:, :, D + half * 512: D + (half + 1) * 512], (KD, 512))
            for mm in range(4):
                m = half * 4 + mm
                ab, gb = m % 2, 2 + m % 2
                s = m % 2
                for k in range(KD):
                    op("pe", lambda e, k=k, mm=mm, ab=ab, wa=wa: e.matmul(PS[ab][:, 0:W], lhsT=wa[:, k, mm * 128:(mm + 1) * 128],
                                                                           rhs=XB[:, k, 0:W], start=(k == 0), stop=(k == KD - 1)),
                       [rwa, rXB], [rPS[ab]], signal=(k == KD - 1))
                for k in range(KD):
                    op("pe", lambda e, k=k, mm=mm, gb=gb, wgt=wgt: e.matmul(PS[gb][:, 0:W], lhsT=wgt[:, k, mm * 128:(mm + 1) * 128],
                                                                             rhs=XB[:, k, 0:W], start=(k == 0), stop=(k == KD - 1)),
                       [rwgt, rXB], [rPS[gb]], signal=(k == KD - 1))
                op("act", lambda e, m=m, gb=gb, s=s: e.activation(out=T1[s][:, 0:W], in_=PS[gb][:, 0:W], func=AF.Sigmoid,
                                                                  bias=VBPW1[:, 8 + m:9 + m], scale=1.0), [rPS[gb], rC], [rT1[s]])
                a3 = PS[ab][:, 0:W].rearrange("p (s t) -> p s t", s=nseq)
                g3 = T1[s][:, 0:W].rearrange("p (s t) -> p s t", s=nseq)
                op("dve", lambda e, m=m, a3=a3, g3=g3: e.scalar_tensor_tensor(out=gtb[:, m, :, HW_:PADL], in0=a3, scalar=VBPW1[:, m:m + 1],
                                                                              in1=g3, op0=ALU.add, op1=ALU.mult),
                   [rPS[ab], rT1[s], rC], [rGTB])
                if sample or last_prompt:
                    op("dve", lambda e, m=m, a3=a3, g3=g3: e.scalar_tensor_tensor(out=GL[:, m, 0:nseq, :], in0=a3[:, :, L - HW_:L],
                                                                                  scalar=VBPW1[:, m:m + 1], in1=g3[:, :, L - HW_:L],
                                                                                  op0=ALU.add, op1=ALU.mult),
                       [rPS[ab], rT1[s], rC], [rGL])
        if not sample:
            op("dve", lambda e: e.tensor_copy(out=HIST[:], in_=gtb[:, :, 0, L:PADL]), [rGTB], [rHIST])
        if sample or last_prompt:
            for s in range(nseq):
                xs = s % 2
                for m in range(KD):
                    bank = 6 + m // 4
                    op("pe", lambda e, m=m, s=s, bank=bank: e.transpose(out=PS[bank][0:HW_, (m % 4) * 128:(m % 4 + 1) * 128],
                                                                        in_=GL[:, m, s, :], identity=identF[:]),
                       [rGL, rC], [rPS[bank]], signal=(m % 4 == 3))
                op("act", lambda e, xs=xs: e.copy(out=XS[xs][0:HW_, 0:512], in_=PS[6][0:HW_, :]), [rPS[6]], [rXS[xs]])
                op("dve", lambda e, xs=xs: e.tensor_copy(out=XS[xs][0:HW_, 512:1024], in_=PS[7][0:HW_, :]), [rPS[7]], [rXS[xs]])
                r0 = (HW_ * (1 + s)) if sample else 0
                dma("sp", cs_o[r0:r0 + HW_, :], XS[xs][0:HW_, :], [rXS[xs]], [], "ys%d" % xs)
        for m in range(KD):
            ds = m % 2
            for k in range(NTAP):
                op(POOLENG, lambda e, m=m, k=k, ds=ds: e.tensor_scalar(out=DG[ds][:, k, :], in0=identB[:], scalar1=WDW[:, m, k:k + 1],
                                                                       scalar2=None, op0=ALU.mult), [rC], [rDG[ds]])
            bank = 4 + m % 2
            for s in range(nseq):
                for k in range(NTAP):
                    op("pe", lambda e, m=m, k=k, s=s, ds=ds, bank=bank: e.matmul(PS[bank][:, s * L:(s + 1) * L], lhsT=DG[ds][:, k, :],
                                                                                  rhs=gtb[:, m, s, k:k + L], start=(k == 0), stop=(k == NTAP - 1)),
                       [rDG[ds], rGTB], [rPS[bank]], signal=(k == NTAP - 1 and s == nseq - 1))
            op("act", lambda e, m=m, bank=bank: e.activation(out=YC[:, m, 0:W], in_=PS[bank][:, 0:W], func=AF.Identity,
                                                             bias=VBDW[:, m:m + 1], scale=1.0), [rPS[bank], rC], [rYC])

        def outs(m, t2, rt2):
            op("act", lambda e: e.activation(out=ST[:, m, 0:W], in_=t2, func=AF.Silu, bias=VCLB[:, m:m + 1], scale=VCLG[:, m:m + 1]),
               [rt2, rC], [rST])
        layer_norm([(YC[:, m, 0:W], rYC) for m in range(KD)], W, KD, None, outs)
        for half in range(2):
            wp, rwp = wload(w_pw2_v[:, :, half * 512:(half + 1) * 512], (KD, 512))
            for mm in range(4):
                m = half * 4 + mm
                bank = 4 + m % 2
                for k in range(KD):
                    op("pe", lambda e, k=k, mm=mm, bank=bank, wp=wp: e.matmul(PS[bank][:, 0:W], lhsT=wp[:, k, mm * 128:(mm + 1) * 128],
                                                                               rhs=ST[:, k, 0:W], start=(k == 0), stop=(k == KD - 1)),
                       [rwp, rST], [rPS[bank]], signal=(k == KD - 1))
                op("dve", lambda e, m=m, bank=bank: e.scalar_tensor_tensor(out=XF[:, m, 0:W], in0=PS[bank][:, 0:W], scalar=VBPW2[:, m:m + 1],
                                                                           in1=XF[:, m, 0:W], op0=ALU.add, op1=ALU.add),
                   [rPS[bank], rC, rXFm[m]], [rXFm[m]])

    def router(W, sparse=False):
        nb = W // 128
        L_, M1, L2, M2, G = RT["L"], RT["M1"], RT["L2"], RT["M2"], RT["G"]
        for b in range(nb):
            for k in range(KD):
                op("pe", lambda e, k=k, b=b: e.matmul(PS[7][:, 0:NE], lhsT=XF[:, k, b * 128:(b + 1) * 128], rhs=WR[:, k, :],
                                                       start=(k == 0), stop=(k == KD - 1)),
                   [rXFm[k], rC], [rPS[7]], signal=(k == KD - 1))
            ops = [
                lambda e: e.tensor_tensor(out=L_[:], in0=PS[7][:, 0:NE], in1=BR[:], op=ALU.add),
                lambda e: e.tensor_reduce(out=RS["m1"][:], in_=L_[:], axis=AX.X, op=ALU.max),
                lambda e: e.tensor_scalar(out=M1[:], in0=L_[:], scalar1=RS["m1"][:], scalar2=None, op0=ALU.is_equal),
                lambda e: e.scalar_tensor_tensor(out=L2[:], in0=M1[:], scalar=-1e30, in1=L_[:], op0=ALU.mult, op1=ALU.add),
                lambda e: e.tensor_reduce(out=RS["m2"][:], in_=L2[:], axis=AX.X, op=ALU.max),
                lambda e: e.tensor_scalar(out=M2[:], in0=L2[:], scalar1=RS["m2"][:], scalar2=None, op0=ALU.is_equal),
                lambda e: e.tensor_tensor(out=RS["d"][:], in0=RS["m2"][:], in1=RS["m1"][:], op=ALU.subtract),
            ]
            for f in ops:
                op("dve", f, [rPS[7], rC, rRT], [rRT])
            op("act", lambda e: e.activation(out=RS["e"][:], in_=RS["d"][:], func=AF.Exp), [rRT], [rRT])
            ops = [
                lambda e: e.tensor_scalar(out=RS["den"][:], in0=RS["e"][:], scalar1=1.0, scalar2=None, op0=ALU.add),
                lambda e: e.reciprocal(out=RS["w1"][:], in_=RS["den"][:]),
                lambda e: e.tensor_tensor(out=RS["w2"][:], in0=RS["e"][:], in1=RS["w1"][:], op=ALU.mult),
                lambda e: e.tensor_scalar(out=G[:], in0=M1[:], scalar1=RS["w1"][:], scalar2=None, op0=ALU.mult),
                lambda e: e.scalar_tensor_tensor(out=G[:], in0=M2[:], scalar=RS["w2"][:], in1=G[:], op0=ALU.mult, op1=ALU.add),
            ]
            for f in ops:
                op("dve", f, [rRT], [rRT])
            op("pe", lambda e: e.transpose(out=PS[6][0:NE, 0:128], in_=G[:], identity=identF[:]), [rRT, rC], [rPS[6]])
            op("dve", lambda e, b=b: e.tensor_copy(out=GTS[0:8, b * 128:(b + 1) * 128], in_=PS[6][0:NE, 0:128]), [rPS[6]], [rGTS])
            if sparse:
                op("dve", lambda e, b=b: e.tensor_tensor(out=MSKF[:, b, :], in0=M1[:], in1=M2[:], op=ALU.add), [rRT], [rMSK])
                op("dve", lambda e, b=b: e.tensor_copy(out=MSK[:, b, :], in_=MSKF[:, b, :]), [rMSK], [rMSK])
        if sparse:
            for b in range(nb):
                terms = [(onesB, bb) for bb in range(b)] + [(UTRI, b)]
                for i, (lt, bb) in enumerate(terms):
                    op("pe", lambda e, lt=lt, bb=bb, b=b, i=i, n=len(terms): e.matmul(PS[6][:, b * NE:(b + 1) * NE], lhsT=lt[:], rhs=MSK[:, bb, :],
                                                                                      start=(i == 0), stop=(i == n - 1)),
                       [rMSK, rC], [rPS[6]], signal=False)
            for b in range(nb):
                op("pe", lambda e, b=b: e.matmul(PS[6][:, 4 * NE:5 * NE], lhsT=onesB[:], rhs=MSK[:, b, :], start=(b == 0), stop=(b == nb - 1)),
                   [rMSK, rC], [rPS[6]], signal=(b == nb - 1))
            op("dve", lambda e: e.tensor_copy(out=POS[:], in_=PS[6][:, 0:4 * NE].rearrange("p (b e) -> p b e", b=4)), [rPS[6]], [rPOS])
            op("dve", lambda e: e.tensor_copy(out=CNT[:], in_=PS[6][0:1, 4 * NE:5 * NE]), [rPS[6]], [rCNT])
            for b in range(nb):
                op("pe", lambda e, b=b: e.transpose(out=PS[7][0:NE, b * 128:(b + 1) * 128], in_=POS[:, b, :], identity=identF[:]),
                   [rPOS, rC], [rPS[7]], signal=(b == nb - 1))
            op("dve", lambda e: e.tensor_copy(out=POST[:], in_=PS[7][0:NE, :]), [rPS[7]], [rPOST])
            for b in range(nb):
                bank = 4 + b % 2
                psb = PS[bank][:].bitcast(BF16)
                for k in range(KD):
                    op("pe", lambda e, k=k, b=b, psb=psb: e.transpose(out=psb[:, k * 128:(k + 1) * 128], in_=XB[:, k, b * 128:(b + 1) * 128],
                                                                       identity=identB[:]),
                       [rXB, rC], [rPS[bank]], signal=(k == KD - 1))
                if b % 2 == 0:
                    op("act", lambda e, b=b, psb=psb: e.copy(out=XTOK[:, b, :], in_=psb), [rPS[bank]], [rXTOK])
                else:
                    op("dve", lambda e, b=b, psb=psb: e.tensor_copy(out=XTOK[:, b, :], in_=psb), [rPS[bank]], [rXTOK])
        for m in range(KD):
            op("act", lambda e, m=m: e.mul(out=XF[:, m, 0:W], in_=XF[:, m, 0:W], mul=ALPHA), [rXFm[m]], [rXFm[m]])

    ff_views = [(ffg.rearrange("(k p) n -> p k n", p=128), ffu.rearrange("(k p) n -> p k n", p=128),
                 ffd.rearrange("(c p) n -> p c n", p=128))]
    moe_views = [(mog[e_].rearrange("(k p) n -> p k n", p=128), mou[e_].rearrange("(k p) n -> p k n", p=128),
                  mod[e_].rearrange("(c p) n -> p c n", p=128)) for e_ in range(NE)]

    tiles = [(i * WT, WT, 1, WT, False, i == NPT - 1) for i in range(NPT)]
    tiles.append((SEQ, NSAMP * LS, NSAMP, LS, True, False))
    stop = dbg.get("stop")
    if "tiles" in dbg:
        tiles = [tiles[i] for i in dbg["tiles"]]
    for ti_, (tok0, W, nseq, L, sample, last_prompt) in enumerate(tiles):
        ring_state["piece"] = 0
        ring_state["first"] = (ti_ == 0)
        if not dbg.get("noload"):
            load_tile(tok0, W)
        if sample:
            gv_rows = {0: 128, 1: 256}
        elif last_prompt:
            gv_rows = {3: 0}
        else:
            gv_rows = {}
        phases = [
            ("load", lambda: None),
            ("gmlp", lambda: gmlp(W, sample, gv_rows)),
            ("ln0", lambda: deepnorm_ln(W, 0, scaled=True)),
            ("ffn", lambda: experts(W, ff_views, gated=False)),
            ("ln1", lambda: deepnorm_ln(W, 1, scaled=True)),
            ("conv", lambda: conv_module(W, nseq, L, sample, last_prompt)),
            ("ln2", lambda: deepnorm_ln(W, 2, scaled=False)),
            ("router", lambda: router(W, sparse=(not sample) and not dbg.get("dense"))),
            ("moe", lambda: (experts(W, moe_views[:dbg.get("nexp", NE)], gated=True) if (sample or dbg.get("dense"))
                             else experts_sparse(moe_views[:dbg.get("nexp", NE)]))),
            ("ln3", lambda: deepnorm_ln(W, 3, scaled=False, make_xb=False)),
        ]
        for name, fn in phases:
            fn()
            if stop == name:
                break
        if not dbg.get("nostore"):
            store_tile(tok0, W)
    B.final_wait("sp")
    B.final_wait("pool")
    B.emit()


_NC_CACHE = {}


def kernel(x_prompt, x_sample, cache_conv, gm_w_in, gm_b_in, gm_lnv_g, gm_lnv_b, gm_w_s, gm_b_s, gm_w_out, gm_b_out,
           cv_w_pw1, cv_b_pw1, cv_w_dw, cv_b_dw, cv_ln_g, cv_ln_b, cv_w_pw2, cv_b_pw2,
           ff_w_gate, ff_w_up, ff_w_down, moe_w_router, moe_b_router, moe_w_gate, moe_w_up, moe_w_down, ln_g, ln_b):
    f = lambda a: np.ascontiguousarray(np.asarray(a, dtype=np.float32))
    shared = {
        "gm_w_in": f(gm_w_in[0]), "gm_b_in": f(gm_b_in).reshape(1, -1), "gm_lnv_g": f(gm_lnv_g).reshape(1, -1),
        "gm_lnv_b": f(gm_lnv_b).reshape(1, -1), "gm_w_s": f(gm_w_s[0]), "gm_b_s": f(gm_b_s[0]),
        "gm_w_out": f(gm_w_out[0]), "gm_b_out": f(gm_b_out).reshape(1, -1),
        "cv_w_pw1": f(cv_w_pw1[0]), "cv_b_pw1": f(cv_b_pw1).reshape(1, -1), "cv_w_dw": f(cv_w_dw[0]),
        "cv_b_dw": f(cv_b_dw).reshape(1, -1), "cv_ln_g": f(cv_ln_g).reshape(1, -1), "cv_ln_b": f(cv_ln_b).reshape(1, -1),
        "cv_w_pw2": f(cv_w_pw2[0]), "cv_b_pw2": f(cv_b_pw2).reshape(1, -1),
        "ff_w_gate": f(ff_w_gate[0]), "ff_w_up": f(ff_w_up[0]), "ff_w_down": f(ff_w_down[0]),
        "moe_w_router": f(moe_w_router[0]), "moe_b_router": f(moe_b_router).reshape(1, -1),
        "moe_w_gate": f(moe_w_gate[0]), "moe_w_up": f(moe_w_up[0]), "moe_w_down": f(moe_w_down[0]),
        "ln_g": f(ln_g).reshape(1, -1), "ln_b": f(ln_b).reshape(1, -1),
    }
    xp = f(x_prompt)
    xs = f(x_sample)
    cc = f(cache_conv)
    in_maps = []
    for c in range(NCORE):
        xin = np.concatenate([xp[c], xs[c * NSAMP:(c + 1) * NSAMP].reshape(NSAMP * LS, D)], axis=0)
        m = dict(shared)
        m["xin"] = np.ascontiguousarray(xin)
        m["cache"] = np.ascontiguousarray(cc[0, c * NSAMP:(c + 1) * NSAMP])
        in_maps.append(m)
    if "nc" not in _NC_CACHE:
        _NC_CACHE["nc"] = build_program()
    res = run_bass_kernel_spmd(_NC_CACHE["nc"], in_maps, core_ids=list(range(NCORE)))
    y_p = np.empty((8, SEQ, D), np.float32)
    y_s = np.empty((32, LS, D), np.float32)
    gv_p = np.empty((1, 8, 128, DV), np.float32)
    gv_s = np.empty((1, 32, LS, DV), np.float32)
    cs_p = np.empty((1, 8, HW_, D), np.float32)
    cs_s = np.empty((1, 32, HW_, D), np.float32)
    for c in range(NCORE):
        r = res.results[c]
        y_p[c] = r["y_o"][0:SEQ]
        y_s[c * NSAMP:(c + 1) * NSAMP] = r["y_o"][SEQ:].reshape(NSAMP, LS, D)
        gv_p[0, c] = r["gv_o"][0:128]
        gv_s[0, c * NSAMP:(c + 1) * NSAMP] = r["gv_o"][128:].reshape(NSAMP, LS, DV)
        cs_p[0, c] = r["cs_o"][0:HW_]
        cs_s[0, c * NSAMP:(c + 1) * NSAMP] = r["cs_o"][HW_:].reshape(NSAMP, HW_, D)
    return y_p, y_s, gv_p, gv_s, cs_p, cs_s
```

```python
import numpy as np
from contextlib import ExitStack
import concourse.bass as bass
import concourse.mybir as mybir
from concourse.bass_utils import run_bass_kernel_spmd

F32 = mybir.dt.float32
BF16 = mybir.dt.bfloat16
AF = mybir.ActivationFunctionType
ALU = mybir.AluOpType
AX = mybir.AxisListType

D = 1024
KD = 8
DV = 3072
CV = 24
FF = 3584
NE = 8
SEQ = 4096
NCORE = 8
NSAMP = 4
LS = 64
HW_ = 30
NTAP = 31
ALPHA = float((2.0 * 2) ** 0.25)
EPS = 1e-5
TOK = SEQ + NSAMP * LS
WT = 512
NPT = SEQ // WT
SLOT = 7168
NSLOT = 5
import os as _os
NOSELF = bool(_os.environ.get("NOSELF"))
POOLENG = "pool" if _os.environ.get("USEPOOL") else "dve"


class Res:
    __slots__ = ("w", "r", "const", "excl")

    def __init__(self, const=False, excl=False):
        self.w = None
        self.r = {}
        self.const = const
        self.excl = excl


class Builder:
    def __init__(self, nc, es):
        self.nc = nc
        self.es = es
        self.engs = {"pe": nc.tensor, "act": nc.scalar, "dve": nc.vector, "pool": nc.gpsimd, "sp": nc.sync}
        self.prog = {k: [] for k in self.engs}
        self.cnt = {k: 0 for k in self.engs}
        self.waited = {k: {} for k in self.engs}
        self.S = {k: es.enter_context(nc.semaphore("S_" + k)) for k in ("pe", "act", "dve", "pool")}
        self.dsem = {}
        self.regs = {"pe": es.enter_context(nc.tensor.register("cnt_pe")),
                     "act": es.enter_context(nc.scalar.register("cnt_act")),
                     "dve": es.enter_context(nc.vector.register("cnt_dve"))}
        self.semobj = {}
        for k, s in self.S.items():
            self.semobj[id(s)] = s

    def sb(self, name, shape, dt):
        return self.es.enter_context(self.nc.sbuf_tensor(name, shape, dt))

    def dma_sem(self, name):
        if name not in self.dsem:
            s = self.es.enter_context(self.nc.semaphore("D_" + name))
            self.dsem[name] = [s, 0]
            self.semobj[id(s)] = s
        return self.dsem[name]

    def _collect(self, eng, reads, writes):
        waits = {}
        own = id(self.S[eng]) if eng in self.S else None

        def need(ev):
            if ev is None:
                return
            sid, val = ev
            if sid == own and (eng == "pe" or NOSELF):
                return
            if self.waited[eng].get(sid, 0) >= val:
                return
            if waits.get(sid, 0) < val:
                waits[sid] = val

        for r in reads:
            need(r.w)
        for w in writes:
            need(w.w)
            for sid, val in w.r.items():
                need((sid, val))
        for sid, val in waits.items():
            self.waited[eng][sid] = val
        return [(self.semobj[sid], val) for sid, val in waits.items()]

    def _commit(self, ev, reads, writes):
        for r in reads:
            if not r.const:
                if r.r.get(ev[0], 0) < ev[1]:
                    r.r[ev[0]] = ev[1]
        for w in writes:
            w.w = ev
            w.r = {}

    def op(self, eng, fn, reads=(), writes=(), signal=True):
        if eng != "pe":
            ex = [r for r in reads if r.excl]
            if ex:
                reads = [r for r in reads if not r.excl]
                writes = list(writes) + ex
        waits = self._collect(eng, reads, writes)
        if signal:
            self.cnt[eng] += 1
            ev = (id(self.S[eng]), self.cnt[eng])
        else:
            ev = (id(self.S[eng]), self.cnt[eng] + 1)
        self._commit(ev, reads, writes)
        self.prog[eng].append((waits, fn, (self.S[eng], 1) if signal else None))

    def dma(self, q, out, in_, reads, writes, semname, **kw):
        waits = self._collect(q, reads, writes)
        ds = self.dma_sem(semname)
        ds[1] += 16
        ev = (id(ds[0]), ds[1])
        self._commit(ev, reads, writes)
        self.prog[q].append((waits, lambda e: e.dma_start(out=out, in_=in_, **kw), (ds[0], 16)))

    def if_begin(self, engs, ei, thresh):
        self._if = {}
        for eng in engs:
            entry = ["IF", ei, thresh, 0]
            self.prog[eng].append(entry)
            self._if[eng] = (entry, self.cnt[eng], dict(self.waited[eng]))

    def if_end(self):
        for eng, (entry, c0, w0) in self._if.items():
            entry[3] = self.cnt[eng] - c0
            self.prog[eng].append(["ENDIF"])
            self.waited[eng] = w0
        self._if = {}

    def barrier(self, engs):
        for eng in engs:
            waits = []
            for k, sem in self.S.items():
                if self.cnt[k] > 0 and not (eng == "pe" and k == "pe"):
                    if self.waited[eng].get(id(sem), 0) < self.cnt[k]:
                        self.waited[eng][id(sem)] = self.cnt[k]
                        waits.append((sem, self.cnt[k]))
            for name, (sem, tot) in self.dsem.items():
                if tot > 0 and self.waited[eng].get(id(sem), 0) < tot:
                    self.waited[eng][id(sem)] = tot
                    waits.append((sem, tot))
            self.prog[eng].append((waits, None, None))

    def final_wait(self, q):
        waits = []
        for name, (s, tot) in self.dsem.items():
            if tot > 0:
                waits.append((s, tot))
        self.prog[q].append((waits, None, None))

    def emit(self):
        nc = self.nc
        block = self.es.enter_context(nc.Block())

        def replay(key, e):
            prog = self.prog[key]
            i = 0
            n = len(prog)

            def run(entry):
                waits, fn, inc = entry
                for s_, v in waits:
                    e.wait_ge(s_, v)
                if fn is None:
                    return
                ins = fn(e)
                if inc is not None:
                    ins.then_inc(inc[0], inc[1])

            while i < n:
                entry = prog[i]
                if entry[0] == "IF":
                    _, ei, thresh, k = entry
                    j = i + 1
                    while prog[j][0] != "ENDIF":
                        j += 1
                    reg = self.regs[key]
                    with e.If_lt(reg, thresh + 1):
                        if k > 0:
                            e.drain().then_inc(self.S[key], k)
                    with e.Else():
                        for ent in prog[i + 1:j]:
                            run(ent)
                    i = j + 1
                    continue
                run(entry)
                i += 1

        @block.tensor
        def _(e):
            replay("pe", e)

        @block.scalar
        def _(e):
            replay("act", e)

        @block.vector
        def _(e):
            replay("dve", e)

        @block.gpsimd
        def _(e):
            replay("pool", e)

        @block.sync
        def _(e):
            replay("sp", e)


def build_program(dbg=None):
    nc = bass.Bass("TRN2", target_bir_lowering=False)
    es = ExitStack()
    with es:
        _build(nc, es, dbg or {})
    return nc


def _build(nc, es, dbg):
    B = Builder(nc, es)
    op, dma, sb = B.op, B.dma, B.sb

    def din(name, shape):
        return nc.dram_tensor(name, shape, F32, kind="ExternalInput").ap()

    def dout(name, shape):
        return nc.dram_tensor(name, shape, F32, kind="ExternalOutput").ap()

    xin = din("xin", [TOK, D])
    cache = din("cache", [NSAMP, HW_, D])
    w_in = din("gm_w_in", [D, 2 * DV])
    b_in = din("gm_b_in", [1, 2 * DV])
    lnv_g = din("gm_lnv_g", [1, DV])
    lnv_b = din("gm_lnv_b", [1, DV])
    w_s = din("gm_w_s", [8, 128, 128])
    b_s = din("gm_b_s", [8, 128])
    w_out = din("gm_w_out", [DV, D])
    b_out = din("gm_b_out", [1, D])
    w_pw1 = din("cv_w_pw1", [D, 2 * D])
    b_pw1 = din("cv_b_pw1", [1, 2 * D])
    w_dw = din("cv_w_dw", [NTAP, D])
    b_dw = din("cv_b_dw", [1, D])
    cln_g = din("cv_ln_g", [1, D])
    cln_b = din("cv_ln_b", [1, D])
    w_pw2 = din("cv_w_pw2", [D, D])
    b_pw2 = din("cv_b_pw2", [1, D])
    ffg = din("ff_w_gate", [D, FF])
    ffu = din("ff_w_up", [D, FF])
    ffd = din("ff_w_down", [FF, D])
    w_r = din("moe_w_router", [D, NE])
    b_r = din("moe_b_router", [1, NE])
    mog = din("moe_w_gate", [NE, D, FF])
    mou = din("moe_w_up", [NE, D, FF])
    mod = din("moe_w_down", [NE, FF, D])
    ln_g = din("ln_g", [1, 4 * D])
    ln_b = din("ln_b", [1, 4 * D])
    y_o = dout("y_o", [TOK, D])
    gv_o = dout("gv_o", [128 + NSAMP * LS, DV])
    cs_o = dout("cs_o", [HW_ * (1 + NSAMP), D])

    XF = sb("XF", [128, KD, WT], F32)
    XB = sb("XB", [128, KD, WT], BF16)
    RING = [sb("ring%d" % i, [128, SLOT], BF16) for i in range(NSLOT)]
    PH = sb("PH", [128, 24576], BF16)
    XS0 = sb("XS0", [128, D], F32)
    XS = [XS0, XS0]
    BROWF = PH[0:1, 0:4096].bitcast(F32).rearrange("o (a n) -> o a n", a=2)
    BROWT = PH[0:1, 4096:8192].bitcast(F32).rearrange("o (a n) -> o a n", a=2)
    WSA = PH[:, 8192:10240].bitcast(F32).rearrange("p (g j) -> p g j", g=8)
    IOTA_I = PH[:, 10240:11264].bitcast(mybir.dt.int32)
    UTRI_F = PH[:, 11264:11520].bitcast(F32)
    LNS = sb("LNS", [128, 13312], BF16)

    def lnv(off, n, f32=True):
        v = LNS[:, off:off + n]
        return v.bitcast(F32) if f32 else v
    MEAN, VAR, RSTD = lnv(0, 1024), lnv(1024, 1024), lnv(2048, 1024)
    T1 = [lnv(3072 + i * 1024, 1024) for i in range(2)]
    T2 = [lnv(5120 + i * 1024, 1024) for i in range(2)]
    SQ = [lnv(7168 + i * 512, 512, False) for i in range(2)]
    ZB = [lnv(8192 + i * 512, 512, False) for i in range(2)]
    identF = sb("identF", [128, 128], F32)
    identB = sb("identB", [128, 128], BF16)
    onesB = sb("onesB", [128, 128], BF16)
    WST = [sb("WST%d" % i, [128, 8, 128], BF16) for i in range(2)]
    BROW = sb("BROW", [1, 4, 1024], BF16)
    SEL = sb("SEL", [8, NE, 128], F32)
    WR = sb("WR", [128, KD, NE], F32)
    BR = sb("BR", [128, NE], F32)
    GTS = sb("GTS", [8, WT], F32)
    VB_IN = sb("VB_IN", [128, 48], F32)
    VLNVG = sb("VLNVG", [128, CV], F32)
    VLNVB = sb("VLNVB", [128, CV], F32)
    VBOUT = sb("VBOUT", [128, KD], F32)
    VBPW1 = sb("VBPW1", [128, 16], F32)
    WDW = sb("WDW", [128, KD, NTAP], F32)
    VBDW = sb("VBDW", [128, KD], F32)
    VCLG = sb("VCLG", [128, KD], F32)
    VCLB = sb("VCLB", [128, KD], F32)
    VBPW2 = sb("VBPW2", [128, KD], F32)
    VLNG = sb("VLNG", [128, 32], F32)
    VLNB = sb("VLNB", [128, 32], F32)
    VLNGA = sb("VLNGA", [128, 32], F32)
    VLNBA = sb("VLNBA", [128, 32], F32)
    EPST = sb("EPST", [128, 1], F32)
    HIST = sb("HIST", [128, KD, HW_], BF16)
    GL = sb("GL", [128, KD, NSAMP, HW_], F32)
    RT = {n: sb("RT_" + n, [128, NE], F32) for n in ("L", "M1", "L2", "M2", "G")}
    RS = {n: sb("RS_" + n, [128, 1], F32) for n in ("m1", "m2", "d", "e", "den", "w1", "w2")}
    I32 = mybir.dt.int32
    CAP = int(dbg.get("cap", 256))
    PM = LNS
    IOTA_F = sb("IOTA_F", [128, WT], F32)
    SLOT_I = sb("SLOT_I", [128, 4], I32)
    SLOTID = sb("SLOTID", [128, 4], F32)
    UTRI = sb("UTRI", [128, 128], BF16)
    MSKF = sb("MSKF", [128, 4, NE], F32)
    MSK = sb("MSK", [128, 4, NE], BF16)
    POS = sb("POS", [128, 4, NE], F32)
    POST = sb("POST", [8, WT], F32)
    CNT = sb("CNT", [1, NE], I32)
    PS = [es.enter_context(nc.psum_tensor("ps%d" % i, [128, 512], F32)) for i in range(8)]

    R = {}

    def res(name, const=False):
        if name not in R:
            R[name] = Res(const)
        return R[name]

    rPS = [res("ps%d" % i) for i in range(8)]
    for r_ in rPS:
        r_.excl = True
    rRING = [res("ring%d" % i) for i in range(NSLOT)]
    rXFm = [res("XF%d" % m) for m in range(KD)]
    rXB = res("XB")
    rXS = [res("XS0"), res("XS0")]
    rC = res("consts")
    rCd, rWSA, rCp, rCv = res("Cd"), res("WSA"), res("Cp"), res("Cv")

    def phv(off, n):
        return PH[:, off:off + n]

    VNT = phv(0, 12288).rearrange("p (c t) -> p c t", c=CV)
    OT = VNT
    VN = phv(12288, 12288).rearrange("p (b c) -> p b c", b=4)
    GTB = phv(0, 4336)
    DG = [phv(4336 + i * 3968, 3968).rearrange("p (k c) -> p k c", k=NTAP) for i in range(2)]
    ST = phv(12272, 4096).rearrange("p (m t) -> p m t", m=KD)
    YC = phv(16384, 8192).bitcast(F32).rearrange("p (m t) -> p m t", m=KD)
    HT = [phv(i * 3584, 3584).rearrange("p (f t) -> p f t", f=7) for i in range(2)]
    SS = [phv(7168 + i * 512, 512) for i in range(2)]
    TT = [phv(8192 + i * 512, 512) for i in range(2)]
    GBC = [phv(9216 + i * 1024, 1024).bitcast(F32) for i in range(2)]
    rVNT, rVN, rGTB, rST, rYC = res("VNT"), res("VN"), res("GTB"), res("ST"), res("YC")
    rVNTc = [res("VNTc%d" % c) for c in range(CV)]
    rDG = [res("DG0"), res("DG1")]
    rHT = [res("HT0"), res("HT1")]
    rSS = [res("SS0"), res("SS1")]
    rTT = [res("TT0"), res("TT1")]
    rGBC = [res("GBC0"), res("GBC1")]
    rT1 = [res("T1_0"), res("T1_1")]
    rT2 = [res("T2_0"), res("T2_1")]
    rSQ = [res("SQ0"), res("SQ1")]
    rZB = [res("ZB0"), res("ZB1")]
    rSTAT = res("STAT")
    rHIST, rGL, rGTS, rRT = res("HIST"), res("GL"), res("GTS"), res("RT")
    WA, WB = CAP, WT - CAP
    A_SB, B_SB = list(range(CAP // 128)), list(range(CAP // 128, 4))
    HTA = [phv(i * 1792, 7 * WA).rearrange("p (f t) -> p f t", f=7) for i in range(2)] if WA <= 256 else None
    HTBv = phv(3584, 7 * WB).rearrange("p (f t) -> p f t", f=7)
    SSA = [phv(6272 + i * 384, 384) for i in range(2)]
    XTOK = phv(7040, 4096).rearrange("p (b c) -> p b c", b=4)
    PE_ = [phv(11136 + i * 512, 512) for i in range(4)]
    Q_ = [phv(13184 + i * 512, 512) for i in range(4)]
    XG = [phv(15232 + i * 4096, 4096).rearrange("p (k t) -> p k t", k=KD) for i in range(2)]
    GBC1 = phv(23424, 1024).bitcast(F32)
    ACC = [PM[:, i * 2048:(i + 1) * 2048].bitcast(F32) for i in range(4)]
    ACCB = [PM[:, 8192 + i * 1024: 8192 + (i + 1) * 1024] for i in range(4)]
    POSBC = PM[:, 12288:13312].bitcast(F32)
    rXTOK, rPE_, rGBC1, rPOSBC, rHTB = res("XTOK"), res("PE_"), res("GBC1"), res("POSBC"), res("HTB")
    rQ = [res("Q%d" % i) for i in range(4)]
    rXG = [res("XG0"), res("XG1")]
    rHTA = [res("HTA0"), res("HTA1")]
    rSSA = [res("SSA0"), res("SSA1")]
    rACC = [res("ACC%d" % i) for i in range(4)]
    rACCB = [res("ACCB%d" % i) for i in range(4)]
    rMSK, rPOS, rCNT, rPOST = res("MSK"), res("POS"), res("CNT"), res("POST")

    def vec_load(dst, src_row, n):
        dma("sp", dst, src_row.rearrange("o (c p) -> p (o c)", p=128), [], [rCd], "const",
            allow_slow_non_contiguous=True)

    skip = dbg.get("skip", ())
    if 'vec' not in skip:
        vec_load(VB_IN[:], b_in, 48)
        vec_load(VLNVG[:], lnv_g, CV)
        vec_load(VLNVB[:], lnv_b, CV)
        vec_load(VBOUT[:], b_out, KD)
        vec_load(VBPW1[:], b_pw1, 16)
        vec_load(VBDW[:], b_dw, KD)
        vec_load(VCLG[:], cln_g, KD)
        vec_load(VCLB[:], cln_b, KD)
        vec_load(VBPW2[:], b_pw2, KD)
        vec_load(VLNG[:], ln_g, 32)
        vec_load(VLNB[:], ln_b, 32)
    if 'wdw' not in skip:
        for k in range(NTAP):
            dma("sp", WDW[:, :, k], w_dw[k:k + 1, :].rearrange("o (c p) -> p (o c)", p=128), [], [rCd], "const",
                allow_slow_non_contiguous=True)
    if 'wr' not in skip:
        dma("sp", WR[:], w_r.rearrange("(k p) e -> p k e", p=128), [], [rCd], "const")
    if 'br' not in skip:
        dma("sp", BR[:], b_r.partition_broadcast(128), [], [rCd], "const")
    if 'wsa' not in skip:
        dma("sp", WSA[:], w_s.rearrange("g i j -> i g j"), [], [rWSA], "wsa")
    if 'browf' not in skip:
        dma("sp", BROWF[:, 0, :], b_s.rearrange("(o g) i -> o (g i)", o=1), [], [rCd], "const")
        for h in range(2):
            dma("sp", BROWF[:, 1, :].rearrange("o (g i) -> o g i", g=8)[:, :, h * 64:(h + 1) * 64],
                b_s[:, 0:64].rearrange("(o g) i -> o g i", o=1), [], [rCd], "const")
    if 'pool' not in skip:
        op("pool", lambda e: e.memset(identF[:], 0.0), [], [rCp])
        op("pool", lambda e: e.affine_select(out=identF[:], in_=identF[:], compare_op=ALU.not_equal, fill=1.0,
                                             base=0, pattern=[[-1, 128]], channel_multiplier=1), [], [rCp])
    if 'sel' not in skip:
        op("pool", lambda e: e.memset(SEL[:], 0.0), [], [rCp])
        for ee in range(NE):
            op("pool", lambda e, ee=ee: e.affine_select(out=SEL[:, ee, :], in_=SEL[:, ee, :], compare_op=ALU.not_equal,
                                                        fill=1.0, base=-ee, pattern=[[0, 128]], channel_multiplier=1),
               [], [rCp])
    if 'pool2' not in skip:
        op("pool", lambda e: e.memset(EPST[:], EPS), [], [rCp])
        op("pool", lambda e: e.memset(onesB[:], 1.0), [], [rCp])
        op("pool", lambda e: e.memset(HIST[:], 0.0), [], [rHIST])
        op("pool", lambda e: e.iota(out=IOTA_I[:], pattern=[[1, WT]], base=0, channel_multiplier=0), [], [rCp])
        op("pool", lambda e: e.iota(out=SLOT_I[:], pattern=[[128, 4]], base=0, channel_multiplier=1), [], [rCp])
        op("pool", lambda e: e.memset(UTRI_F[:], 1.0), [], [rCp])
        op("pool", lambda e: e.affine_select(out=UTRI_F[:], in_=UTRI_F[:], compare_op=ALU.is_gt, fill=0.0, base=0,
                                             pattern=[[1, 128]], channel_multiplier=-1), [], [rCp])
        op("dve", lambda e: e.tensor_copy(out=IOTA_F[:], in_=IOTA_I[:]), [rCp], [rCv])
        op("dve", lambda e: e.tensor_copy(out=SLOTID[:], in_=SLOT_I[:]), [rCp], [rCv])
        op("dve", lambda e: e.tensor_copy(out=UTRI[:], in_=UTRI_F[:]), [rCp], [rCv])
    if 'identb' not in skip:
        op("dve", lambda e: e.tensor_copy(out=identB[:], in_=identF[:]), [rCp], [rCv])
    if 'lnga' not in skip:
        op("dve", lambda e: e.tensor_scalar(out=VLNGA[:], in0=VLNG[:], scalar1=ALPHA, scalar2=None, op0=ALU.mult), [rCd], [rCv])
        op("dve", lambda e: e.tensor_scalar(out=VLNBA[:], in0=VLNB[:], scalar1=ALPHA, scalar2=None, op0=ALU.mult), [rCd], [rCv])
    if 'brow' not in skip:
        op("dve", lambda e: e.tensor_copy(out=BROW[:, 0, :], in_=BROWF[:, 0, :]), [rCd, rCv], [rCv])
        op("dve", lambda e: e.tensor_copy(out=BROW[:, 2, :], in_=BROWF[:, 1, :]), [rCd, rCv], [rCv])
        op("dve", lambda e: e.tensor_tensor(out=BROWT[:, 0, :], in0=BROWF[:, 0, :], in1=BROW[:, 0, :], op=ALU.subtract), [rCd, rCv], [rCv])
        op("dve", lambda e: e.tensor_tensor(out=BROWT[:, 1, :], in0=BROWF[:, 1, :], in1=BROW[:, 2, :], op=ALU.subtract), [rCd, rCv], [rCv])
        op("dve", lambda e: e.tensor_copy(out=BROW[:, 1, :], in_=BROWT[:, 0, :]), [rCd, rCv], [rCv])
        op("dve", lambda e: e.tensor_copy(out=BROW[:, 3, :], in_=BROWT[:, 1, :]), [rCd, rCv], [rCv])
    if 'wstp' not in skip:
        op("dve", lambda e: e.memset(WST[0][:], 0.0), [], [rCv])
        op("dve", lambda e: e.memset(WST[1][:], 0.0), [], [rCv])
        for g in range(8):
            bank = g % 2
            op("pe", lambda e, g=g, bank=bank: e.transpose(out=PS[bank][:, 0:128], in_=WSA[:, g, :], identity=identF[:]),
               [rWSA, rCp], [rPS[bank]])
            op("dve", lambda e, g=g, bank=bank: e.tensor_copy(out=WST[0][0:64, g, :], in_=PS[bank][0:64, 0:128]),
               [rPS[bank]], [rCv])
            op("dve", lambda e, g=g, bank=bank: e.tensor_copy(out=WST[0][64:128, g, 64:128], in_=PS[bank][64:128, 64:128]),
               [rPS[bank]], [rCv])
    if 'wsts' not in skip:
        op("dve", lambda e: e.memset(WSA[:], 0.0), [], [rWSA])
        dma("sp", WSA[0:64, :, 0:64], w_s[:, 0:64, 0:64].rearrange("g i j -> i g j"), [], [rWSA], "wsa")
        dma("sp", WSA[64:128, :, 64:128], w_s[:, 0:64, 0:64].rearrange("g i j -> i g j"), [], [rWSA], "wsa")
        for g in range(8):
            bank = g % 2
            op("pe", lambda e, g=g, bank=bank: e.transpose(out=PS[bank][:, 0:128], in_=WSA[:, g, :], identity=identF[:]),
               [rWSA, rCp], [rPS[bank]])
            op("dve", lambda e, g=g, bank=bank: e.tensor_copy(out=WST[1][:, g, :], in_=PS[bank][:, 0:128]),
               [rPS[bank]], [rCv])
    B.barrier(["pe", "act", "dve", "pool"])
    rC.w = None
    rC.r = {}
    rC.const = True

    ring_state = {"i": 0, "piece": 0, "first": True}
    NPIECE = 126
    WSCR = nc.dram_tensor("wscr", [NPIECE, 128, SLOT], BF16, kind="Internal").ap()
    rSCR = [Res() for _ in range(NPIECE)]

    def wload(src3, shape):
        i = ring_state["i"] % NSLOT
        ring_state["i"] += 1
        j = ring_state["piece"]
        ring_state["piece"] += 1
        a, b = shape
        flat = RING[i][:, 0:a * b]
        view = flat.rearrange("p (a b) -> p a b", a=a)
        if ring_state["first"] or dbg.get("noscr"):
            dma("pool", view, src3, [], [rRING[i]], "rgs%d" % i)
            if not dbg.get("noscr"):
                dma("sp", WSCR[j, :, 0:a * b], flat, [rRING[i]], [rSCR[j]], "wst%d" % i)
        else:
            dma("sp", flat, WSCR[j, :, 0:a * b], [rSCR[j]], [rRING[i]], "rgh%d" % i)
        return view, rRING[i]

    w_in_v = w_in.rearrange("(k p) n -> p k n", p=128)
    w_out_v = w_out.rearrange("(c p) n -> p c n", p=128)
    w_pw1_v = w_pw1.rearrange("(k p) n -> p k n", p=128)
    w_pw2_v = w_pw2.rearrange("(k p) n -> p k n", p=128)

    def layer_norm(zs, W, nch, srcs_bf=None, outs=None, fence=()):
        inv = 1.0 / (nch * 128)
        for m in range(nch):
            z, rz = zs[m]
            s = m % 2
            fz = list(fence) if m == 0 else []
            op("act", lambda e, z=z, s=s: e.activation(out=SQ[s][:, 0:W], in_=z, func=AF.Square), [rz], [rSQ[s]] + fz)
            if srcs_bf is None:
                op("dve", lambda e, z=z, s=s: e.tensor_copy(out=ZB[s][:, 0:W], in_=z), [rz], [rZB[s]] + fz)
                zb, rzb = ZB[s][:, 0:W], rZB[s]
            else:
                zb, rzb = srcs_bf[m]
            op("pe", lambda e, zb=zb, m=m: e.matmul(PS[6][:, 0:W], lhsT=onesB[:], rhs=zb, start=(m == 0), stop=(m == nch - 1)),
               [rzb, rC], [rPS[6]], signal=(m == nch - 1))
            op("pe", lambda e, s=s, m=m: e.matmul(PS[7][:, 0:W], lhsT=onesB[:], rhs=SQ[s][:, 0:W], start=(m == 0), stop=(m == nch - 1)),
               [rSQ[s], rC], [rPS[7]], signal=True)
        op("dve", lambda e: e.tensor_scalar(out=MEAN[:, 0:W], in0=PS[6][:, 0:W], scalar1=inv, scalar2=None, op0=ALU.mult),
           [rPS[6]], [rSTAT])
        op("dve", lambda e: e.tensor_tensor(out=VAR[:, 0:W], in0=MEAN[:, 0:W], in1=MEAN[:, 0:W], op=ALU.mult), [rSTAT], [rSTAT])
        op("dve", lambda e: e.scalar_tensor_tensor(out=VAR[:, 0:W], in0=PS[7][:, 0:W], scalar=inv, in1=VAR[:, 0:W],
                                                   op0=ALU.mult, op1=ALU.subtract), [rPS[7], rSTAT], [rSTAT])
        op("act", lambda e: e.activation(out=RSTD[:, 0:W], in_=VAR[:, 0:W], func=AF.Sqrt, bias=EPST[:], scale=1.0),
           [rSTAT, rC], [rSTAT])
        op("dve", lambda e: e.reciprocal(out=RSTD[:, 0:W], in_=RSTD[:, 0:W]), [rSTAT], [rSTAT])
        for m in range(nch):
            z, rz = zs[m]
            s = m % 2
            op("dve", lambda e, z=z, s=s: e.tensor_tensor(out=T1[s][:, 0:W], in0=z, in1=MEAN[:, 0:W], op=ALU.subtract),
               [rz, rSTAT], [rT1[s]])
            op(POOLENG, lambda e, s=s: e.tensor_tensor(out=T2[s][:, 0:W], in0=T1[s][:, 0:W], in1=RSTD[:, 0:W], op=ALU.mult),
               [rT1[s], rSTAT], [rT2[s]])
            outs(m, T2[s][:, 0:W], rT2[s])

    def deepnorm_ln(W, lnidx, scaled, make_xb=True):
        gsrc, bsrc = (VLNGA, VLNBA) if scaled else (VLNG, VLNB)

        def outs(m, t2, rt2):
            col = lnidx * 8 + m
            op("act", lambda e: e.activation(out=XF[:, m, 0:W], in_=t2, func=AF.Identity,
                                             bias=bsrc[:, col:col + 1], scale=gsrc[:, col:col + 1]), [rt2, rC], [rXFm[m]])
            if make_xb:
                if POOLENG == "pool":
                    op("pool", lambda e: e.tensor_scalar(out=XB[:, m, 0:W], in0=t2, scalar1=VLNG[:, col:col + 1],
                                                         scalar2=VLNB[:, col:col + 1], op0=ALU.mult, op1=ALU.add), [rt2, rC], [rXB])
                else:
                    op("act", lambda e: e.activation(out=XB[:, m, 0:W], in_=t2, func=AF.Identity,
                                                     bias=VLNB[:, col:col + 1], scale=VLNG[:, col:col + 1]), [rt2, rC], [rXB])
        layer_norm([(XF[:, m, 0:W], rXFm[m]) for m in range(KD)], W, KD, None, outs,
                   fence=(rACC + rACCB + [rPOSBC]) if lnidx == 3 else ())

    def load_tile(tok0, W):
        nb = W // 128
        LT = _os.environ.get("LT", "dpav")
        for b in range(nb):
            s = b % 2
            if "d" in LT:
                dma("sp", XS[s][:], xin[tok0 + b * 128: tok0 + (b + 1) * 128, :], [], [rXS[s]], "xs%d" % s)
            if "p" in LT:
                for m in range(KD):
                    bank = m // 4
                    op("pe", lambda e, m=m, s=s, bank=bank: e.transpose(out=PS[bank][:, (m % 4) * 128:(m % 4 + 1) * 128],
                                                                        in_=XS[s][:, m * 128:(m + 1) * 128], identity=identF[:]),
                       [rXS[s], rC], [rPS[bank]], signal=(m % 4 == 3))
            for h in range(2):
                src = PS[h][:].rearrange("p (a t) -> p a t", a=4)
                if "a" in LT:
                    op("act", lambda e, h=h, b=b, src=src: e.mul(out=XF[:, 4 * h:4 * h + 4, b * 128:(b + 1) * 128], in_=src, mul=ALPHA),
                       [rPS[h]], rXFm[4 * h:4 * h + 4])
                if "v" in LT:
                    op("dve", lambda e, h=h, b=b, src=src: e.tensor_copy(out=XB[:, 4 * h:4 * h + 4, b * 128:(b + 1) * 128], in_=src),
                       [rPS[h]] + ([rXFm[4 * h]] if _os.environ.get("SER") else []), [rXB])

    def store_tile(tok0, W):
        nb = W // 128
        for b in range(nb):
            s = b % 2
            for m in range(KD):
                bank = 2 + m // 4
                op("pe", lambda e, m=m, b=b, bank=bank: e.transpose(out=PS[bank][:, (m % 4) * 128:(m % 4 + 1) * 128],
                                                                    in_=XF[:, m, b * 128:(b + 1) * 128], identity=identF[:]),
                   [rXFm[m], rC], [rPS[bank]], signal=(m % 4 == 3))
            op("act", lambda e, s=s: e.copy(out=XS[s][:, 0:512], in_=PS[2][:]), [rPS[2]], [rXS[s]])
            op("dve", lambda e, s=s: e.tensor_copy(out=XS[s][:, 512:1024], in_=PS[3][:]), [rPS[3]], [rXS[s]])
            dma("sp", y_o[tok0 + b * 128: tok0 + (b + 1) * 128, :], XS[s][:], [rXS[s]], [], "ys%d" % s)

    def gmlp(W, sample, gv_rows):
        nb = W // 128
        wst = WST[1 if sample else 0]
        bro = 2 if sample else 0
        vps = 0
        for pc in range(4):
            wv, rw = wload(w_in_v[:, :, DV + pc * 768: DV + (pc + 1) * 768], (KD, 768))
            for cc in range(6):
                c = pc * 6 + cc
                bank = vps % 2
                vps += 1
                for k in range(KD):
                    op("pe", lambda e, k=k, cc=cc, bank=bank, wv=wv: e.matmul(PS[bank][:, 0:W], lhsT=wv[:, k, cc * 128:(cc + 1) * 128],
                                                                               rhs=XB[:, k, 0:W], start=(k == 0), stop=(k == KD - 1)),
                       [rw, rXB], [rPS[bank]], signal=(k == KD - 1))
                op("act", lambda e, c=c, bank=bank: e.activation(out=VNT[:, c, 0:W], in_=PS[bank][:, 0:W], func=AF.Gelu,
                                                                 bias=VB_IN[:, 24 + c:25 + c], scale=1.0),
                   [rPS[bank], rC], [rVNTc[c]])

        def outs(c, t2, rt2):
            op("act", lambda e: e.activation(out=VNT[:, c, 0:W], in_=t2, func=AF.Identity,
                                             bias=VLNVB[:, c:c + 1], scale=VLNVG[:, c:c + 1]), [rt2, rC], [rVNTc[c]])
        zs = [(VNT[:, c, 0:W], rVNTc[c]) for c in range(CV)]
        layer_norm(zs, W, CV, zs, outs)
        tb = 0
        for b in range(nb):
            for c8 in range(3):
                bank = 2 + tb % 2
                tb += 1
                psb = PS[bank][:].bitcast(BF16)
                for cc in range(8):
                    c = c8 * 8 + cc
                    op("pe", lambda e, c=c, cc=cc, b=b, psb=psb: e.transpose(out=psb[:, cc * 128:(cc + 1) * 128],
                                                                               in_=VNT[:, c, b * 128:(b + 1) * 128], identity=identB[:]),
                       [rVNTc[c], rC], [rPS[bank]], signal=(cc == 7))
                eng = "act" if c8 == 1 else "dve"
                if eng == "act":
                    op("act", lambda e, b=b, c8=c8, psb=psb: e.copy(out=VN[:, b, c8 * 1024:(c8 + 1) * 1024], in_=psb), [rPS[bank]], [rVN])
                else:
                    op("dve", lambda e, b=b, c8=c8, psb=psb: e.tensor_copy(out=VN[:, b, c8 * 1024:(c8 + 1) * 1024], in_=psb), [rPS[bank]], [rVN])
            if b in gv_rows:
                r0 = gv_rows[b]
                dma("pool", gv_o[r0:r0 + 128, :], VN[:, b, :], [rVN], [], "gv")
        ups = 0
        for pc in range(4):
            wu, rw = wload(w_in_v[:, :, pc * 768:(pc + 1) * 768], (KD, 768))
            for cc in range(6):
                c = pc * 6 + cc
                g = c // 3
                bank = ups % 2
                mb = 2 + ups % 2
                s = ups % 2
                ups += 1
                for k in range(KD):
                    op("pe", lambda e, k=k, cc=cc, bank=bank, wu=wu: e.matmul(PS[bank][:, 0:W], lhsT=wu[:, k, cc * 128:(cc + 1) * 128],
                                                                               rhs=XB[:, k, 0:W], start=(k == 0), stop=(k == KD - 1)),
                       [rw, rXB], [rPS[bank]], signal=(k == KD - 1))
                op("act", lambda e, c=c, bank=bank, s=s: e.activation(out=ZB[s][:, 0:W], in_=PS[bank][:, 0:W], func=AF.Gelu,
                                                                      bias=VB_IN[:, c:c + 1], scale=1.0),
                   [rPS[bank], rC], [rZB[s]])
                for b in range(nb):
                    cols = slice(b * 128, (b + 1) * 128)
                    op("pe", lambda e, b=b, c=c, g=g, mb=mb, cols=cols: e.matmul(PS[mb][:, cols], lhsT=VN[:, b, c * 128:(c + 1) * 128],
                                                                                  rhs=wst[:, g, :], start=True, stop=False),
                       [rVN, rC], [rPS[mb]], signal=False)
                    op("pe", lambda e, g=g, mb=mb, cols=cols: e.matmul(PS[mb][:, cols], lhsT=onesB[0:1, :],
                                                                       rhs=BROW[0:1, bro, g * 128:(g + 1) * 128], start=False, stop=False),
                       [rC], [rPS[mb]], signal=False)
                    op("pe", lambda e, g=g, mb=mb, cols=cols: e.matmul(PS[mb][:, cols], lhsT=onesB[0:1, :],
                                                                       rhs=BROW[0:1, bro + 1, g * 128:(g + 1) * 128], start=False, stop=True),
                       [rC], [rPS[mb]], signal=(b == nb - 1))
                op("dve", lambda e, c=c, mb=mb, s=s: e.tensor_tensor(out=OT[:, c, 0:W], in0=ZB[s][:, 0:W], in1=PS[mb][:, 0:W], op=ALU.mult),
                   [rZB[s], rPS[mb]], [rVNTc[c]])
        for pc in range(4):
            wo, rw = wload(w_out_v[:, :, pc * 256:(pc + 1) * 256], (CV, 256))
            for mm in range(2):
                m = pc * 2 + mm
                bank = 4 + m % 2
                for c in range(CV):
                    op("pe", lambda e, c=c, mm=mm, bank=bank, wo=wo: e.matmul(PS[bank][:, 0:W], lhsT=wo[:, c, mm * 128:(mm + 1) * 128],
                                                                               rhs=OT[:, c, 0:W], start=(c == 0), stop=(c == CV - 1)),
                       [rw, rVNTc[c]], [rPS[bank]], signal=(c == CV - 1))
                op("dve", lambda e, m=m, bank=bank: e.scalar_tensor_tensor(out=XF[:, m, 0:W], in0=PS[bank][:, 0:W], scalar=VBOUT[:, m:m + 1],
                                                                           in1=XF[:, m, 0:W], op0=ALU.add, op1=ALU.add),
                   [rPS[bank], rC, rXFm[m]], [rXFm[m]])

    def experts(W, wg_list, gated):
        cnt = 0
        for ei, (gv, uv, dv) in enumerate(wg_list):
            if gated:
                gs = ei % 2
                op("pe", lambda e, ei=ei: e.matmul(PS[6][:, 0:W], lhsT=SEL[:, ei, :], rhs=GTS[0:8, 0:W], start=True, stop=True),
                   [rC, rGTS], [rPS[6]])
                op("act", lambda e, gs=gs: e.copy(out=GBC[gs][:, 0:W], in_=PS[6][:, 0:W]), [rPS[6]], [rGBC[gs]])
            for gi in range(4):
                wgp, rwg = wload(gv[:, :, gi * 896:(gi + 1) * 896], (KD, 896))
                wup, rwu = wload(uv[:, :, gi * 896:(gi + 1) * 896], (KD, 896))
                wdp, rwd = wload(dv[:, gi * 7:(gi + 1) * 7, :], (7, D))
                hs = cnt % 2
                cnt += 1
                for fc in range(7):
                    s = fc % 2
                    gb, ub = fc % 2, 2 + fc % 2
                    for k in range(KD):
                        op("pe", lambda e, k=k, fc=fc, gb=gb, wgp=wgp: e.matmul(PS[gb][:, 0:W], lhsT=wgp[:, k, fc * 128:(fc + 1) * 128],
                                                                                 rhs=XB[:, k, 0:W], start=(k == 0), stop=(k == KD - 1)),
                           [rwg, rXB], [rPS[gb]], signal=(k == KD - 1))
                    for k in range(KD):
                        op("pe", lambda e, k=k, fc=fc, ub=ub, wup=wup: e.matmul(PS[ub][:, 0:W], lhsT=wup[:, k, fc * 128:(fc + 1) * 128],
                                                                                 rhs=XB[:, k, 0:W], start=(k == 0), stop=(k == KD - 1)),
                           [rwu, rXB], [rPS[ub]], signal=(k == KD - 1))
                    op("act", lambda e, gb=gb, s=s: e.activation(out=SS[s][:, 0:W], in_=PS[gb][:, 0:W], func=AF.Silu), [rPS[gb]], [rSS[s]])
                    if gated:
                        op("dve", lambda e, ub=ub, s=s: e.tensor_tensor(out=TT[s][:, 0:W], in0=SS[s][:, 0:W], in1=PS[ub][:, 0:W], op=ALU.mult),
                           [rSS[s], rPS[ub]], [rTT[s]])
                        op("dve", lambda e, s=s, hs=hs, fc=fc, gs=gs: e.tensor_tensor(out=HT[hs][:, fc, 0:W], in0=TT[s][:, 0:W],
                                                                                      in1=GBC[gs][:, 0:W], op=ALU.mult),
                           [rTT[s], rGBC[gs]], [rHT[hs]])
                    else:
                        op("dve", lambda e, ub=ub, s=s, hs=hs, fc=fc: e.tensor_tensor(out=HT[hs][:, fc, 0:W], in0=SS[s][:, 0:W],
                                                                                      in1=PS[ub][:, 0:W], op=ALU.mult),
                           [rSS[s], rPS[ub]], [rHT[hs]])
                for m in range(KD):
                    yb = 4 + m % 2
                    for fc in range(7):
                        op("pe", lambda e, fc=fc, m=m, yb=yb, hs=hs, wdp=wdp: e.matmul(PS[yb][:, 0:W], lhsT=wdp[:, fc, m * 128:(m + 1) * 128],
                                                                                        rhs=HT[hs][:, fc, 0:W], start=(fc == 0), stop=(fc == 6)),
                           [rwd, rHT[hs]], [rPS[yb]], signal=(fc == 6))
                    op("dve", lambda e, m=m, yb=yb: e.tensor_tensor(out=XF[:, m, 0:W], in0=XF[:, m, 0:W], in1=PS[yb][:, 0:W], op=ALU.add),
                       [rPS[yb], rXFm[m]], [rXFm[m]])

    def experts_sparse(wg_list):
        IFE = ["pe", "act", "dve"]
        cnt = 0

        def gather(ei, xg, rxg, c0, width):
            kper = max(1, 512 // width)
            gi_ = 0
            for k0 in range(0, KD, kper):
                bank = 6 + gi_ % 2
                gi_ += 1
                ks = list(range(k0, min(KD, k0 + kper)))
                for kk, k in enumerate(ks):
                    for tb in range(4):
                        op("pe", lambda e, k=k, kk=kk, tb=tb, bank=bank: e.matmul(PS[bank][:, kk * width:(kk + 1) * width],
                                                                                  lhsT=XTOK[:, tb, k * 128:(k + 1) * 128],
                                                                                  rhs=PE_[tb][:, c0:c0 + width], start=(tb == 0), stop=(tb == 3)),
                           [rXTOK, rPE_], [rPS[bank]], signal=(tb == 3 and kk == len(ks) - 1))
                src = PS[bank][:, 0:len(ks) * width].rearrange("p (a t) -> p a t", a=len(ks))
                dst = xg[:, ks[0]:ks[-1] + 1, c0:c0 + width]
                if gi_ % 2 == 0:
                    op("act", lambda e, src=src, dst=dst: e.copy(out=dst, in_=src), [rPS[bank]], [rxg])
                else:
                    op("dve", lambda e, src=src, dst=dst: e.tensor_copy(out=dst, in_=src), [rPS[bank]], [rxg])

        def ffn_block(gi, xg, rxg, c0, width, ht, rht, sbs, wgp, rwg, wup, rwu, wdp, rwd):
            for fc in range(7):
                s_ = fc % 2
                gb, ub = fc % 2, 2 + fc % 2
                for k in range(KD):
                    op("pe", lambda e, k=k, fc=fc, gb=gb: e.matmul(PS[gb][:, 0:width], lhsT=wgp[:, k, fc * 128:(fc + 1) * 128],
                                                                    rhs=xg[:, k, c0:c0 + width], start=(k == 0), stop=(k == KD - 1)),
                       [rwg, rxg], [rPS[gb]], signal=(k == KD - 1))
                for k in range(KD):
                    op("pe", lambda e, k=k, fc=fc, ub=ub: e.matmul(PS[ub][:, 0:width], lhsT=wup[:, k, fc * 128:(fc + 1) * 128],
                                                                    rhs=xg[:, k, c0:c0 + width], start=(k == 0), stop=(k == KD - 1)),
                       [rwu, rxg], [rPS[ub]], signal=(k == KD - 1))
                op("act", lambda e, gb=gb, s_=s_: e.activation(out=SSA[s_][:, 0:width], in_=PS[gb][:, 0:width], func=AF.Silu),
                   [rPS[gb]], [rSSA[s_]])
                op("dve", lambda e, ub=ub, s_=s_, fc=fc: e.tensor_tensor(out=ht[:, fc, 0:width], in0=SSA[s_][:, 0:width], in1=PS[ub][:, 0:width],
                                                                         op=ALU.mult), [rSSA[s_], rPS[ub]], [rht])
            yi = 0
            for j, sbk in enumerate(sbs):
                for half in range(2):
                    yb = 4 + yi % 2
                    yi += 1
                    for fc in range(7):
                        op("pe", lambda e, fc=fc, j=j, half=half, yb=yb: e.matmul(PS[yb][:, 0:512], lhsT=ht[:, fc, j * 128:(j + 1) * 128],
                                                                                  rhs=wdp[:, fc, half * 512:(half + 1) * 512],
                                                                                  start=(fc == 0), stop=(fc == 6)),
                           [rwd, rht], [rPS[yb]], signal=(fc == 6))
                    dst = ACC[sbk][:, half * 512:(half + 1) * 512]
                    if gi == 0:
                        op("dve", lambda e, dst=dst, yb=yb: e.tensor_copy(out=dst, in_=PS[yb][:, 0:512]), [rPS[yb]], [rACC[sbk]])
                    else:
                        op("dve", lambda e, dst=dst, yb=yb: e.tensor_tensor(out=dst, in0=dst, in1=PS[yb][:, 0:512], op=ALU.add),
                           [rPS[yb], rACC[sbk]], [rACC[sbk]])

        def scatter(sbs):
            for sbk in sbs:
                op("act", lambda e, sbk=sbk: e.copy(out=ACCB[sbk][:], in_=ACC[sbk][:]), [rACC[sbk]], [rACCB[sbk]])
            for m in range(KD):
                bank = 6 + m % 2
                for j, sbk in enumerate(sbs):
                    op("pe", lambda e, m=m, j=j, sbk=sbk, bank=bank, n=len(sbs): e.matmul(PS[bank][:, 0:512], lhsT=ACCB[sbk][:, m * 128:(m + 1) * 128],
                                                                                          rhs=Q_[sbk][:], start=(j == 0), stop=(j == n - 1)),
                       [rACCB[sbk], rQ[sbk]], [rPS[bank]], signal=(j == len(sbs) - 1))
                op("dve", lambda e, m=m, bank=bank: e.tensor_tensor(out=XF[:, m, 0:512], in0=XF[:, m, 0:512], in1=PS[bank][:, 0:512], op=ALU.add),
                   [rPS[bank], rXFm[m]], [rXFm[m]])

        def build_q(sbs):
            for sbk in sbs:
                op("dve", lambda e, sbk=sbk: e.scalar_tensor_tensor(out=Q_[sbk][:], in0=POSBC[:], scalar=SLOTID[:, sbk:sbk + 1], in1=GBC1[:],
                                                                    op0=ALU.is_equal, op1=ALU.mult), [rPOSBC, rGBC1, rC], [rQ[sbk]])

        for ei, (gv, uv, dv) in enumerate(wg_list):
            xg, rxg = XG[ei % 2], rXG[ei % 2]
            for eng in IFE:
                op(eng, lambda e, eng=eng, ei=ei: e.reg_load(B.regs[eng], CNT[0:1, ei:ei + 1]), [rCNT], [], signal=False)
            lnf = (rT1 + rT2 + rSQ + rZB + [rSTAT]) if ei == 0 else []
            for tb in range(4):
                op("dve", lambda e, tb=tb, ei=ei: e.tensor_scalar(out=PE_[tb][:], in0=IOTA_F[:], scalar1=POS[:, tb, ei:ei + 1],
                                                                  scalar2=MSKF[:, tb, ei:ei + 1], op0=ALU.is_equal, op1=ALU.mult),
                   [rPOS, rMSK, rC], [rPE_] + (lnf if tb == 0 else []))
            op("pe", lambda e, ei=ei: e.matmul(PS[6][:, 0:512], lhsT=SEL[:, ei, :], rhs=GTS[0:8, 0:512], start=True, stop=True),
               [rC, rGTS], [rPS[6]])
            op("pe", lambda e, ei=ei: e.matmul(PS[7][:, 0:512], lhsT=SEL[:, ei, :], rhs=POST[0:8, 0:512], start=True, stop=True),
               [rC, rPOST], [rPS[7]])
            op("act", lambda e: e.copy(out=GBC1[:], in_=PS[6][:, 0:512]), [rPS[6]], [rGBC1] + lnf)
            op("act", lambda e: e.copy(out=POSBC[:], in_=PS[7][:, 0:512]), [rPS[7]], [rPOSBC])
            build_q(A_SB)
            gather(ei, xg, rxg, 0, WA)
            if B_SB:
                B.if_begin(IFE, ei, CAP)
                build_q(B_SB)
                gather(ei, xg, rxg, WA, WB)
                B.if_end()
            for gi in range(4):
                wgp, rwg = wload(gv[:, :, gi * 896:(gi + 1) * 896], (KD, 896))
                wup, rwu = wload(uv[:, :, gi * 896:(gi + 1) * 896], (KD, 896))
                wdp, rwd = wload(dv[:, gi * 7:(gi + 1) * 7, :], (7, D))
                hs = cnt % 2
                cnt += 1
                ffn_block(gi, xg, rxg, 0, WA, HTA[hs], rHTA[hs], A_SB, wgp, rwg, wup, rwu, wdp, rwd)
                if B_SB:
                    B.if_begin(IFE, ei, CAP)
                    ffn_block(gi, xg, rxg, WA, WB, HTBv, rHTB, B_SB, wgp, rwg, wup, rwu, wdp, rwd)
                    B.if_end()
            scatter(A_SB)
            if B_SB:
                B.if_begin(IFE, ei, CAP)
                scatter(B_SB)
                B.if_end()

    def conv_module(W, nseq, L, sample, last_prompt):
        PADL = HW_ + L
        gtb = GTB[:, 0:KD * nseq * PADL].rearrange("p (m s t) -> p m s t", m=KD, s=nseq)
        if sample:
            for s in range(nseq):
                xs = s % 2
                dma("sp", XS[xs][0:HW_, :], cache[s], [], [rXS[xs]], "xs%d" % xs)
                bank = s % 2
                for m in range(KD):
                    op("pe", lambda e, m=m, xs=xs, bank=bank: e.transpose(out=PS[bank][:, m * HW_:(m + 1) * HW_],
                                                                          in_=XS[xs][0:HW_, m * 128:(m + 1) * 128], identity=identF[0:HW_, 0:HW_]),
                       [rXS[xs], rC], [rPS[bank]], signal=(m == KD - 1))
                op("dve", lambda e, s=s, bank=bank: e.tensor_copy(out=gtb[:, :, s, 0:HW_],
                                                                  in_=PS[bank][:, 0:KD * HW_].rearrange("p (m t) -> p m t", m=KD)),
                   [rPS[bank]], [rGTB])
        else:
            op("dve", lambda e: e.tensor_copy(out=gtb[:, :, 0, 0:HW_], in_=HIST[:]), [rHIST], [rGTB])
        pieces = {}
        for half in range(2):
            wa, rwa = wload(w_pw1_v[:, :, half * 512:(half + 1) * 512], (KD, 512))
            wgt, rwgt = wload(w_pw1_v[:, :, D + half * 512: D + (half + 1) * 512], (KD, 512))
            for mm in range(4):
                m = half * 4 + mm
                ab, gb = m % 2, 2 + m % 2
                s = m % 2
                for k in range(KD):
                    op("pe", lambda e, k=k, mm=mm, ab=ab, wa=wa: e.matmul(PS[ab][:, 0:W], lhsT=wa[:, k, mm * 128:(mm + 1) * 128],
                                                                           rhs=XB[:, k, 0:W], start=(k == 0), stop=(k == KD - 1)),
                       [rwa, rXB], [rPS[ab]], signal=(k == KD - 1))
                for k in range(KD):
                    op("pe", lambda e, k=k, mm=mm, gb=gb, wgt=wgt: e.matmul(PS[gb][:, 0:W], lhsT=wgt[:, k, mm * 128:(mm + 1) * 128],
                                                                             rhs=XB[:, k, 0:W], start=(k == 0), stop=(k == KD - 1)),
                       [rwgt, rXB], [rPS[gb]], signal=(k == KD - 1))
                op("act", lambda e, m=m, gb=gb, s=s: e.activation(out=T1[s][:, 0:W], in_=PS[gb][:, 0:W], func=AF.Sigmoid,
                                                                  bias=VBPW1[:, 8 + m:9 + m], scale=1.0), [rPS[gb], rC], [rT1[s]])
                a3 = PS[ab][:, 0:W].rearrange("p (s t) -> p s t", s=nseq)
                g3 = T1[s][:, 0:W].rearrange("p (s t) -> p s t", s=nseq)
                op("dve", lambda e, m=m, a3=a3, g3=g3: e.scalar_tensor_tensor(out=gtb[:, m, :, HW_:PADL], in0=a3, scalar=VBPW1[:, m:m + 1],
                                                                              in1=g3, op0=ALU.add, op1=ALU.mult),
                   [rPS[ab], rT1[s], rC], [rGTB])
                if sample or last_prompt:
                    op("dve", lambda e, m=m, a3=a3, g3=g3: e.scalar_tensor_tensor(out=GL[:, m, 0:nseq, :], in0=a3[:, :, L - HW_:L],
                                                                                  scalar=VBPW1[:, m:m + 1], in1=g3[:, :, L - HW_:L],
                                                                                  op0=ALU.add, op1=ALU.mult),
                       [rPS[ab], rT1[s], rC], [rGL])
        if not sample:
            op("dve", lambda e: e.tensor_copy(out=HIST[:], in_=gtb[:, :, 0, L:PADL]), [rGTB], [rHIST])
        if sample or last_prompt:
            for s in range(nseq):
                xs = s % 2
                for m in range(KD):
                    bank = 6 + m // 4
                    op("pe", lambda e, m=m, s=s, bank=bank: e.transpose(out=PS[bank][0:HW_, (m % 4) * 128:(m % 4 + 1) * 128],
                                                                        in_=GL[:, m, s, :], identity=identF[:]),
                       [rGL, rC], [rPS[bank]], signal=(m % 4 == 3))
                op("act", lambda e, xs=xs: e.copy(out=XS[xs][0:HW_, 0:512], in_=PS[6][0:HW_, :]), [rPS[6]], [rXS[xs]])
                op("dve", lambda e, xs=xs: e.tensor_copy(out=XS[xs][0:HW_, 512:1024], in_=PS[7][0:HW_, :]), [rPS[7]], [rXS[xs]])
                r0 = (HW_ * (1 + s)) if sample else 0
                dma("sp", cs_o[r0:r0 + HW_, :], XS[xs][0:HW_, :], [rXS[xs]], [], "ys%d" % xs)
        for m in range(KD):
            ds = m % 2
            for k in range(NTAP):
                op(POOLENG, lambda e, m=m, k=k, ds=ds: e.tensor_scalar(out=DG[ds][:, k, :], in0=identB[:], scalar1=WDW[:, m, k:k + 1],
                                                                       scalar2=None, op0=ALU.mult), [rC], [rDG[ds]])
            bank = 4 + m % 2
            for s in range(nseq):
                for k in range(NTAP):
                    op("pe", lambda e, m=m, k=k, s=s, ds=ds, bank=bank: e.matmul(PS[bank][:, s * L:(s + 1) * L], lhsT=DG[ds][:, k, :],
                                                                                  rhs=gtb[:, m, s, k:k + L], start=(k == 0), stop=(k == NTAP - 1)),
                       [rDG[ds], rGTB], [rPS[bank]], signal=(k == NTAP - 1 and s == nseq - 1))
            op("act", lambda e, m=m, bank=bank: e.activation(out=YC[:, m, 0:W], in_=PS[bank][:, 0:W], func=AF.Identity,
                                                             bias=VBDW[:, m:m + 1], scale=1.0), [rPS[bank], rC], [rYC])

        def outs(m, t2, rt2):
            op("act", lambda e: e.activation(out=ST[:, m, 0:W], in_=t2, func=AF.Silu, bias=VCLB[:, m:m + 1], scale=VCLG[:, m:m + 1]),
               [rt2, rC], [rST])
        layer_norm([(YC[:, m, 0:W], rYC) for m in range(KD)], W, KD, None, outs)
        for half in range(2):
            wp, rwp = wload(w_pw2_v[:, :, half * 512:(half + 1) * 512], (KD, 512))
            for mm in range(4):
                m = half * 4 + mm
                bank = 4 + m % 2
                for k in range(KD):
                    op("pe", lambda e, k=k, mm=mm, bank=bank, wp=wp: e.matmul(PS[bank][:, 0:W], lhsT=wp[:, k, mm * 128:(mm + 1) * 128],
                                                                               rhs=ST[:, k, 0:W], start=(k == 0), stop=(k == KD - 1)),
                       [rwp, rST], [rPS[bank]], signal=(k == KD - 1))
                op("dve", lambda e, m=m, bank=bank: e.scalar_tensor_tensor(out=XF[:, m, 0:W], in0=PS[bank][:, 0:W], scalar=VBPW2[:, m:m + 1],
                                                                           in1=XF[:, m, 0:W], op0=ALU.add, op1=ALU.add),
                   [rPS[bank], rC, rXFm[m]], [rXFm[m]])

    def router(W, sparse=False):
        nb = W // 128
        L_, M1, L2, M2, G = RT["L"], RT["M1"], RT["L2"], RT["M2"], RT["G"]
        for b in range(nb):
            for k in range(KD):
                op("pe", lambda e, k=k, b=b: e.matmul(PS[7][:, 0:NE], lhsT=XF[:, k, b * 128:(b + 1) * 128], rhs=WR[:, k, :],
                                                       start=(k == 0), stop=(k == KD - 1)),
                   [rXFm[k], rC], [rPS[7]], signal=(k == KD - 1))
            ops = [
                lambda e: e.tensor_tensor(out=L_[:], in0=PS[7][:, 0:NE], in1=BR[:], op=ALU.add),
                lambda e: e.tensor_reduce(out=RS["m1"][:], in_=L_[:], axis=AX.X, op=ALU.max),
                lambda e: e.tensor_scalar(out=M1[:], in0=L_[:], scalar1=RS["m1"][:], scalar2=None, op0=ALU.is_equal),
                lambda e: e.scalar_tensor_tensor(out=L2[:], in0=M1[:], scalar=-1e30, in1=L_[:], op0=ALU.mult, op1=ALU.add),
                lambda e: e.tensor_reduce(out=RS["m2"][:], in_=L2[:], axis=AX.X, op=ALU.max),
                lambda e: e.tensor_scalar(out=M2[:], in0=L2[:], scalar1=RS["m2"][:], scalar2=None, op0=ALU.is_equal),
                lambda e: e.tensor_tensor(out=RS["d"][:], in0=RS["m2"][:], in1=RS["m1"][:], op=ALU.subtract),
            ]
            for f in ops:
                op("dve", f, [rPS[7], rC, rRT], [rRT])
            op("act", lambda e: e.activation(out=RS["e"][:], in_=RS["d"][:], func=AF.Exp), [rRT], [rRT])
            ops = [
                lambda e: e.tensor_scalar(out=RS["den"][:], in0=RS["e"][:], scalar1=1.0, scalar2=None, op0=ALU.add),
                lambda e: e.reciprocal(out=RS["w1"][:], in_=RS["den"][:]),
                lambda e: e.tensor_tensor(out=RS["w2"][:], in0=RS["e"][:], in1=RS["w1"][:], op=ALU.mult),
                lambda e: e.tensor_scalar(out=G[:], in0=M1[:], scalar1=RS["w1"][:], scalar2=None, op0=ALU.mult),
                lambda e: e.scalar_tensor_tensor(out=G[:], in0=M2[:], scalar=RS["w2"][:], in1=G[:], op0=ALU.mult, op1=ALU.add),
            ]
            for f in ops:
                op("dve", f, [rRT], [rRT])
            op("pe", lambda e: e.transpose(out=PS[6][0:NE, 0:128], in_=G[:], identity=identF[:]), [rRT, rC], [rPS[6]])
            op("dve", lambda e, b=b: e.tensor_copy(out=GTS[0:8, b * 128:(b + 1) * 128], in_=PS[6][0:NE, 0:128]), [rPS[6]], [rGTS])
            if sparse:
                op("dve", lambda e, b=b: e.tensor_tensor(out=MSKF[:, b, :], in0=M1[:], in1=M2[:], op=ALU.add), [rRT], [rMSK])
                op("dve", lambda e, b=b: e.tensor_copy(out=MSK[:, b, :], in_=MSKF[:, b, :]), [rMSK], [rMSK])
        if sparse:
            for b in range(nb):
                terms = [(onesB, bb) for bb in range(b)] + [(UTRI, b)]
                for i, (lt, bb) in enumerate(terms):
                    op("pe", lambda e, lt=lt, bb=bb, b=b, i=i, n=len(terms): e.matmul(PS[6][:, b * NE:(b + 1) * NE], lhsT=lt[:], rhs=MSK[:, bb, :],
                                                                                      start=(i == 0), stop=(i == n - 1)),
                       [rMSK, rC], [rPS[6]], signal=False)
            for b in range(nb):
                op("pe", lambda e, b=b: e.matmul(PS[6][:, 4 * NE:5 * NE], lhsT=onesB[:], rhs=MSK[:, b, :], start=(b == 0), stop=(b == nb - 1)),
                   [rMSK, rC], [rPS[6]], signal=(b == nb - 1))
            op("dve", lambda e: e.tensor_copy(out=POS[:], in_=PS[6][:, 0:4 * NE].rearrange("p (b e) -> p b e", b=4)), [rPS[6]], [rPOS])
            op("dve", lambda e: e.tensor_copy(out=CNT[:], in_=PS[6][0:1, 4 * NE:5 * NE]), [rPS[6]], [rCNT])
            for b in range(nb):
                op("pe", lambda e, b=b: e.transpose(out=PS[7][0:NE, b * 128:(b + 1) * 128], in_=POS[:, b, :], identity=identF[:]),
                   [rPOS, rC], [rPS[7]], signal=(b == nb - 1))
            op("dve", lambda e: e.tensor_copy(out=POST[:], in_=PS[7][0:NE, :]), [rPS[7]], [rPOST])
            for b in range(nb):
                bank = 4 + b % 2
                psb = PS[bank][:].bitcast(BF16)
                for k in range(KD):
                    op("pe", lambda e, k=k, b=b, psb=psb: e.transpose(out=psb[:, k * 128:(k + 1) * 128], in_=XB[:, k, b * 128:(b + 1) * 128],
                                                                       identity=identB[:]),
                       [rXB, rC], [rPS[bank]], signal=(k == KD - 1))
                if b % 2 == 0:
                    op("act", lambda e, b=b, psb=psb: e.copy(out=XTOK[:, b, :], in_=psb), [rPS[bank]], [rXTOK])
                else:
                    op("dve", lambda e, b=b, psb=psb: e.tensor_copy(out=XTOK[:, b, :], in_=psb), [rPS[bank]], [rXTOK])
        for m in range(KD):
            op("act", lambda e, m=m: e.mul(out=XF[:, m, 0:W], in_=XF[:, m, 0:W], mul=ALPHA), [rXFm[m]], [rXFm[m]])

    ff_views = [(ffg.rearrange("(k p) n -> p k n", p=128), ffu.rearrange("(k p) n -> p k n", p=128),
                 ffd.rearrange("(c p) n -> p c n", p=128))]
    moe_views = [(mog[e_].rearrange("(k p) n -> p k n", p=128), mou[e_].rearrange("(k p) n -> p k n", p=128),
                  mod[e_].rearrange("(c p) n -> p c n", p=128)) for e_ in range(NE)]

    tiles = [(i * WT, WT, 1, WT, False, i == NPT - 1) for i in range(NPT)]
    tiles.append((SEQ, NSAMP * LS, NSAMP, LS, True, False))
    stop = dbg.get("stop")
    if "tiles" in dbg:
        tiles = [tiles[i] for i in dbg["tiles"]]
    for ti_, (tok0, W, nseq, L, sample, last_prompt) in enumerate(tiles):
        ring_state["piece"] = 0
        ring_state["first"] = (ti_ == 0)
        if not dbg.get("noload"):
            load_tile(tok0, W)
        if sample:
            gv_rows = {0: 128, 1: 256}
        elif last_prompt:
            gv_rows = {3: 0}
        else:
            gv_rows = {}
        phases = [
            ("load", lambda: None),
            ("gmlp", lambda: gmlp(W, sample, gv_rows)),
            ("ln0", lambda: deepnorm_ln(W, 0, scaled=True)),
            ("ffn", lambda: experts(W, ff_views, gated=False)),
            ("ln1", lambda: deepnorm_ln(W, 1, scaled=True)),
            ("conv", lambda: conv_module(W, nseq, L, sample, last_prompt)),
            ("ln2", lambda: deepnorm_ln(W, 2, scaled=False)),
            ("router", lambda: router(W, sparse=(not sample) and not dbg.get("dense"))),
            ("moe", lambda: (experts(W, moe_views[:dbg.get("nexp", NE)], gated=True) if (sample or dbg.get("dense"))
                             else experts_sparse(moe_views[:dbg.get("nexp", NE)]))),
            ("ln3", lambda: deepnorm_ln(W, 3, scaled=False, make_xb=False)),
        ]
        for name, fn in phases:
            fn()
            if stop == name:
                break
        if not dbg.get("nostore"):
            store_tile(tok0, W)
    B.final_wait("sp")
    B.final_wait("pool")
    B.emit()


_NC_CACHE = {}


def kernel(x_prompt, x_sample, cache_conv, gm_w_in, gm_b_in, gm_lnv_g, gm_lnv_b, gm_w_s, gm_b_s, gm_w_out, gm_b_out,
           cv_w_pw1, cv_b_pw1, cv_w_dw, cv_b_dw, cv_ln_g, cv_ln_b, cv_w_pw2, cv_b_pw2,
           ff_w_gate, ff_w_up, ff_w_down, moe_w_router, moe_b_router, moe_w_gate, moe_w_up, moe_w_down, ln_g, ln_b):
    f = lambda a: np.ascontiguousarray(np.asarray(a, dtype=np.float32))
    shared = {
        "gm_w_in": f(gm_w_in[0]), "gm_b_in": f(gm_b_in).reshape(1, -1), "gm_lnv_g": f(gm_lnv_g).reshape(1, -1),
        "gm_lnv_b": f(gm_lnv_b).reshape(1, -1), "gm_w_s": f(gm_w_s[0]), "gm_b_s": f(gm_b_s[0]),
        "gm_w_out": f(gm_w_out[0]), "gm_b_out": f(gm_b_out).reshape(1, -1),
        "cv_w_pw1": f(cv_w_pw1[0]), "cv_b_pw1": f(cv_b_pw1).reshape(1, -1), "cv_w_dw": f(cv_w_dw[0]),
        "cv_b_dw": f(cv_b_dw).reshape(1, -1), "cv_ln_g": f(cv_ln_g).reshape(1, -1), "cv_ln_b": f(cv_ln_b).reshape(1, -1),
        "cv_w_pw2": f(cv_w_pw2[0]), "cv_b_pw2": f(cv_b_pw2).reshape(1, -1),
        "ff_w_gate": f(ff_w_gate[0]), "ff_w_up": f(ff_w_up[0]), "ff_w_down": f(ff_w_down[0]),
        "moe_w_router": f(moe_w_router[0]), "moe_b_router": f(moe_b_router).reshape(1, -1),
        "moe_w_gate": f(moe_w_gate[0]), "moe_w_up": f(moe_w_up[0]), "moe_w_down": f(moe_w_down[0]),
        "ln_g": f(ln_g).reshape(1, -1), "ln_b": f(ln_b).reshape(1, -1),
    }
    xp = f(x_prompt)
    xs = f(x_sample)
    cc = f(cache_conv)
    in_maps = []
    for c in range(NCORE):
        xin = np.concatenate([xp[c], xs[c * NSAMP:(c + 1) * NSAMP].reshape(NSAMP * LS, D)], axis=0)
        m = dict(shared)
        m["xin"] = np.ascontiguousarray(xin)
        m["cache"] = np.ascontiguousarray(cc[0, c * NSAMP:(c + 1) * NSAMP])
        in_maps.append(m)
    if "nc" not in _NC_CACHE:
        _NC_CACHE["nc"] = build_program()
    res = run_bass_kernel_spmd(_NC_CACHE["nc"], in_maps, core_ids=list(range(NCORE)))
    y_p = np.empty((8, SEQ, D), np.float32)
    y_s = np.empty((32, LS, D), np.float32)
    gv_p = np.empty((1, 8, 128, DV), np.float32)
    gv_s = np.empty((1, 32, LS, DV), np.float32)
    cs_p = np.empty((1, 8, HW_, D), np.float32)
    cs_s = np.empty((1, 32, HW_, D), np.float32)
    for c in range(NCORE):
        r = res.results[c]
        y_p[c] = r["y_o"][0:SEQ]
        y_s[c * NSAMP:(c + 1) * NSAMP] = r["y_o"][SEQ:].reshape(NSAMP, LS, D)
        gv_p[0, c] = r["gv_o"][0:128]
        gv_s[0, c * NSAMP:(c + 1) * NSAMP] = r["gv_o"][128:].reshape(NSAMP, LS, DV)
        cs_p[0, c] = r["cs_o"][0:HW_]
        cs_s[0, c * NSAMP:(c + 1) * NSAMP] = r["cs_o"][HW_:].reshape(NSAMP, HW_, D)
    return y_p, y_s, gv_p, gv_s, cs_p, cs_s
```

```python
import numpy as np
from contextlib import ExitStack
import concourse.bass as bass
import concourse.mybir as mybir
from concourse.bass_utils import run_bass_kernel_spmd

F32 = mybir.dt.float32
BF16 = mybir.dt.bfloat16
AF = mybir.ActivationFunctionType
ALU = mybir.AluOpType
AX = mybir.AxisListType

D = 1024
KD = 8
DV = 3072
CV = 24
FF = 3584
NE = 8
SEQ = 4096
NCORE = 8
NSAMP = 4
LS = 64
HW_ = 30
NTAP = 31
ALPHA = float((2.0 * 2) ** 0.25)
EPS = 1e-5
TOK = SEQ + NSAMP * LS
WT = 512
NPT = SEQ // WT
SLOT = 7168
NSLOT = 5
import os as _os
NOSELF = bool(_os.environ.get("NOSELF"))
POOLENG = "pool" if _os.environ.get("USEPOOL") else "dve"


class Res:
    __slots__ = ("w", "r", "const", "excl")

    def __init__(self, const=False, excl=False):
        self.w = None
        self.r = {}
        self.const = const
        self.excl = excl


class Builder:
    def __init__(self, nc, es):
        self.nc = nc
        self.es = es
        self.engs = {"pe": nc.tensor, "act": nc.scalar, "dve": nc.vector, "pool": nc.gpsimd, "sp": nc.sync}
        self.prog = {k: [] for k in self.engs}
        self.cnt = {k: 0 for k in self.engs}
        self.waited = {k: {} for k in self.engs}
        self.S = {k: es.enter_context(nc.semaphore("S_" + k)) for k in ("pe", "act", "dve", "pool")}
        self.dsem = {}
        self.regs = {"pe": es.enter_context(nc.tensor.register("cnt_pe")),
                     "act": es.enter_context(nc.scalar.register("cnt_act")),
                     "dve": es.enter_context(nc.vector.register("cnt_dve"))}
        self.semobj = {}
        for k, s in self.S.items():
            self.semobj[id(s)] = s

    def sb(self, name, shape, dt):
        return self.es.enter_context(self.nc.sbuf_tensor(name, shape, dt))

    def dma_sem(self, name):
        if name not in self.dsem:
            s = self.es.enter_context(self.nc.semaphore("D_" + name))
            self.dsem[name] = [s, 0]
            self.semobj[id(s)] = s
        return self.dsem[name]

    def _collect(self, eng, reads, writes):
        waits = {}
        own = id(self.S[eng]) if eng in self.S else None

        def need(ev):
            if ev is None:
                return
            sid, val = ev
            if sid == own and (eng == "pe" or NOSELF):
                return
            if self.waited[eng].get(sid, 0) >= val:
                return
            if waits.get(sid, 0) < val:
                waits[sid] = val

        for r in reads:
            need(r.w)
        for w in writes:
            need(w.w)
            for sid, val in w.r.items():
                need((sid, val))
        for sid, val in waits.items():
            self.waited[eng][sid] = val
        return [(self.semobj[sid], val) for sid, val in waits.items()]

    def _commit(self, ev, reads, writes):
        for r in reads:
            if not r.const:
                if r.r.get(ev[0], 0) < ev[1]:
                    r.r[ev[0]] = ev[1]
        for w in writes:
            w.w = ev
            w.r = {}

    def op(self, eng, fn, reads=(), writes=(), signal=True):
        if eng != "pe":
            ex = [r for r in reads if r.excl]
            if ex:
                reads = [r for r in reads if not r.excl]
                writes = list(writes) + ex
        waits = self._collect(eng, reads, writes)
        if signal:
            self.cnt[eng] += 1
            ev = (id(self.S[eng]), self.cnt[eng])
        else:
            ev = (id(self.S[eng]), self.cnt[eng] + 1)
        self._commit(ev, reads, writes)
        self.prog[eng].append((waits, fn, (self.S[eng], 1) if signal else None))

    def dma(self, q, out, in_, reads, writes, semname, **kw):
        waits = self._collect(q, reads, writes)
        ds = self.dma_sem(semname)
        ds[1] += 16
        ev = (id(ds[0]), ds[1])
        self._commit(ev, reads, writes)
        self.prog[q].append((waits, lambda e: e.dma_start(out=out, in_=in_, **kw), (ds[0], 16)))

    def if_begin(self, engs, ei, thresh):
        self._if = {}
        for eng in engs:
            entry = ["IF", ei, thresh, 0]
            self.prog[eng].append(entry)
            self._if[eng] = (entry, self.cnt[eng], dict(self.waited[eng]))

    def if_end(self):
        for eng, (entry, c0, w0) in self._if.items():
            entry[3] = self.cnt[eng] - c0
            self.prog[eng].append(["ENDIF"])
            self.waited[eng] = w0
        self._if = {}

    def barrier(self, engs):
        for eng in engs:
            waits = []
            for k, sem in self.S.items():
                if self.cnt[k] > 0 and not (eng == "pe" and k == "pe"):
                    if self.waited[eng].get(id(sem), 0) < self.cnt[k]:
                        self.waited[eng][id(sem)] = self.cnt[k]
                        waits.append((sem, self.cnt[k]))
            for name, (sem, tot) in self.dsem.items():
                if tot > 0 and self.waited[eng].get(id(sem), 0) < tot:
                    self.waited[eng][id(sem)] = tot
                    waits.append((sem, tot))
            self.prog[eng].append((waits, None, None))

    def final_wait(self, q):
        waits = []
        for name, (s, tot) in self.dsem.items():
            if tot > 0:
                waits.append((s, tot))
        self.prog[q].append((waits, None, None))

    def emit(self):
        nc = self.nc
        block = self.es.enter_context(nc.Block())

        def replay(key, e):
            prog = self.prog[key]
            i = 0
            n = len(prog)

            def run(entry):
                waits, fn, inc = entry
                for s_, v in waits:
                    e.wait_ge(s_, v)
                if fn is None:
                    return
                ins = fn(e)
                if inc is not None:
                    ins.then_inc(inc[0], inc[1])

            while i < n:
                entry = prog[i]
                if entry[0] == "IF":
                    _, ei, thresh, k = entry
                    j = i + 1
                    while prog[j][0] != "ENDIF":
                        j += 1
                    reg = self.regs[key]
                    with e.If_lt(reg, thresh + 1):
                        if k > 0:
                            e.drain().then_inc(self.S[key], k)
                    with e.Else():
                        for ent in prog[i + 1:j]:
                            run(ent)
                    i = j + 1
                    continue
                run(entry)
                i += 1

        @block.tensor
        def _(e):
            replay("pe", e)

        @block.scalar
        def _(e):
            replay("act", e)

        @block.vector
        def _(e):
            replay("dve", e)

        @block.gpsimd
        def _(e):
            replay("pool", e)

        @block.sync
        def _(e):
            replay("sp", e)


def build_program(dbg=None):
    nc = bass.Bass("TRN2", target_bir_lowering=False)
    es = ExitStack()
    with es:
        _build(nc, es, dbg or {})
    return nc


def _build(nc, es, dbg):
    B = Builder(nc, es)
    op, dma, sb = B.op, B.dma, B.sb

    def din(name, shape):
        return nc.dram_tensor(name, shape, F32, kind="ExternalInput").ap()

    def dout(name, shape):
        return nc.dram_tensor(name, shape, F32, kind="ExternalOutput").ap()

    xin = din("xin", [TOK, D])
    cache = din("cache", [NSAMP, HW_, D])
    w_in = din("gm_w_in", [D, 2 * DV])
    b_in = din("gm_b_in", [1, 2 * DV])
    lnv_g = din("gm_lnv_g", [1, DV])
    lnv_b = din("gm_lnv_b", [1, DV])
    w_s = din("gm_w_s", [8, 128, 128])
    b_s = din("gm_b_s", [8, 128])
    w_out = din("gm_w_out", [DV, D])
    b_out = din("gm_b_out", [1, D])
    w_pw1 = din("cv_w_pw1", [D, 2 * D])
    b_pw1 = din("cv_b_pw1", [1, 2 * D])
    w_dw = din("cv_w_dw", [NTAP, D])
    b_dw = din("cv_b_dw", [1, D])
    cln_g = din("cv_ln_g", [1, D])
    cln_b = din("cv_ln_b", [1, D])
    w_pw2 = din("cv_w_pw2", [D, D])
    b_pw2 = din("cv_b_pw2", [1, D])
    ffg = din("ff_w_gate", [D, FF])
    ffu = din("ff_w_up", [D, FF])
    ffd = din("ff_w_down", [FF, D])
    w_r = din("moe_w_router", [D, NE])
    b_r = din("moe_b_router", [1, NE])
    mog = din("moe_w_gate", [NE, D, FF])
    mou = din("moe_w_up", [NE, D, FF])
    mod = din("moe_w_down", [NE, FF, D])
    ln_g = din("ln_g", [1, 4 * D])
    ln_b = din("ln_b", [1, 4 * D])
    y_o = dout("y_o", [TOK, D])
    gv_o = dout("gv_o", [128 + NSAMP * LS, DV])
    cs_o = dout("cs_o", [HW_ * (1 + NSAMP), D])

    XF = sb("XF", [128, KD, WT], F32)
    XB = sb("XB", [128, KD, WT], BF16)
    RING = [sb("ring%d" % i, [128, SLOT], BF16) for i in range(NSLOT)]
    PH = sb("PH", [128, 24576], BF16)
    XS0 = sb("XS0", [128, D], F32)
    XS = [XS0, XS0]
    BROWF = PH[0:1, 0:4096].bitcast(F32).rearrange("o (a n) -> o a n", a=2)
    BROWT = PH[0:1, 4096:8192].bitcast(F32).rearrange("o (a n) -> o a n", a=2)
    WSA = PH[:, 8192:10240].bitcast(F32).rearrange("p (g j) -> p g j", g=8)
    IOTA_I = PH[:, 10240:11264].bitcast(mybir.dt.int32)
    UTRI_F = PH[:, 11264:11520].bitcast(F32)
    LNS = sb("LNS", [128, 13312], BF16)

    def lnv(off, n, f32=True):
        v = LNS[:, off:off + n]
        return v.bitcast(F32) if f32 else v
    MEAN, VAR, RSTD = lnv(0, 1024), lnv(1024, 1024), lnv(2048, 1024)
    T1 = [lnv(3072 + i * 1024, 1024) for i in range(2)]
    T2 = [lnv(5120 + i * 1024, 1024) for i in range(2)]
    SQ = [lnv(7168 + i * 512, 512, False) for i in range(2)]
    ZB = [lnv(8192 + i * 512, 512, False) for i in range(2)]
    identF = sb("identF", [128, 128], F32)
    identB = sb("identB", [128, 128], BF16)
    onesB = sb("onesB", [128, 128], BF16)
    WST = [sb("WST%d" % i, [128, 8, 128], BF16) for i in range(2)]
    BROW = sb("BROW", [1, 4, 1024], BF16)
    SEL = sb("SEL", [8, NE, 128], F32)
    WR = sb("WR", [128, KD, NE], F32)
    BR = sb("BR", [128, NE], F32)
    GTS = sb("GTS", [8, WT], F32)
    VB_IN = sb("VB_IN", [128, 48], F32)
    VLNVG = sb("VLNVG", [128, CV], F32)
    VLNVB = sb("VLNVB", [128, CV], F32)
    VBOUT = sb("VBOUT", [128, KD], F32)
    VBPW1 = sb("VBPW1", [128, 16], F32)
    WDW = sb("WDW", [128, KD, NTAP], F32)
    VBDW = sb("VBDW", [128, KD], F32)
    VCLG = sb("VCLG", [128, KD], F32)
    VCLB = sb("VCLB", [128, KD], F32)
    VBPW2 = sb("VBPW2", [128, KD], F32)
    VLNG = sb("VLNG", [128, 32], F32)
    VLNB = sb("VLNB", [128, 32], F32)
    VLNGA = sb("VLNGA", [128, 32], F32)
    VLNBA = sb("VLNBA", [128, 32], F32)
    EPST = sb("EPST", [128, 1], F32)
    HIST = sb("HIST", [128, KD, HW_], BF16)
    GL = sb("GL", [128, KD, NSAMP, HW_], F32)
    RT = {n: sb("RT_" + n, [128, NE], F32) for n in ("L", "M1", "L2", "M2", "G")}
    RS = {n: sb("RS_" + n, [128, 1], F32) for n in ("m1", "m2", "d", "e", "den", "w1", "w2")}
    I32 = mybir.dt.int32
    CAP = int(dbg.get("cap", 192))
    PM = LNS
    IOTA_F = sb("IOTA_F", [128, WT], F32)
    SLOT_I = sb("SLOT_I", [128, 4], I32)
    SLOTID = sb("SLOTID", [128, 4], F32)
    UTRI = sb("UTRI", [128, 128], BF16)
    MSKF = sb("MSKF", [128, 4, NE], F32)
    MSK = sb("MSK", [128, 4, NE], BF16)
    POS = sb("POS", [128, 4, NE], F32)
    POST = sb("POST", [8, WT], F32)
    CNT = sb("CNT", [1, NE], I32)
    PS = [es.enter_context(nc.psum_tensor("ps%d" % i, [128, 512], F32)) for i in range(8)]

    R = {}

    def res(name, const=False):
        if name not in R:
            R[name] = Res(const)
        return R[name]

    rPS = [res("ps%d" % i) for i in range(8)]
    for r_ in rPS:
        r_.excl = True
    rRING = [res("ring%d" % i) for i in range(NSLOT)]
    rXFm = [res("XF%d" % m) for m in range(KD)]
    rXB = res("XB")
    rXS = [res("XS0"), res("XS0")]
    rC = res("consts")
    rCd, rWSA, rCp, rCv = res("Cd"), res("WSA"), res("Cp"), res("Cv")

    def phv(off, n):
        return PH[:, off:off + n]

    VNT = phv(0, 12288).rearrange("p (c t) -> p c t", c=CV)
    OT = VNT
    VN = phv(12288, 12288).rearrange("p (b c) -> p b c", b=4)
    GTB = phv(0, 4336)
    DG = [phv(4336 + i * 3968, 3968).rearrange("p (k c) -> p k c", k=NTAP) for i in range(2)]
    ST = phv(12272, 4096).rearrange("p (m t) -> p m t", m=KD)
    YC = phv(16384, 8192).bitcast(F32).rearrange("p (m t) -> p m t", m=KD)
    HT = [phv(i * 3584, 3584).rearrange("p (f t) -> p f t", f=7) for i in range(2)]
    SS = [phv(7168 + i * 512, 512) for i in range(2)]
    TT = [phv(8192 + i * 512, 512) for i in range(2)]
    GBC = [phv(9216 + i * 1024, 1024).bitcast(F32) for i in range(2)]
    rVNT, rVN, rGTB, rST, rYC = res("VNT"), res("VN"), res("GTB"), res("ST"), res("YC")
    rVNTc = [res("VNTc%d" % c) for c in range(CV)]
    rDG = [res("DG0"), res("DG1")]
    rHT = [res("HT0"), res("HT1")]
    rSS = [res("SS0"), res("SS1")]
    rTT = [res("TT0"), res("TT1")]
    rGBC = [res("GBC0"), res("GBC1")]
    rT1 = [res("T1_0"), res("T1_1")]
    rT2 = [res("T2_0"), res("T2_1")]
    rSQ = [res("SQ0"), res("SQ1")]
    rZB = [res("ZB0"), res("ZB1")]
    rSTAT = res("STAT")
    rHIST, rGL, rGTS, rRT = res("HIST"), res("GL"), res("GTS"), res("RT")
    WA = CAP
    THR = int(dbg.get("thr", CAP))
    if CAP == 256:
        A_BLK = [(0, 0, 128, 0, 128), (1, 128, 128, 0, 128)]
        B_BLK = [(2, 0, 128, 0, 128), (3, 128, 128, 0, 128)]
        B_C0 = 256
    else:
        assert CAP == 192
        A_BLK = [(0, 0, 128, 0, 128), (1, 128, 64, 0, 64)]
        B_BLK = [(1, 0, 128, 64, 64), (2, 128, 128, 0, 128), (3, 256, 128, 0, 128)]
        B_C0 = 128
    WB = WT - B_C0
    B_SB = [1]
    HTA = [phv(i * 1792, 7 * WA).rearrange("p (f t) -> p f t", f=7) for i in range(2)]
    HTBv = phv(3584, 7 * WB).rearrange("p (f t) -> p f t", f=7)
    SSA = [phv(6272 + i * 384, 384) for i in range(2)]
    XTOK = phv(7040, 4096).rearrange("p (b c) -> p b c", b=4)
    PE_ = [phv(11136 + i * 512, 512) for i in range(4)]
    Q_ = [phv(13184 + i * 512, 512) for i in range(4)]
    XG = [phv(15232 + i * 4096, 4096).rearrange("p (k t) -> p k t", k=KD) for i in range(2)]
    GBC1 = phv(23424, 1024).bitcast(F32)
    ACC = [PM[:, i * 2048:(i + 1) * 2048].bitcast(F32) for i in range(4)]
    ACCB = [PM[:, 8192 + i * 1024: 8192 + (i + 1) * 1024] for i in range(4)]
    POSBC = PM[:, 12288:13312].bitcast(F32)
    rXTOK, rPE_, rGBC1, rPOSBC, rHTB = res("XTOK"), res("PE_"), res("GBC1"), res("POSBC"), res("HTB")
    rQ = [res("Q%d" % i) for i in range(4)]
    rXG = [res("XG0"), res("XG1")]
    rHTA = [res("HTA0"), res("HTA1")]
    rSSA = [res("SSA0"), res("SSA1")]
    rACC = [res("ACC%d" % i) for i in range(4)]
    rACCB = [res("ACCB%d" % i) for i in range(4)]
    rMSK, rPOS, rCNT, rPOST = res("MSK"), res("POS"), res("CNT"), res("POST")

    def vec_load(dst, src_row, n):
        dma("sp", dst, src_row.rearrange("o (c p) -> p (o c)", p=128), [], [rCd], "const",
            allow_slow_non_contiguous=True)

    skip = dbg.get("skip", ())
    if 'vec' not in skip:
        vec_load(VB_IN[:], b_in, 48)
        vec_load(VLNVG[:], lnv_g, CV)
        vec_load(VLNVB[:], lnv_b, CV)
        vec_load(VBOUT[:], b_out, KD)
        vec_load(VBPW1[:], b_pw1, 16)
        vec_load(VBDW[:], b_dw, KD)
        vec_load(VCLG[:], cln_g, KD)
        vec_load(VCLB[:], cln_b, KD)
        vec_load(VBPW2[:], b_pw2, KD)
        vec_load(VLNG[:], ln_g, 32)
        vec_load(VLNB[:], ln_b, 32)
    if 'wdw' not in skip:
        for k in range(NTAP):
            dma("sp", WDW[:, :, k], w_dw[k:k + 1, :].rearrange("o (c p) -> p (o c)", p=128), [], [rCd], "const",
                allow_slow_non_contiguous=True)
    if 'wr' not in skip:
        dma("sp", WR[:], w_r.rearrange("(k p) e -> p k e", p=128), [], [rCd], "const")
    if 'br' not in skip:
        dma("sp", BR[:], b_r.partition_broadcast(128), [], [rCd], "const")
    if 'wsa' not in skip:
        dma("sp", WSA[:], w_s.rearrange("g i j -> i g j"), [], [rWSA], "wsa")
    if 'browf' not in skip:
        dma("sp", BROWF[:, 0, :], b_s.rearrange("(o g) i -> o (g i)", o=1), [], [rCd], "const")
        for h in range(2):
            dma("sp", BROWF[:, 1, :].rearrange("o (g i) -> o g i", g=8)[:, :, h * 64:(h + 1) * 64],
                b_s[:, 0:64].rearrange("(o g) i -> o g i", o=1), [], [rCd], "const")
    if 'pool' not in skip:
        op("pool", lambda e: e.memset(identF[:], 0.0), [], [rCp])
        op("pool", lambda e: e.affine_select(out=identF[:], in_=identF[:], compare_op=ALU.not_equal, fill=1.0,
                                             base=0, pattern=[[-1, 128]], channel_multiplier=1), [], [rCp])
    if 'sel' not in skip:
        op("pool", lambda e: e.memset(SEL[:], 0.0), [], [rCp])
        for ee in range(NE):
            op("pool", lambda e, ee=ee: e.affine_select(out=SEL[:, ee, :], in_=SEL[:, ee, :], compare_op=ALU.not_equal,
                                                        fill=1.0, base=-ee, pattern=[[0, 128]], channel_multiplier=1),
               [], [rCp])
    if 'pool2' not in skip:
        op("pool", lambda e: e.memset(EPST[:], EPS), [], [rCp])
        op("pool", lambda e: e.memset(onesB[:], 1.0), [], [rCp])
        op("pool", lambda e: e.memset(HIST[:], 0.0), [], [rHIST])
        op("pool", lambda e: e.iota(out=IOTA_I[:], pattern=[[1, WT]], base=0, channel_multiplier=0), [], [rCp])
        op("pool", lambda e: e.iota(out=SLOT_I[:], pattern=[[128, 4]], base=0, channel_multiplier=1), [], [rCp])
        op("pool", lambda e: e.memset(UTRI_F[:], 1.0), [], [rCp])
        op("pool", lambda e: e.affine_select(out=UTRI_F[:], in_=UTRI_F[:], compare_op=ALU.is_gt, fill=0.0, base=0,
                                             pattern=[[1, 128]], channel_multiplier=-1), [], [rCp])
        op("dve", lambda e: e.tensor_copy(out=IOTA_F[:], in_=IOTA_I[:]), [rCp], [rCv])
        op("dve", lambda e: e.tensor_copy(out=SLOTID[:], in_=SLOT_I[:]), [rCp], [rCv])
        op("dve", lambda e: e.tensor_copy(out=UTRI[:], in_=UTRI_F[:]), [rCp], [rCv])
    if 'identb' not in skip:
        op("dve", lambda e: e.tensor_copy(out=identB[:], in_=identF[:]), [rCp], [rCv])
    if 'lnga' not in skip:
        op("dve", lambda e: e.tensor_scalar(out=VLNGA[:], in0=VLNG[:], scalar1=ALPHA, scalar2=None, op0=ALU.mult), [rCd], [rCv])
        op("dve", lambda e: e.tensor_scalar(out=VLNBA[:], in0=VLNB[:], scalar1=ALPHA, scalar2=None, op0=ALU.mult), [rCd], [rCv])
    if 'brow' not in skip:
        op("dve", lambda e: e.tensor_copy(out=BROW[:, 0, :], in_=BROWF[:, 0, :]), [rCd, rCv], [rCv])
        op("dve", lambda e: e.tensor_copy(out=BROW[:, 2, :], in_=BROWF[:, 1, :]), [rCd, rCv], [rCv])
        op("dve", lambda e: e.tensor_tensor(out=BROWT[:, 0, :], in0=BROWF[:, 0, :], in1=BROW[:, 0, :], op=ALU.subtract), [rCd, rCv], [rCv])
        op("dve", lambda e: e.tensor_tensor(out=BROWT[:, 1, :], in0=BROWF[:, 1, :], in1=BROW[:, 2, :], op=ALU.subtract), [rCd, rCv], [rCv])
        op("dve", lambda e: e.tensor_copy(out=BROW[:, 1, :], in_=BROWT[:, 0, :]), [rCd, rCv], [rCv])
        op("dve", lambda e: e.tensor_copy(out=BROW[:, 3, :], in_=BROWT[:, 1, :]), [rCd, rCv], [rCv])
    if 'wstp' not in skip:
        op("dve", lambda e: e.memset(WST[0][:], 0.0), [], [rCv])
        op("dve", lambda e: e.memset(WST[1][:], 0.0), [], [rCv])
        for g in range(8):
            bank = g % 2
            op("pe", lambda e, g=g, bank=bank: e.transpose(out=PS[bank][:, 0:128], in_=WSA[:, g, :], identity=identF[:]),
               [rWSA, rCp], [rPS[bank]])
            op("dve", lambda e, g=g, bank=bank: e.tensor_copy(out=WST[0][0:64, g, :], in_=PS[bank][0:64, 0:128]),
               [rPS[bank]], [rCv])
            op("dve", lambda e, g=g, bank=bank: e.tensor_copy(out=WST[0][64:128, g, 64:128], in_=PS[bank][64:128, 64:128]),
               [rPS[bank]], [rCv])
    if 'wsts' not in skip:
        op("dve", lambda e: e.memset(WSA[:], 0.0), [], [rWSA])
        dma("sp", WSA[0:64, :, 0:64], w_s[:, 0:64, 0:64].rearrange("g i j -> i g j"), [], [rWSA], "wsa")
        dma("sp", WSA[64:128, :, 64:128], w_s[:, 0:64, 0:64].rearrange("g i j -> i g j"), [], [rWSA], "wsa")
        for g in range(8):
            bank = g % 2
            op("pe", lambda e, g=g, bank=bank: e.transpose(out=PS[bank][:, 0:128], in_=WSA[:, g, :], identity=identF[:]),
               [rWSA, rCp], [rPS[bank]])
            op("dve", lambda e, g=g, bank=bank: e.tensor_copy(out=WST[1][:, g, :], in_=PS[bank][:, 0:128]),
               [rPS[bank]], [rCv])
    B.barrier(["pe", "act", "dve", "pool"])
    rC.w = None
    rC.r = {}
    rC.const = True

    ring_state = {"i": 0, "piece": 0, "first": True}
    NPIECE = 126
    WSCR = nc.dram_tensor("wscr", [NPIECE, 128, SLOT], BF16, kind="Internal").ap()
    rSCR = [Res() for _ in range(NPIECE)]

    def wload(src3, shape):
        i = ring_state["i"] % NSLOT
        ring_state["i"] += 1
        j = ring_state["piece"]
        ring_state["piece"] += 1
        a, b = shape
        flat = RING[i][:, 0:a * b]
        view = flat.rearrange("p (a b) -> p a b", a=a)
        if ring_state["first"] or dbg.get("noscr"):
            dma("pool", view, src3, [], [rRING[i]], "rgs%d" % i)
            if not dbg.get("noscr"):
                dma("sp", WSCR[j, :, 0:a * b], flat, [rRING[i]], [rSCR[j]], "wst%d" % i)
        else:
            dma("sp", flat, WSCR[j, :, 0:a * b], [rSCR[j]], [rRING[i]], "rgh%d" % i)
        return view, rRING[i]

    w_in_v = w_in.rearrange("(k p) n -> p k n", p=128)
    w_out_v = w_out.rearrange("(c p) n -> p c n", p=128)
    w_pw1_v = w_pw1.rearrange("(k p) n -> p k n", p=128)
    w_pw2_v = w_pw2.rearrange("(k p) n -> p k n", p=128)

    def layer_norm(zs, W, nch, srcs_bf=None, outs=None, fence=()):
        inv = 1.0 / (nch * 128)
        for m in range(nch):
            z, rz = zs[m]
            s = m % 2
            fz = list(fence) if m == 0 else []
            op("act", lambda e, z=z, s=s: e.activation(out=SQ[s][:, 0:W], in_=z, func=AF.Square), [rz], [rSQ[s]] + fz)
            if srcs_bf is None:
                op("dve", lambda e, z=z, s=s: e.tensor_copy(out=ZB[s][:, 0:W], in_=z), [rz], [rZB[s]] + fz)
                zb, rzb = ZB[s][:, 0:W], rZB[s]
            else:
                zb, rzb = srcs_bf[m]
            op("pe", lambda e, zb=zb, m=m: e.matmul(PS[6][:, 0:W], lhsT=onesB[:], rhs=zb, start=(m == 0), stop=(m == nch - 1)),
               [rzb, rC], [rPS[6]], signal=(m == nch - 1))
            op("pe", lambda e, s=s, m=m: e.matmul(PS[7][:, 0:W], lhsT=onesB[:], rhs=SQ[s][:, 0:W], start=(m == 0), stop=(m == nch - 1)),
               [rSQ[s], rC], [rPS[7]], signal=True)
        op("dve", lambda e: e.tensor_scalar(out=MEAN[:, 0:W], in0=PS[6][:, 0:W], scalar1=inv, scalar2=None, op0=ALU.mult),
           [rPS[6]], [rSTAT])
        op("dve", lambda e: e.tensor_tensor(out=VAR[:, 0:W], in0=MEAN[:, 0:W], in1=MEAN[:, 0:W], op=ALU.mult), [rSTAT], [rSTAT])
        op("dve", lambda e: e.scalar_tensor_tensor(out=VAR[:, 0:W], in0=PS[7][:, 0:W], scalar=inv, in1=VAR[:, 0:W],
                                                   op0=ALU.mult, op1=ALU.subtract), [rPS[7], rSTAT], [rSTAT])
        op("act", lambda e: e.activation(out=RSTD[:, 0:W], in_=VAR[:, 0:W], func=AF.Sqrt, bias=EPST[:], scale=1.0),
           [rSTAT, rC], [rSTAT])
        op("dve", lambda e: e.reciprocal(out=RSTD[:, 0:W], in_=RSTD[:, 0:W]), [rSTAT], [rSTAT])
        for m in range(nch):
            z, rz = zs[m]
            s = m % 2
            op("dve", lambda e, z=z, s=s: e.tensor_tensor(out=T1[s][:, 0:W], in0=z, in1=MEAN[:, 0:W], op=ALU.subtract),
               [rz, rSTAT], [rT1[s]])
            op(POOLENG, lambda e, s=s: e.tensor_tensor(out=T2[s][:, 0:W], in0=T1[s][:, 0:W], in1=RSTD[:, 0:W], op=ALU.mult),
               [rT1[s], rSTAT], [rT2[s]])
            outs(m, T2[s][:, 0:W], rT2[s])

    def deepnorm_ln(W, lnidx, scaled, make_xb=True):
        gsrc, bsrc = (VLNGA, VLNBA) if scaled else (VLNG, VLNB)

        def outs(m, t2, rt2):
            col = lnidx * 8 + m
            op("act", lambda e: e.activation(out=XF[:, m, 0:W], in_=t2, func=AF.Identity,
                                             bias=bsrc[:, col:col + 1], scale=gsrc[:, col:col + 1]), [rt2, rC], [rXFm[m]])
            if make_xb:
                if POOLENG == "pool":
                    op("pool", lambda e: e.tensor_scalar(out=XB[:, m, 0:W], in0=t2, scalar1=VLNG[:, col:col + 1],
                                                         scalar2=VLNB[:, col:col + 1], op0=ALU.mult, op1=ALU.add), [rt2, rC], [rXB])
                else:
                    op("act", lambda e: e.activation(out=XB[:, m, 0:W], in_=t2, func=AF.Identity,
                                                     bias=VLNB[:, col:col + 1], scale=VLNG[:, col:col + 1]), [rt2, rC], [rXB])
        layer_norm([(XF[:, m, 0:W], rXFm[m]) for m in range(KD)], W, KD, None, outs,
                   fence=(rACC + rACCB + [rPOSBC]) if lnidx == 3 else ())

    def load_tile(tok0, W):
        nb = W // 128
        LT = _os.environ.get("LT", "dpav")
        for b in range(nb):
            s = b % 2
            if "d" in LT:
                dma("sp", XS[s][:], xin[tok0 + b * 128: tok0 + (b + 1) * 128, :], [], [rXS[s]], "xs%d" % s)
            if "p" in LT:
                for m in range(KD):
                    bank = m // 4
                    op("pe", lambda e, m=m, s=s, bank=bank: e.transpose(out=PS[bank][:, (m % 4) * 128:(m % 4 + 1) * 128],
                                                                        in_=XS[s][:, m * 128:(m + 1) * 128], identity=identF[:]),
                       [rXS[s], rC], [rPS[bank]], signal=(m % 4 == 3))
            for h in range(2):
                src = PS[h][:].rearrange("p (a t) -> p a t", a=4)
                if "a" in LT:
                    op("act", lambda e, h=h, b=b, src=src: e.mul(out=XF[:, 4 * h:4 * h + 4, b * 128:(b + 1) * 128], in_=src, mul=ALPHA),
                       [rPS[h]], rXFm[4 * h:4 * h + 4])
                if "v" in LT:
                    op("dve", lambda e, h=h, b=b, src=src: e.tensor_copy(out=XB[:, 4 * h:4 * h + 4, b * 128:(b + 1) * 128], in_=src),
                       [rPS[h]] + ([rXFm[4 * h]] if _os.environ.get("SER") else []), [rXB])

    def store_tile(tok0, W):
        nb = W // 128
        for b in range(nb):
            s = b % 2
            for m in range(KD):
                bank = 2 + m // 4
                op("pe", lambda e, m=m, b=b, bank=bank: e.transpose(out=PS[bank][:, (m % 4) * 128:(m % 4 + 1) * 128],
                                                                    in_=XF[:, m, b * 128:(b + 1) * 128], identity=identF[:]),
                   [rXFm[m], rC], [rPS[bank]], signal=(m % 4 == 3))
            op("act", lambda e, s=s: e.copy(out=XS[s][:, 0:512], in_=PS[2][:]), [rPS[2]], [rXS[s]])
            op("dve", lambda e, s=s: e.tensor_copy(out=XS[s][:, 512:1024], in_=PS[3][:]), [rPS[3]], [rXS[s]])
            dma("sp", y_o[tok0 + b * 128: tok0 + (b + 1) * 128, :], XS[s][:], [rXS[s]], [], "ys%d" % s)

    def gmlp(W, sample, gv_rows):
        nb = W // 128
        wst = WST[1 if sample else 0]
        bro = 2 if sample else 0
        vps = 0
        for pc in range(4):
            wv, rw = wload(w_in_v[:, :, DV + pc * 768: DV + (pc + 1) * 768], (KD, 768))
            for cc in range(6):
                c = pc * 6 + cc
                bank = vps % 2
                vps += 1
                for k in range(KD):
                    op("pe", lambda e, k=k, cc=cc, bank=bank, wv=wv: e.matmul(PS[bank][:, 0:W], lhsT=wv[:, k, cc * 128:(cc + 1) * 128],
                                                                               rhs=XB[:, k, 0:W], start=(k == 0), stop=(k == KD - 1)),
                       [rw, rXB], [rPS[bank]], signal=(k == KD - 1))
                op("act", lambda e, c=c, bank=bank: e.activation(out=VNT[:, c, 0:W], in_=PS[bank][:, 0:W], func=AF.Gelu,
                                                                 bias=VB_IN[:, 24 + c:25 + c], scale=1.0),
                   [rPS[bank], rC], [rVNTc[c]])

        def outs(c, t2, rt2):
            op("act", lambda e: e.activation(out=VNT[:, c, 0:W], in_=t2, func=AF.Identity,
                                             bias=VLNVB[:, c:c + 1], scale=VLNVG[:, c:c + 1]), [rt2, rC], [rVNTc[c]])
        zs = [(VNT[:, c, 0:W], rVNTc[c]) for c in range(CV)]
        layer_norm(zs, W, CV, zs, outs)
        tb = 0
        for b in range(nb):
            for c8 in range(3):
                bank = 2 + tb % 2
                tb += 1
                psb = PS[bank][:].bitcast(BF16)
                for cc in range(8):
                    c = c8 * 8 + cc
                    op("pe", lambda e, c=c, cc=cc, b=b, psb=psb: e.transpose(out=psb[:, cc * 128:(cc + 1) * 128],
                                                                               in_=VNT[:, c, b * 128:(b + 1) * 128], identity=identB[:]),
                       [rVNTc[c], rC], [rPS[bank]], signal=(cc == 7))
                eng = "act" if c8 == 1 else "dve"
                if eng == "act":
                    op("act", lambda e, b=b, c8=c8, psb=psb: e.copy(out=VN[:, b, c8 * 1024:(c8 + 1) * 1024], in_=psb), [rPS[bank]], [rVN])
                else:
                    op("dve", lambda e, b=b, c8=c8, psb=psb: e.tensor_copy(out=VN[:, b, c8 * 1024:(c8 + 1) * 1024], in_=psb), [rPS[bank]], [rVN])
            if b in gv_rows:
                r0 = gv_rows[b]
                dma("pool", gv_o[r0:r0 + 128, :], VN[:, b, :], [rVN], [], "gv")
        ups = 0
        for pc in range(4):
            wu, rw = wload(w_in_v[:, :, pc * 768:(pc + 1) * 768], (KD, 768))
            for cc in range(6):
                c = pc * 6 + cc
                g = c // 3
                bank = ups % 2
                mb = 2 + ups % 2
                s = ups % 2
                ups += 1
                for k in range(KD):
                    op("pe", lambda e, k=k, cc=cc, bank=bank, wu=wu: e.matmul(PS[bank][:, 0:W], lhsT=wu[:, k, cc * 128:(cc + 1) * 128],
                                                                               rhs=XB[:, k, 0:W], start=(k == 0), stop=(k == KD - 1)),
                       [rw, rXB], [rPS[bank]], signal=(k == KD - 1))
                op("act", lambda e, c=c, bank=bank, s=s: e.activation(out=ZB[s][:, 0:W], in_=PS[bank][:, 0:W], func=AF.Gelu,
                                                                      bias=VB_IN[:, c:c + 1], scale=1.0),
                   [rPS[bank], rC], [rZB[s]])
                for b in range(nb):
                    cols = slice(b * 128, (b + 1) * 128)
                    op("pe", lambda e, b=b, c=c, g=g, mb=mb, cols=cols: e.matmul(PS[mb][:, cols], lhsT=VN[:, b, c * 128:(c + 1) * 128],
                                                                                  rhs=wst[:, g, :], start=True, stop=False),
                       [rVN, rC], [rPS[mb]], signal=False)
                    op("pe", lambda e, g=g, mb=mb, cols=cols: e.matmul(PS[mb][:, cols], lhsT=onesB[0:1, :],
                                                                       rhs=BROW[0:1, bro, g * 128:(g + 1) * 128], start=False, stop=False),
                       [rC], [rPS[mb]], signal=False)
                    op("pe", lambda e, g=g, mb=mb, cols=cols: e.matmul(PS[mb][:, cols], lhsT=onesB[0:1, :],
                                                                       rhs=BROW[0:1, bro + 1, g * 128:(g + 1) * 128], start=False, stop=True),
                       [rC], [rPS[mb]], signal=(b == nb - 1))
                op("dve", lambda e, c=c, mb=mb, s=s: e.tensor_tensor(out=OT[:, c, 0:W], in0=ZB[s][:, 0:W], in1=PS[mb][:, 0:W], op=ALU.mult),
                   [rZB[s], rPS[mb]], [rVNTc[c]])
        for pc in range(4):
            wo, rw = wload(w_out_v[:, :, pc * 256:(pc + 1) * 256], (CV, 256))
            for mm in range(2):
                m = pc * 2 + mm
                bank = 4 + m % 2
                for c in range(CV):
                    op("pe", lambda e, c=c, mm=mm, bank=bank, wo=wo: e.matmul(PS[bank][:, 0:W], lhsT=wo[:, c, mm * 128:(mm + 1) * 128],
                                                                               rhs=OT[:, c, 0:W], start=(c == 0), stop=(c == CV - 1)),
                       [rw, rVNTc[c]], [rPS[bank]], signal=(c == CV - 1))
                op("dve", lambda e, m=m, bank=bank: e.scalar_tensor_tensor(out=XF[:, m, 0:W], in0=PS[bank][:, 0:W], scalar=VBOUT[:, m:m + 1],
                                                                           in1=XF[:, m, 0:W], op0=ALU.add, op1=ALU.add),
                   [rPS[bank], rC, rXFm[m]], [rXFm[m]])

    def experts(W, wg_list, gated):
        cnt = 0
        for ei, (gv, uv, dv) in enumerate(wg_list):
            if gated:
                gs = ei % 2
                op("pe", lambda e, ei=ei: e.matmul(PS[6][:, 0:W], lhsT=SEL[:, ei, :], rhs=GTS[0:8, 0:W], start=True, stop=True),
                   [rC, rGTS], [rPS[6]])
                op("act", lambda e, gs=gs: e.copy(out=GBC[gs][:, 0:W], in_=PS[6][:, 0:W]), [rPS[6]], [rGBC[gs]])
            for gi in range(4):
                wgp, rwg = wload(gv[:, :, gi * 896:(gi + 1) * 896], (KD, 896))
                wup, rwu = wload(uv[:, :, gi * 896:(gi + 1) * 896], (KD, 896))
                wdp, rwd = wload(dv[:, gi * 7:(gi + 1) * 7, :], (7, D))
                hs = cnt % 2
                cnt += 1
                for fc in range(7):
                    s = fc % 2
                    gb, ub = fc % 2, 2 + fc % 2
                    for k in range(KD):
                        op("pe", lambda e, k=k, fc=fc, gb=gb, wgp=wgp: e.matmul(PS[gb][:, 0:W], lhsT=wgp[:, k, fc * 128:(fc + 1) * 128],
                                                                                 rhs=XB[:, k, 0:W], start=(k == 0), stop=(k == KD - 1)),
                           [rwg, rXB], [rPS[gb]], signal=(k == KD - 1))
                    for k in range(KD):
                        op("pe", lambda e, k=k, fc=fc, ub=ub, wup=wup: e.matmul(PS[ub][:, 0:W], lhsT=wup[:, k, fc * 128:(fc + 1) * 128],
                                                                                 rhs=XB[:, k, 0:W], start=(k == 0), stop=(k == KD - 1)),
                           [rwu, rXB], [rPS[ub]], signal=(k == KD - 1))
                    op("act", lambda e, gb=gb, s=s: e.activation(out=SS[s][:, 0:W], in_=PS[gb][:, 0:W], func=AF.Silu), [rPS[gb]], [rSS[s]])
                    if gated:
                        op("dve", lambda e, ub=ub, s=s: e.tensor_tensor(out=TT[s][:, 0:W], in0=SS[s][:, 0:W], in1=PS[ub][:, 0:W], op=ALU.mult),
                           [rSS[s], rPS[ub]], [rTT[s]])
                        op("dve", lambda e, s=s, hs=hs, fc=fc, gs=gs: e.tensor_tensor(out=HT[hs][:, fc, 0:W], in0=TT[s][:, 0:W],
                                                                                      in1=GBC[gs][:, 0:W], op=ALU.mult),
                           [rTT[s], rGBC[gs]], [rHT[hs]])
                    else:
                        op("dve", lambda e, ub=ub, s=s, hs=hs, fc=fc: e.tensor_tensor(out=HT[hs][:, fc, 0:W], in0=SS[s][:, 0:W],
                                                                                      in1=PS[ub][:, 0:W], op=ALU.mult),
                           [rSS[s], rPS[ub]], [rHT[hs]])
                for m in range(KD):
                    yb = 4 + m % 2
                    for fc in range(7):
                        op("pe", lambda e, fc=fc, m=m, yb=yb, hs=hs, wdp=wdp: e.matmul(PS[yb][:, 0:W], lhsT=wdp[:, fc, m * 128:(m + 1) * 128],
                                                                                        rhs=HT[hs][:, fc, 0:W], start=(fc == 0), stop=(fc == 6)),
                           [rwd, rHT[hs]], [rPS[yb]], signal=(fc == 6))
                    op("dve", lambda e, m=m, yb=yb: e.tensor_tensor(out=XF[:, m, 0:W], in0=XF[:, m, 0:W], in1=PS[yb][:, 0:W], op=ALU.add),
                       [rPS[yb], rXFm[m]], [rXFm[m]])

    def experts_sparse(wg_list):
        IFE = ["pe", "act", "dve"]
        cnt = 0

        def gather(ei, xg, rxg, c0, width):
            kper = max(1, 512 // width)
            gi_ = 0
            for k0 in range(0, KD, kper):
                bank = 6 + gi_ % 2
                gi_ += 1
                ks = list(range(k0, min(KD, k0 + kper)))
                for kk, k in enumerate(ks):
                    for tb in range(4):
                        op("pe", lambda e, k=k, kk=kk, tb=tb, bank=bank: e.matmul(PS[bank][:, kk * width:(kk + 1) * width],
                                                                                  lhsT=XTOK[:, tb, k * 128:(k + 1) * 128],
                                                                                  rhs=PE_[tb][:, c0:c0 + width], start=(tb == 0), stop=(tb == 3)),
                           [rXTOK, rPE_], [rPS[bank]], signal=(tb == 3 and kk == len(ks) - 1))
                src = PS[bank][:, 0:len(ks) * width].rearrange("p (a t) -> p a t", a=len(ks))
                dst = xg[:, ks[0]:ks[-1] + 1, c0:c0 + width]
                if gi_ % 2 == 0:
                    op("act", lambda e, src=src, dst=dst: e.copy(out=dst, in_=src), [rPS[bank]], [rxg])
                else:
                    op("dve", lambda e, src=src, dst=dst: e.tensor_copy(out=dst, in_=src), [rPS[bank]], [rxg])

        def ffn_block(gi, xg, rxg, c0, width, ht, rht, sbs, wgp, rwg, wup, rwu, wdp, rwd):
            for fc in range(7):
                s_ = fc % 2
                gb, ub = fc % 2, 2 + fc % 2
                for k in range(KD):
                    op("pe", lambda e, k=k, fc=fc, gb=gb: e.matmul(PS[gb][:, 0:width], lhsT=wgp[:, k, fc * 128:(fc + 1) * 128],
                                                                    rhs=xg[:, k, c0:c0 + width], start=(k == 0), stop=(k == KD - 1)),
                       [rwg, rxg], [rPS[gb]], signal=(k == KD - 1))
                for k in range(KD):
                    op("pe", lambda e, k=k, fc=fc, ub=ub: e.matmul(PS[ub][:, 0:width], lhsT=wup[:, k, fc * 128:(fc + 1) * 128],
                                                                    rhs=xg[:, k, c0:c0 + width], start=(k == 0), stop=(k == KD - 1)),
                       [rwu, rxg], [rPS[ub]], signal=(k == KD - 1))
                op("act", lambda e, gb=gb, s_=s_: e.activation(out=SSA[s_][:, 0:width], in_=PS[gb][:, 0:width], func=AF.Silu),
                   [rPS[gb]], [rSSA[s_]])
                op("dve", lambda e, ub=ub, s_=s_, fc=fc: e.tensor_tensor(out=ht[:, fc, 0:width], in0=SSA[s_][:, 0:width], in1=PS[ub][:, 0:width],
                                                                         op=ALU.mult), [rSSA[s_], rPS[ub]], [rht])
            yi = 0
            for (sbk, hc0, mw, r0, nr) in sbs:
                for half in range(2):
                    yb = 4 + yi % 2
                    yi += 1
                    for fc in range(7):
                        op("pe", lambda e, fc=fc, hc0=hc0, mw=mw, half=half, yb=yb: e.matmul(PS[yb][0:mw, 0:512], lhsT=ht[:, fc, hc0:hc0 + mw],
                                                                                            rhs=wdp[:, fc, half * 512:(half + 1) * 512],
                                                                                            start=(fc == 0), stop=(fc == 6)),
                           [rwd, rht], [rPS[yb]], signal=(fc == 6))
                    dst = ACC[sbk][r0:r0 + nr, half * 512:(half + 1) * 512]
                    src = PS[yb][r0:r0 + nr, 0:512]
                    if gi == 0:
                        op("dve", lambda e, dst=dst, src=src: e.tensor_copy(out=dst, in_=src), [rPS[yb]], [rACC[sbk]])
                    else:
                        op("dve", lambda e, dst=dst, src=src: e.tensor_tensor(out=dst, in0=dst, in1=src, op=ALU.add),
                           [rPS[yb], rACC[sbk]], [rACC[sbk]])

        def scatter(sbs):
            for (sbk, hc0, mw, r0, nr) in sbs:
                op("act", lambda e, sbk=sbk, r0=r0, nr=nr: e.copy(out=ACCB[sbk][r0:r0 + nr, :], in_=ACC[sbk][r0:r0 + nr, :]),
                   [rACC[sbk]], [rACCB[sbk]])
            for m in range(KD):
                bank = 6 + m % 2
                for j, (sbk, hc0, mw, r0, nr) in enumerate(sbs):
                    op("pe", lambda e, m=m, j=j, sbk=sbk, r0=r0, nr=nr, bank=bank, n=len(sbs): e.matmul(
                        PS[bank][:, 0:512], lhsT=ACCB[sbk][r0:r0 + nr, m * 128:(m + 1) * 128], rhs=Q_[sbk][r0:r0 + nr, :],
                        start=(j == 0), stop=(j == n - 1)),
                       [rACCB[sbk], rQ[sbk]], [rPS[bank]], signal=(j == len(sbs) - 1))
                op("dve", lambda e, m=m, bank=bank: e.tensor_tensor(out=XF[:, m, 0:512], in0=XF[:, m, 0:512], in1=PS[bank][:, 0:512], op=ALU.add),
                   [rPS[bank], rXFm[m]], [rXFm[m]])

        def build_q(sbs):
            for sbk in sbs:
                op("dve", lambda e, sbk=sbk: e.scalar_tensor_tensor(out=Q_[sbk][:], in0=POSBC[:], scalar=SLOTID[:, sbk:sbk + 1], in1=GBC1[:],
                                                                    op0=ALU.is_equal, op1=ALU.mult), [rPOSBC, rGBC1, rC], [rQ[sbk]])

        for ei, (gv, uv, dv) in enumerate(wg_list):
            xg, rxg = XG[ei % 2], rXG[ei % 2]
            for eng in IFE:
                op(eng, lambda e, eng=eng, ei=ei: e.reg_load(B.regs[eng], CNT[0:1, ei:ei + 1]), [rCNT], [], signal=False)
            lnf = (rT1 + rT2 + rSQ + rZB + [rSTAT]) if ei == 0 else []
            for tb in range(4):
                op("dve", lambda e, tb=tb, ei=ei: e.tensor_scalar(out=PE_[tb][:], in0=IOTA_F[:], scalar1=POS[:, tb, ei:ei + 1],
                                                                  scalar2=MSKF[:, tb, ei:ei + 1], op0=ALU.is_equal, op1=ALU.mult),
                   [rPOS, rMSK, rC], [rPE_] + (lnf if tb == 0 else []))
            op("pe", lambda e, ei=ei: e.matmul(PS[6][:, 0:512], lhsT=SEL[:, ei, :], rhs=GTS[0:8, 0:512], start=True, stop=True),
               [rC, rGTS], [rPS[6]])
            op("pe", lambda e, ei=ei: e.matmul(PS[7][:, 0:512], lhsT=SEL[:, ei, :], rhs=POST[0:8, 0:512], start=True, stop=True),
               [rC, rPOST], [rPS[7]])
            op("act", lambda e: e.copy(out=GBC1[:], in_=PS[6][:, 0:512]), [rPS[6]], [rGBC1] + lnf)
            op("act", lambda e: e.copy(out=POSBC[:], in_=PS[7][:, 0:512]), [rPS[7]], [rPOSBC])
            build_q([0, 1, 2, 3])
            gather(ei, xg, rxg, 0, WA)
            B.if_begin(IFE, ei, THR)
            gather(ei, xg, rxg, WA, WT - WA)
            B.if_end()
            for gi in range(4):
                wgp, rwg = wload(gv[:, :, gi * 896:(gi + 1) * 896], (KD, 896))
                wup, rwu = wload(uv[:, :, gi * 896:(gi + 1) * 896], (KD, 896))
                wdp, rwd = wload(dv[:, gi * 7:(gi + 1) * 7, :], (7, D))
                hs = cnt % 2
                cnt += 1
                ffn_block(gi, xg, rxg, 0, WA, HTA[hs], rHTA[hs], A_BLK, wgp, rwg, wup, rwu, wdp, rwd)
                B.if_begin(IFE, ei, THR)
                ffn_block(gi, xg, rxg, B_C0, WB, HTBv, rHTB, B_BLK, wgp, rwg, wup, rwu, wdp, rwd)
                if gi == 3:
                    scatter(B_BLK)
                B.if_end()
            scatter(A_BLK)

    def conv_module(W, nseq, L, sample, last_prompt):
        PADL = HW_ + L
        gtb = GTB[:, 0:KD * nseq * PADL].rearrange("p (m s t) -> p m s t", m=KD, s=nseq)
        if sample:
            for s in range(nseq):
                xs = s % 2
                dma("sp", XS[xs][0:HW_, :], cache[s], [], [rXS[xs]], "xs%d" % xs)
                bank = s % 2
                for m in range(KD):
                    op("pe", lambda e, m=m, xs=xs, bank=bank: e.transpose(out=PS[bank][:, m * HW_:(m + 1) * HW_],
                                                                          in_=XS[xs][0:HW_, m * 128:(m + 1) * 128], identity=identF[0:HW_, 0:HW_]),
                       [rXS[xs], rC], [rPS[bank]], signal=(m == KD - 1))
                op("dve", lambda e, s=s, bank=bank: e.tensor_copy(out=gtb[:, :, s, 0:HW_],
                                                                  in_=PS[bank][:, 0:KD * HW_].rearrange("p (m t) -> p m t", m=KD)),
                   [rPS[bank]], [rGTB])
        else:
            op("dve", lambda e: e.tensor_copy(out=gtb[:, :, 0, 0:HW_], in_=HIST[:]), [rHIST], [rGTB])
        pieces = {}
        for half in range(2):
            wa, rwa = wload(w_pw1_v[:, :, half * 512:(half + 1) * 512], (KD, 512))
            wgt, rwgt = wload(w_pw1_v[:, :, D + half * 512: D + (half + 1) * 512], (KD, 512))
            for mm in range(4):
                m = half * 4 + mm
                ab, gb = m % 2, 2 + m % 2
                s = m % 2
                for k in range(KD):
                    op("pe", lambda e, k=k, mm=mm, ab=ab, wa=wa: e.matmul(PS[ab][:, 0:W], lhsT=wa[:, k, mm * 128:(mm + 1) * 128],
                                                                           rhs=XB[:, k, 0:W], start=(k == 0), stop=(k == KD - 1)),
                       [rwa, rXB], [rPS[ab]], signal=(k == KD - 1))
                for k in range(KD):
                    op("pe", lambda e, k=k, mm=mm, gb=gb, wgt=wgt: e.matmul(PS[gb][:, 0:W], lhsT=wgt[:, k, mm * 128:(mm + 1) * 128],
                                                                             rhs=XB[:, k, 0:W], start=(k == 0), stop=(k == KD - 1)),
                       [rwgt, rXB], [rPS[gb]], signal=(k == KD - 1))
                op("act", lambda e, m=m, gb=gb, s=s: e.activation(out=T1[s][:, 0:W], in_=PS[gb][:, 0:W], func=AF.Sigmoid,
                                                                  bias=VBPW1[:, 8 + m:9 + m], scale=1.0), [rPS[gb], rC], [rT1[s]])
                a3 = PS[ab][:, 0:W].rearrange("p (s t) -> p s t", s=nseq)
                g3 = T1[s][:, 0:W].rearrange("p (s t) -> p s t", s=nseq)
                op("dve", lambda e, m=m, a3=a3, g3=g3: e.scalar_tensor_tensor(out=gtb[:, m, :, HW_:PADL], in0=a3, scalar=VBPW1[:, m:m + 1],
                                                                              in1=g3, op0=ALU.add, op1=ALU.mult),
                   [rPS[ab], rT1[s], rC], [rGTB])
                if sample or last_prompt:
                    op("dve", lambda e, m=m, a3=a3, g3=g3: e.scalar_tensor_tensor(out=GL[:, m, 0:nseq, :], in0=a3[:, :, L - HW_:L],
                                                                                  scalar=VBPW1[:, m:m + 1], in1=g3[:, :, L - HW_:L],
                                                                                  op0=ALU.add, op1=ALU.mult),
                       [rPS[ab], rT1[s], rC], [rGL])
        if not sample:
            op("dve", lambda e: e.tensor_copy(out=HIST[:], in_=gtb[:, :, 0, L:PADL]), [rGTB], [rHIST])
        if sample or last_prompt:
            for s in range(nseq):
                xs = s % 2
                for m in range(KD):
                    bank = 6 + m // 4
                    op("pe", lambda e, m=m, s=s, bank=bank: e.transpose(out=PS[bank][0:HW_, (m % 4) * 128:(m % 4 + 1) * 128],
                                                                        in_=GL[:, m, s, :], identity=identF[:]),
                       [rGL, rC], [rPS[bank]], signal=(m % 4 == 3))
                op("act", lambda e, xs=xs: e.copy(out=XS[xs][0:HW_, 0:512], in_=PS[6][0:HW_, :]), [rPS[6]], [rXS[xs]])
                op("dve", lambda e, xs=xs: e.tensor_copy(out=XS[xs][0:HW_, 512:1024], in_=PS[7][0:HW_, :]), [rPS[7]], [rXS[xs]])
                r0 = (HW_ * (1 + s)) if sample else 0
                dma("sp", cs_o[r0:r0 + HW_, :], XS[xs][0:HW_, :], [rXS[xs]], [], "ys%d" % xs)
        for m in range(KD):
            ds = m % 2
            for k in range(NTAP):
                op(POOLENG, lambda e, m=m, k=k, ds=ds: e.tensor_scalar(out=DG[ds][:, k, :], in0=identB[:], scalar1=WDW[:, m, k:k + 1],
                                                                       scalar2=None, op0=ALU.mult), [rC], [rDG[ds]])
            bank = 4 + m % 2
            for s in range(nseq):
                for k in range(NTAP):
                    op("pe", lambda e, m=m, k=k, s=s, ds=ds, bank=bank: e.matmul(PS[bank][:, s * L:(s + 1) * L], lhsT=DG[ds][:, k, :],
                                                                                  rhs=gtb[:, m, s, k:k + L], start=(k == 0), stop=(k == NTAP - 1)),
                       [rDG[ds], rGTB], [rPS[bank]], signal=(k == NTAP - 1 and s == nseq - 1))
            op("act", lambda e, m=m, bank=bank: e.activation(out=YC[:, m, 0:W], in_=PS[bank][:, 0:W], func=AF.Identity,
                                                             bias=VBDW[:, m:m + 1], scale=1.0), [rPS[bank], rC], [rYC])

        def outs(m, t2, rt2):
            op("act", lambda e: e.activation(out=ST[:, m, 0:W], in_=t2, func=AF.Silu, bias=VCLB[:, m:m + 1], scale=VCLG[:, m:m + 1]),
               [rt2, rC], [rST])
        layer_norm([(YC[:, m, 0:W], rYC) for m in range(KD)], W, KD, None, outs)
        for half in range(2):
            wp, rwp = wload(w_pw2_v[:, :, half * 512:(half + 1) * 512], (KD, 512))
            for mm in range(4):
                m = half * 4 + mm
                bank = 4 + m % 2
                for k in range(KD):
                    op("pe", lambda e, k=k, mm=mm, bank=bank, wp=wp: e.matmul(PS[bank][:, 0:W], lhsT=wp[:, k, mm * 128:(mm + 1) * 128],
                                                                               rhs=ST[:, k, 0:W], start=(k == 0), stop=(k == KD - 1)),
                       [rwp, rST], [rPS[bank]], signal=(k == KD - 1))
                op("dve", lambda e, m=m, bank=bank: e.scalar_tensor_tensor(out=XF[:, m, 0:W], in0=PS[bank][:, 0:W], scalar=VBPW2[:, m:m + 1],
                                                                           in1=XF[:, m, 0:W], op0=ALU.add, op1=ALU.add),
                   [rPS[bank], rC, rXFm[m]], [rXFm[m]])

    def router(W, sparse=False):
        nb = W // 128
        L_, M1, L2, M2, G = RT["L"], RT["M1"], RT["L2"], RT["M2"], RT["G"]
        for b in range(nb):
            for k in range(KD):
                op("pe", lambda e, k=k, b=b: e.matmul(PS[7][:, 0:NE], lhsT=XF[:, k, b * 128:(b + 1) * 128], rhs=WR[:, k, :],
                                                       start=(k == 0), stop=(k == KD - 1)),
                   [rXFm[k], rC], [rPS[7]], signal=(k == KD - 1))
            ops = [
                lambda e: e.tensor_tensor(out=L_[:], in0=PS[7][:, 0:NE], in1=BR[:], op=ALU.add),
                lambda e: e.tensor_reduce(out=RS["m1"][:], in_=L_[:], axis=AX.X, op=ALU.max),
                lambda e: e.tensor_scalar(out=M1[:], in0=L_[:], scalar1=RS["m1"][:], scalar2=None, op0=ALU.is_equal),
                lambda e: e.scalar_tensor_tensor(out=L2[:], in0=M1[:], scalar=-1e30, in1=L_[:], op0=ALU.mult, op1=ALU.add),
                lambda e: e.tensor_reduce(out=RS["m2"][:], in_=L2[:], axis=AX.X, op=ALU.max),
                lambda e: e.tensor_scalar(out=M2[:], in0=L2[:], scalar1=RS["m2"][:], scalar2=None, op0=ALU.is_equal),
                lambda e: e.tensor_tensor(out=RS["d"][:], in0=RS["m2"][:], in1=RS["m1"][:], op=ALU.subtract),
            ]
            for f in ops:
                op("dve", f, [rPS[7], rC, rRT], [rRT])
            op("act", lambda e: e.activation(out=RS["e"][:], in_=RS["d"][:], func=AF.Exp), [rRT], [rRT])
            ops = [
                lambda e: e.tensor_scalar(out=RS["den"][:], in0=RS["e"][:], scalar1=1.0, scalar2=None, op0=ALU.add),
                lambda e: e.reciprocal(out=RS["w1"][:], in_=RS["den"][:]),
                lambda e: e.tensor_tensor(out=RS["w2"][:], in0=RS["e"][:], in1=RS["w1"][:], op=ALU.mult),
                lambda e: e.tensor_scalar(out=G[:], in0=M1[:], scalar1=RS["w1"][:], scalar2=None, op0=ALU.mult),
                lambda e: e.scalar_tensor_tensor(out=G[:], in0=M2[:], scalar=RS["w2"][:], in1=G[:], op0=ALU.mult, op1=ALU.add),
            ]
            for f in ops:
                op("dve", f, [rRT], [rRT])
            op("pe", lambda e: e.transpose(out=PS[6][0:NE, 0:128], in_=G[:], identity=identF[:]), [rRT, rC], [rPS[6]])
            op("dve", lambda e, b=b: e.tensor_copy(out=GTS[0:8, b * 128:(b + 1) * 128], in_=PS[6][0:NE, 0:128]), [rPS[6]], [rGTS])
            if sparse:
                op("dve", lambda e, b=b: e.tensor_tensor(out=MSKF[:, b, :], in0=M1[:], in1=M2[:], op=ALU.add), [rRT], [rMSK])
                op("dve", lambda e, b=b: e.tensor_copy(out=MSK[:, b, :], in_=MSKF[:, b, :]), [rMSK], [rMSK])
        if sparse:
            for b in range(nb):
                terms = [(onesB, bb) for bb in range(b)] + [(UTRI, b)]
                for i, (lt, bb) in enumerate(terms):
                    op("pe", lambda e, lt=lt, bb=bb, b=b, i=i, n=len(terms): e.matmul(PS[6][:, b * NE:(b + 1) * NE], lhsT=lt[:], rhs=MSK[:, bb, :],
                                                                                      start=(i == 0), stop=(i == n - 1)),
                       [rMSK, rC], [rPS[6]], signal=False)
            for b in range(nb):
                op("pe", lambda e, b=b: e.matmul(PS[6][:, 4 * NE:5 * NE], lhsT=onesB[:], rhs=MSK[:, b, :], start=(b == 0), stop=(b == nb - 1)),
                   [rMSK, rC], [rPS[6]], signal=(b == nb - 1))
            op("dve", lambda e: e.tensor_copy(out=POS[:], in_=PS[6][:, 0:4 * NE].rearrange("p (b e) -> p b e", b=4)), [rPS[6]], [rPOS])
            op("dve", lambda e: e.tensor_copy(out=CNT[:], in_=PS[6][0:1, 4 * NE:5 * NE]), [rPS[6]], [rCNT])
            for b in range(nb):
                op("pe", lambda e, b=b: e.transpose(out=PS[7][0:NE, b * 128:(b + 1) * 128], in_=POS[:, b, :], identity=identF[:]),
                   [rPOS, rC], [rPS[7]], signal=(b == nb - 1))
            op("dve", lambda e: e.tensor_copy(out=POST[:], in_=PS[7][0:NE, :]), [rPS[7]], [rPOST])
            for b in range(nb):
                bank = 4 + b % 2
                psb = PS[bank][:].bitcast(BF16)
                for k in range(KD):
                    op("pe", lambda e, k=k, b=b, psb=psb: e.transpose(out=psb[:, k * 128:(k + 1) * 128], in_=XB[:, k, b * 128:(b + 1) * 128],
                                                                       identity=identB[:]),
                       [rXB, rC], [rPS[bank]], signal=(k == KD - 1))
                if b % 2 == 0:
                    op("act", lambda e, b=b, psb=psb: e.copy(out=XTOK[:, b, :], in_=psb), [rPS[bank]], [rXTOK])
                else:
                    op("dve", lambda e, b=b, psb=psb: e.tensor_copy(out=XTOK[:, b, :], in_=psb), [rPS[bank]], [rXTOK])
        for m in range(KD):
            op("act", lambda e, m=m: e.mul(out=XF[:, m, 0:W], in_=XF[:, m, 0:W], mul=ALPHA), [rXFm[m]], [rXFm[m]])

    ff_views = [(ffg.rearrange("(k p) n -> p k n", p=128), ffu.rearrange("(k p) n -> p k n", p=128),
                 ffd.rearrange("(c p) n -> p c n", p=128))]
    moe_views = [(mog[e_].rearrange("(k p) n -> p k n", p=128), mou[e_].rearrange("(k p) n -> p k n", p=128),
                  mod[e_].rearrange("(c p) n -> p c n", p=128)) for e_ in range(NE)]

    tiles = [(i * WT, WT, 1, WT, False, i == NPT - 1) for i in range(NPT)]
    tiles.append((SEQ, NSAMP * LS, NSAMP, LS, True, False))
    stop = dbg.get("stop")
    if "tiles" in dbg:
        tiles = [tiles[i] for i in dbg["tiles"]]
    for ti_, (tok0, W, nseq, L, sample, last_prompt) in enumerate(tiles):
        ring_state["piece"] = 0
        ring_state["first"] = (ti_ == 0)
        if not dbg.get("noload"):
            load_tile(tok0, W)
        if sample:
            gv_rows = {0: 128, 1: 256}
        elif last_prompt:
            gv_rows = {3: 0}
        else:
            gv_rows = {}
        phases = [
            ("load", lambda: None),
            ("gmlp", lambda: gmlp(W, sample, gv_rows)),
            ("ln0", lambda: deepnorm_ln(W, 0, scaled=True)),
            ("ffn", lambda: experts(W, ff_views, gated=False)),
            ("ln1", lambda: deepnorm_ln(W, 1, scaled=True)),
            ("conv", lambda: conv_module(W, nseq, L, sample, last_prompt)),
            ("ln2", lambda: deepnorm_ln(W, 2, scaled=False)),
            ("router", lambda: router(W, sparse=(not sample) and not dbg.get("dense"))),
            ("moe", lambda: (experts(W, moe_views[:dbg.get("nexp", NE)], gated=True) if (sample or dbg.get("dense"))
                             else experts_sparse(moe_views[:dbg.get("nexp", NE)]))),
            ("ln3", lambda: deepnorm_ln(W, 3, scaled=False, make_xb=False)),
        ]
        for name, fn in phases:
            fn()
            if stop == name:
                break
        if not dbg.get("nostore"):
            store_tile(tok0, W)
    B.final_wait("sp")
    B.final_wait("pool")
    B.emit()


_NC_CACHE = {}


def kernel(x_prompt, x_sample, cache_conv, gm_w_in, gm_b_in, gm_lnv_g, gm_lnv_b, gm_w_s, gm_b_s, gm_w_out, gm_b_out,
           cv_w_pw1, cv_b_pw1, cv_w_dw, cv_b_dw, cv_ln_g, cv_ln_b, cv_w_pw2, cv_b_pw2,
           ff_w_gate, ff_w_up, ff_w_down, moe_w_router, moe_b_router, moe_w_gate, moe_w_up, moe_w_down, ln_g, ln_b):
    f = lambda a: np.ascontiguousarray(np.asarray(a, dtype=np.float32))
    shared = {
        "gm_w_in": f(gm_w_in[0]), "gm_b_in": f(gm_b_in).reshape(1, -1), "gm_lnv_g": f(gm_lnv_g).reshape(1, -1),
        "gm_lnv_b": f(gm_lnv_b).reshape(1, -1), "gm_w_s": f(gm_w_s[0]), "gm_b_s": f(gm_b_s[0]),
        "gm_w_out": f(gm_w_out[0]), "gm_b_out": f(gm_b_out).reshape(1, -1),
        "cv_w_pw1": f(cv_w_pw1[0]), "cv_b_pw1": f(cv_b_pw1).reshape(1, -1), "cv_w_dw": f(cv_w_dw[0]),
        "cv_b_dw": f(cv_b_dw).reshape(1, -1), "cv_ln_g": f(cv_ln_g).reshape(1, -1), "cv_ln_b": f(cv_ln_b).reshape(1, -1),
        "cv_w_pw2": f(cv_w_pw2[0]), "cv_b_pw2": f(cv_b_pw2).reshape(1, -1),
        "ff_w_gate": f(ff_w_gate[0]), "ff_w_up": f(ff_w_up[0]), "ff_w_down": f(ff_w_down[0]),
        "moe_w_router": f(moe_w_router[0]), "moe_b_router": f(moe_b_router).reshape(1, -1),
        "moe_w_gate": f(moe_w_gate[0]), "moe_w_up": f(moe_w_up[0]), "moe_w_down": f(moe_w_down[0]),
        "ln_g": f(ln_g).reshape(1, -1), "ln_b": f(ln_b).reshape(1, -1),
    }
    xp = f(x_prompt)
    xs = f(x_sample)
    cc = f(cache_conv)
    in_maps = []
    for c in range(NCORE):
        xin = np.concatenate([xp[c], xs[c * NSAMP:(c + 1) * NSAMP].reshape(NSAMP * LS, D)], axis=0)
        m = dict(shared)
        m["xin"] = np.ascontiguousarray(xin)
        m["cache"] = np.ascontiguousarray(cc[0, c * NSAMP:(c + 1) * NSAMP])
        in_maps.append(m)
    if "nc" not in _NC_CACHE:
        _NC_CACHE["nc"] = build_program()
    res = run_bass_kernel_spmd(_NC_CACHE["nc"], in_maps, core_ids=list(range(NCORE)))
    y_p = np.empty((8, SEQ, D), np.float32)
    y_s = np.empty((32, LS, D), np.float32)
    gv_p = np.empty((1, 8, 128, DV), np.float32)
    gv_s = np.empty((1, 32, LS, DV), np.float32)
    cs_p = np.empty((1, 8, HW_, D), np.float32)
    cs_s = np.empty((1, 32, HW_, D), np.float32)
    for c in range(NCORE):
        r = res.results[c]
        y_p[c] = r["y_o"][0:SEQ]
        y_s[c * NSAMP:(c + 1) * NSAMP] = r["y_o"][SEQ:].reshape(NSAMP, LS, D)
        gv_p[0, c] = r["gv_o"][0:128]
        gv_s[0, c * NSAMP:(c + 1) * NSAMP] = r["gv_o"][128:].reshape(NSAMP, LS, DV)
        cs_p[0, c] = r["cs_o"][0:HW_]
        cs_s[0, c * NSAMP:(c + 1) * NSAMP] = r["cs_o"][HW_:].reshape(NSAMP, HW_, D)
    return y_p, y_s, gv_p, gv_s, cs_p, cs_s
```

```python
import numpy as np
from contextlib import ExitStack
import concourse.bass as bass
import concourse.mybir as mybir
from concourse.bass_utils import run_bass_kernel_spmd

F32 = mybir.dt.float32
BF16 = mybir.dt.bfloat16
AF = mybir.ActivationFunctionType
ALU = mybir.AluOpType
AX = mybir.AxisListType

D = 1024
KD = 8
DV = 3072
CV = 24
FF = 3584
NE = 8
SEQ = 4096
NCORE = 8
NSAMP = 4
LS = 64
HW_ = 30
NTAP = 31
ALPHA = float((2.0 * 2) ** 0.25)
EPS = 1e-5
TOK = SEQ + NSAMP * LS
WT = 512
NPT = SEQ // WT
SLOT = 7168
NSLOT = 5
import os as _os
NOSELF = bool(_os.environ.get("NOSELF"))
POOLENG = "pool" if _os.environ.get("USEPOOL") else "dve"


class Res:
    __slots__ = ("w", "r", "const", "excl")

    def __init__(self, const=False, excl=False):
        self.w = None
        self.r = {}
        self.const = const
        self.excl = excl


class Builder:
    def __init__(self, nc, es):
        self.nc = nc
        self.es = es
        self.engs = {"pe": nc.tensor, "act": nc.scalar, "dve": nc.vector, "pool": nc.gpsimd, "sp": nc.sync}
        self.prog = {k: [] for k in self.engs}
        self.cnt = {k: 0 for k in self.engs}
        self.waited = {k: {} for k in self.engs}
        self.S = {k: es.enter_context(nc.semaphore("S_" + k)) for k in ("pe", "act", "dve", "pool")}
        self.dsem = {}
        self.regs = {"pe": es.enter_context(nc.tensor.register("cnt_pe")),
                     "act": es.enter_context(nc.scalar.register("cnt_act")),
                     "dve": es.enter_context(nc.vector.register("cnt_dve")),
                     "sp": es.enter_context(nc.sync.register("cnt_sp"))}
        self.semobj = {}
        for k, s in self.S.items():
            self.semobj[id(s)] = s

    def sb(self, name, shape, dt):
        return self.es.enter_context(self.nc.sbuf_tensor(name, shape, dt))

    def dma_sem(self, name):
        if name not in self.dsem:
            s = self.es.enter_context(self.nc.semaphore("D_" + name))
            self.dsem[name] = [s, 0]
            self.semobj[id(s)] = s
        return self.dsem[name]

    def _collect(self, eng, reads, writes):
        waits = {}
        own = id(self.S[eng]) if eng in self.S else None

        def need(ev):
            if ev is None:
                return
            sid, val = ev
            if sid == own and (eng == "pe" or NOSELF):
                return
            if self.waited[eng].get(sid, 0) >= val:
                return
            if waits.get(sid, 0) < val:
                waits[sid] = val

        for r in reads:
            need(r.w)
        for w in writes:
            need(w.w)
            for sid, val in w.r.items():
                need((sid, val))
        for sid, val in waits.items():
            self.waited[eng][sid] = val
        return [(self.semobj[sid], val) for sid, val in waits.items()]

    def _commit(self, ev, reads, writes):
        for r in reads:
            if not r.const:
                if r.r.get(ev[0], 0) < ev[1]:
                    r.r[ev[0]] = ev[1]
        for w in writes:
            w.w = ev
            w.r = {}

    def op(self, eng, fn, reads=(), writes=(), signal=True):
        if eng != "pe":
            ex = [r for r in reads if r.excl]
            if ex:
                reads = [r for r in reads if not r.excl]
                writes = list(writes) + ex
        waits = self._collect(eng, reads, writes)
        if signal:
            self.cnt[eng] += 1
            ev = (id(self.S[eng]), self.cnt[eng])
        else:
            ev = (id(self.S[eng]), self.cnt[eng] + 1)
        self._commit(ev, reads, writes)
        self.prog[eng].append((waits, fn, (self.S[eng], 1) if signal else None))

    def dma(self, q, out, in_, reads, writes, semname, **kw):
        waits = self._collect(q, reads, writes)
        ds = self.dma_sem(semname)
        ds[1] += 16
        ev = (id(ds[0]), ds[1])
        self._commit(ev, reads, writes)
        self.prog[q].append((waits, lambda e: e.dma_start(out=out, in_=in_, **kw), (ds[0], 16)))

    def raw(self, eng, fn, reads=()):
        waits = self._collect(eng, reads, [])
        self.prog[eng].append((waits, fn, None))

    def if_begin(self, engs, ei, thresh):
        self._if = {}
        self._if_ds = {name: v[1] for name, v in self.dsem.items()}
        for eng in engs:
            entry = ["IF", ei, thresh, 0, []]
            self.prog[eng].append(entry)
            self._if[eng] = (entry, self.cnt[eng], dict(self.waited[eng]))

    def if_end(self):
        for eng, (entry, c0, w0) in self._if.items():
            entry[3] = self.cnt[eng] - c0
            if eng == "sp":
                for name, (sem, tot) in self.dsem.items():
                    pre = self._if_ds.get(name, 0)
                    if tot > pre:
                        entry[4].append((sem, pre, tot - pre))
            self.prog[eng].append(["ENDIF"])
            self.waited[eng] = w0
        self._if = {}

    def barrier(self, engs):
        for eng in engs:
            waits = []
            for k, sem in self.S.items():
                if self.cnt[k] > 0 and not (eng == "pe" and k == "pe"):
                    if self.waited[eng].get(id(sem), 0) < self.cnt[k]:
                        self.waited[eng][id(sem)] = self.cnt[k]
                        waits.append((sem, self.cnt[k]))
            for name, (sem, tot) in self.dsem.items():
                if tot > 0 and self.waited[eng].get(id(sem), 0) < tot:
                    self.waited[eng][id(sem)] = tot
                    waits.append((sem, tot))
            self.prog[eng].append((waits, None, None))

    def final_wait(self, q):
        waits = []
        for name, (s, tot) in self.dsem.items():
            if tot > 0:
                waits.append((s, tot))
        self.prog[q].append((waits, None, None))

    def emit(self):
        nc = self.nc
        block = self.es.enter_context(nc.Block())

        def replay(key, e):
            prog = self.prog[key]
            i = 0
            n = len(prog)

            def run(entry):
                waits, fn, inc = entry
                for s_, v in waits:
                    e.wait_ge(s_, v)
                if fn is None:
                    return
                ins = fn(e)
                if inc is not None:
                    ins.then_inc(inc[0], inc[1])

            while i < n:
                entry = prog[i]
                if entry[0] == "IF":
                    _, ei, thresh, k, dcomp = entry
                    j = i + 1
                    while prog[j][0] != "ENDIF":
                        j += 1
                    reg = self.regs[key]
                    with e.If_lt(reg, thresh + 1):
                        if k > 0:
                            e.drain().then_inc(self.S[key], k)
                        for sem_, pre_, delta_ in dcomp:
                            e.wait_ge(sem_, pre_)
                            e.sem_inc(sem_, delta_)
                    with e.Else():
                        for ent in prog[i + 1:j]:
                            run(ent)
                    i = j + 1
                    continue
                run(entry)
                i += 1

        @block.tensor
        def _(e):
            replay("pe", e)

        @block.scalar
        def _(e):
            replay("act", e)

        @block.vector
        def _(e):
            replay("dve", e)

        @block.gpsimd
        def _(e):
            replay("pool", e)

        @block.sync
        def _(e):
            replay("sp", e)


def build_program(dbg=None):
    nc = bass.Bass("TRN2", target_bir_lowering=False)
    es = ExitStack()
    with es:
        _build(nc, es, dbg or {})
    return nc


def _build(nc, es, dbg):
    B = Builder(nc, es)
    op, dma, sb = B.op, B.dma, B.sb

    def din(name, shape):
        return nc.dram_tensor(name, shape, F32, kind="ExternalInput").ap()

    def dout(name, shape):
        return nc.dram_tensor(name, shape, F32, kind="ExternalOutput").ap()

    xin = din("xin", [TOK, D])
    cache = din("cache", [NSAMP, HW_, D])
    w_in = din("gm_w_in", [D, 2 * DV])
    b_in = din("gm_b_in", [1, 2 * DV])
    lnv_g = din("gm_lnv_g", [1, DV])
    lnv_b = din("gm_lnv_b", [1, DV])
    w_s = din("gm_w_s", [8, 128, 128])
    b_s = din("gm_b_s", [8, 128])
    w_out = din("gm_w_out", [DV, D])
    b_out = din("gm_b_out", [1, D])
    w_pw1 = din("cv_w_pw1", [D, 2 * D])
    b_pw1 = din("cv_b_pw1", [1, 2 * D])
    w_dw = din("cv_w_dw", [NTAP, D])
    b_dw = din("cv_b_dw", [1, D])
    cln_g = din("cv_ln_g", [1, D])
    cln_b = din("cv_ln_b", [1, D])
    w_pw2 = din("cv_w_pw2", [D, D])
    b_pw2 = din("cv_b_pw2", [1, D])
    ffg = din("ff_w_gate", [D, FF])
    ffu = din("ff_w_up", [D, FF])
    ffd = din("ff_w_down", [FF, D])
    w_r = din("moe_w_router", [D, NE])
    b_r = din("moe_b_router", [1, NE])
    mog = din("moe_w_gate", [NE, D, FF])
    mou = din("moe_w_up", [NE, D, FF])
    mod = din("moe_w_down", [NE, FF, D])
    ln_g = din("ln_g", [1, 4 * D])
    ln_b = din("ln_b", [1, 4 * D])
    y_o = dout("y_o", [TOK, D])
    gv_o = dout("gv_o", [128 + NSAMP * LS, DV])
    cs_o = dout("cs_o", [HW_ * (1 + NSAMP), D])

    XF = sb("XF", [128, KD, WT], F32)
    XB = sb("XB", [128, KD, WT], BF16)
    RING = [sb("ring%d" % i, [128, SLOT], BF16) for i in range(NSLOT)]
    PH = sb("PH", [128, 24576], BF16)
    XS0 = sb("XS0", [128, D], F32)
    XS = [XS0, XS0]
    BROWF = PH[0:1, 0:4096].bitcast(F32).rearrange("o (a n) -> o a n", a=2)
    BROWT = PH[0:1, 4096:8192].bitcast(F32).rearrange("o (a n) -> o a n", a=2)
    WSA = PH[:, 8192:10240].bitcast(F32).rearrange("p (g j) -> p g j", g=8)
    IOTA_I = PH[:, 10240:11264].bitcast(mybir.dt.int32)
    UTRI_F = PH[:, 11264:11520].bitcast(F32)
    LNS = sb("LNS", [128, 13312], BF16)

    def lnv(off, n, f32=True):
        v = LNS[:, off:off + n]
        return v.bitcast(F32) if f32 else v
    MEAN, VAR, RSTD = lnv(0, 1024), lnv(1024, 1024), lnv(2048, 1024)
    T1 = [lnv(3072 + i * 1024, 1024) for i in range(2)]
    T2 = [lnv(5120 + i * 1024, 1024) for i in range(2)]
    SQ = [lnv(7168 + i * 512, 512, False) for i in range(2)]
    ZB = [lnv(8192 + i * 512, 512, False) for i in range(2)]
    identF = sb("identF", [128, 128], F32)
    identB = sb("identB", [128, 128], BF16)
    onesB = sb("onesB", [128, 128], BF16)
    WST = [sb("WST%d" % i, [128, 8, 128], BF16) for i in range(2)]
    BROW = sb("BROW", [1, 4, 1024], BF16)
    SEL = sb("SEL", [8, NE, 128], F32)
    WR = sb("WR", [128, KD, NE], F32)
    BR = sb("BR", [128, NE], F32)
    GTS = sb("GTS", [8, WT], F32)
    VB_IN = sb("VB_IN", [128, 48], F32)
    VLNVG = sb("VLNVG", [128, CV], F32)
    VLNVB = sb("VLNVB", [128, CV], F32)
    VBOUT = sb("VBOUT", [128, KD], F32)
    VBPW1 = sb("VBPW1", [128, 16], F32)
    WDW = sb("WDW", [128, KD, NTAP], F32)
    VBDW = sb("VBDW", [128, KD], F32)
    VCLG = sb("VCLG", [128, KD], F32)
    VCLB = sb("VCLB", [128, KD], F32)
    VBPW2 = sb("VBPW2", [128, KD], F32)
    VLNG = sb("VLNG", [128, 32], F32)
    VLNB = sb("VLNB", [128, 32], F32)
    VLNGA = sb("VLNGA", [128, 32], F32)
    VLNBA = sb("VLNBA", [128, 32], F32)
    EPST = sb("EPST", [128, 1], F32)
    HIST = sb("HIST", [128, KD, HW_], BF16)
    GL = sb("GL", [128, KD, NSAMP, HW_], F32)
    RT = {n: sb("RT_" + n, [128, NE], F32) for n in ("L", "M1", "L2", "M2", "G")}
    RS = {n: sb("RS_" + n, [128, 1], F32) for n in ("m1", "m2", "d", "e", "den", "w1", "w2")}
    I32 = mybir.dt.int32
    CAP = int(dbg.get("cap", 192))
    PM = LNS
    IOTA_F = sb("IOTA_F", [128, WT], F32)
    SLOT_I = sb("SLOT_I", [128, 4], I32)
    SLOTID = sb("SLOTID", [128, 4], F32)
    UTRI = sb("UTRI", [128, 128], BF16)
    MSKF = sb("MSKF", [128, 4, NE], F32)
    MSK = sb("MSK", [128, 4, NE], BF16)
    POS = sb("POS", [128, 4, NE], F32)
    POST = sb("POST", [8, WT], F32)
    CNT = sb("CNT", [1, NE], I32)
    CNTM = sb("CNTM", [1, 1], I32)
    CNTMF = sb("CNTMF", [1, 1], F32)
    PS = [es.enter_context(nc.psum_tensor("ps%d" % i, [128, 512], F32)) for i in range(8)]

    R = {}

    def res(name, const=False):
        if name not in R:
            R[name] = Res(const)
        return R[name]

    rPS = [res("ps%d" % i) for i in range(8)]
    for r_ in rPS:
        r_.excl = True
    rRING = [res("ring%d" % i) for i in range(NSLOT)]
    rXFm = [res("XF%d" % m) for m in range(KD)]
    rXB = res("XB")
    rXS = [res("XS0"), res("XS0")]
    rC = res("consts")
    rCd, rWSA, rCp, rCv = res("Cd"), res("WSA"), res("Cp"), res("Cv")

    def phv(off, n):
        return PH[:, off:off + n]

    VNT = phv(0, 12288).rearrange("p (c t) -> p c t", c=CV)
    OT = VNT
    VN = phv(12288, 12288).rearrange("p (b c) -> p b c", b=4)
    GTB = phv(0, 4336)
    DG = [phv(4336 + i * 3968, 3968).rearrange("p (k c) -> p k c", k=NTAP) for i in range(2)]
    ST = phv(12272, 4096).rearrange("p (m t) -> p m t", m=KD)
    YC = phv(16384, 8192).bitcast(F32).rearrange("p (m t) -> p m t", m=KD)
    HT = [phv(i * 3584, 3584).rearrange("p (f t) -> p f t", f=7) for i in range(2)]
    SS = [phv(7168 + i * 512, 512) for i in range(2)]
    TT = [phv(8192 + i * 512, 512) for i in range(2)]
    GBC = [phv(9216 + i * 1024, 1024).bitcast(F32) for i in range(2)]
    rVNT, rVN, rGTB, rST, rYC = res("VNT"), res("VN"), res("GTB"), res("ST"), res("YC")
    rVNTc = [res("VNTc%d" % c) for c in range(CV)]
    rDG = [res("DG0"), res("DG1")]
    rHT = [res("HT0"), res("HT1")]
    rSS = [res("SS0"), res("SS1")]
    rTT = [res("TT0"), res("TT1")]
    rGBC = [res("GBC0"), res("GBC1")]
    rT1 = [res("T1_0"), res("T1_1")]
    rT2 = [res("T2_0"), res("T2_1")]
    rSQ = [res("SQ0"), res("SQ1")]
    rZB = [res("ZB0"), res("ZB1")]
    rSTAT = res("STAT")
    rHIST, rGL, rGTS, rRT = res("HIST"), res("GL"), res("GTS"), res("RT")
    WA = CAP
    THR = int(dbg.get("thr", CAP))
    if CAP == 256:
        A_BLK = [(0, 0, 128, 0, 128), (1, 128, 128, 0, 128)]
        B_BLK = [(2, 0, 128, 0, 128), (3, 128, 128, 0, 128)]
        B_C0 = 256
    else:
        assert CAP == 192
        A_BLK = [(0, 0, 128, 0, 128), (1, 128, 64, 0, 64)]
        B_BLK = [(1, 0, 128, 64, 64), (2, 128, 128, 0, 128), (3, 256, 128, 0, 128)]
        B_C0 = 128
    WB = WT - B_C0
    B_SB = [1]
    HTA = [phv(i * 1792, 7 * WA).rearrange("p (f t) -> p f t", f=7) for i in range(2)]
    HTBv = phv(3584, 7 * WB).rearrange("p (f t) -> p f t", f=7)
    SSA = [phv(6272 + i * 384, 384) for i in range(2)]
    XTOK = phv(7040, 4096).rearrange("p (b c) -> p b c", b=4)
    PE_ = [phv(11136 + i * 512, 512) for i in range(4)]
    Q_ = [phv(13184 + i * 512, 512) for i in range(4)]
    XG = [phv(15232 + i * 4096, 4096).rearrange("p (k t) -> p k t", k=KD) for i in range(2)]
    GBC1 = phv(23424, 1024).bitcast(F32)
    ACC = [PM[:, i * 2048:(i + 1) * 2048].bitcast(F32) for i in range(4)]
    ACCB = [PM[:, 8192 + i * 1024: 8192 + (i + 1) * 1024] for i in range(4)]
    POSBC = PM[:, 12288:13312].bitcast(F32)
    rXTOK, rPE_, rGBC1, rPOSBC, rHTB = res("XTOK"), res("PE_"), res("GBC1"), res("POSBC"), res("HTB")
    rQ = [res("Q%d" % i) for i in range(4)]
    rXG = [res("XG0"), res("XG1")]
    rHTA = [res("HTA0"), res("HTA1")]
    rSSA = [res("SSA0"), res("SSA1")]
    rACC = [res("ACC%d" % i) for i in range(4)]
    rACCB = [res("ACCB%d" % i) for i in range(4)]
    rMSK, rPOS, rCNT, rPOST = res("MSK"), res("POS"), res("CNT"), res("POST")

    def vec_load(dst, src_row, n):
        dma("sp", dst, src_row.rearrange("o (c p) -> p (o c)", p=128), [], [rCd], "const",
            allow_slow_non_contiguous=True)

    skip = dbg.get("skip", ())
    if 'vec' not in skip:
        vec_load(VB_IN[:], b_in, 48)
        vec_load(VLNVG[:], lnv_g, CV)
        vec_load(VLNVB[:], lnv_b, CV)
        vec_load(VBOUT[:], b_out, KD)
        vec_load(VBPW1[:], b_pw1, 16)
        vec_load(VBDW[:], b_dw, KD)
        vec_load(VCLG[:], cln_g, KD)
        vec_load(VCLB[:], cln_b, KD)
        vec_load(VBPW2[:], b_pw2, KD)
        vec_load(VLNG[:], ln_g, 32)
        vec_load(VLNB[:], ln_b, 32)
    if 'wdw' not in skip:
        for k in range(NTAP):
            dma("sp", WDW[:, :, k], w_dw[k:k + 1, :].rearrange("o (c p) -> p (o c)", p=128), [], [rCd], "const",
                allow_slow_non_contiguous=True)
    if 'wr' not in skip:
        dma("sp", WR[:], w_r.rearrange("(k p) e -> p k e", p=128), [], [rCd], "const")
    if 'br' not in skip:
        dma("sp", BR[:], b_r.partition_broadcast(128), [], [rCd], "const")
    if 'wsa' not in skip:
        dma("sp", WSA[:], w_s.rearrange("g i j -> i g j"), [], [rWSA], "wsa")
    if 'browf' not in skip:
        dma("sp", BROWF[:, 0, :], b_s.rearrange("(o g) i -> o (g i)", o=1), [], [rCd], "const")
        for h in range(2):
            dma("sp", BROWF[:, 1, :].rearrange("o (g i) -> o g i", g=8)[:, :, h * 64:(h + 1) * 64],
                b_s[:, 0:64].rearrange("(o g) i -> o g i", o=1), [], [rCd], "const")
    if 'pool' not in skip:
        op("pool", lambda e: e.memset(identF[:], 0.0), [], [rCp])
        op("pool", lambda e: e.affine_select(out=identF[:], in_=identF[:], compare_op=ALU.not_equal, fill=1.0,
                                             base=0, pattern=[[-1, 128]], channel_multiplier=1), [], [rCp])
    if 'sel' not in skip:
        op("pool", lambda e: e.memset(SEL[:], 0.0), [], [rCp])
        for ee in range(NE):
            op("pool", lambda e, ee=ee: e.affine_select(out=SEL[:, ee, :], in_=SEL[:, ee, :], compare_op=ALU.not_equal,
                                                        fill=1.0, base=-ee, pattern=[[0, 128]], channel_multiplier=1),
               [], [rCp])
    if 'pool2' not in skip:
        op("pool", lambda e: e.memset(EPST[:], EPS), [], [rCp])
        op("pool", lambda e: e.memset(onesB[:], 1.0), [], [rCp])
        op("pool", lambda e: e.memset(HIST[:], 0.0), [], [rHIST])
        op("pool", lambda e: e.iota(out=IOTA_I[:], pattern=[[1, WT]], base=0, channel_multiplier=0), [], [rCp])
        op("pool", lambda e: e.iota(out=SLOT_I[:], pattern=[[128, 4]], base=0, channel_multiplier=1), [], [rCp])
        op("pool", lambda e: e.memset(UTRI_F[:], 1.0), [], [rCp])
        op("pool", lambda e: e.affine_select(out=UTRI_F[:], in_=UTRI_F[:], compare_op=ALU.is_gt, fill=0.0, base=0,
                                             pattern=[[1, 128]], channel_multiplier=-1), [], [rCp])
        op("dve", lambda e: e.tensor_copy(out=IOTA_F[:], in_=IOTA_I[:]), [rCp], [rCv])
        op("dve", lambda e: e.tensor_copy(out=SLOTID[:], in_=SLOT_I[:]), [rCp], [rCv])
        op("dve", lambda e: e.tensor_copy(out=UTRI[:], in_=UTRI_F[:]), [rCp], [rCv])
    if 'identb' not in skip:
        op("dve", lambda e: e.tensor_copy(out=identB[:], in_=identF[:]), [rCp], [rCv])
    if 'lnga' not in skip:
        op("dve", lambda e: e.tensor_scalar(out=VLNGA[:], in0=VLNG[:], scalar1=ALPHA, scalar2=None, op0=ALU.mult), [rCd], [rCv])
        op("dve", lambda e: e.tensor_scalar(out=VLNBA[:], in0=VLNB[:], scalar1=ALPHA, scalar2=None, op0=ALU.mult), [rCd], [rCv])
    if 'brow' not in skip:
        op("dve", lambda e: e.tensor_copy(out=BROW[:, 0, :], in_=BROWF[:, 0, :]), [rCd, rCv], [rCv])
        op("dve", lambda e: e.tensor_copy(out=BROW[:, 2, :], in_=BROWF[:, 1, :]), [rCd, rCv], [rCv])
        op("dve", lambda e: e.tensor_tensor(out=BROWT[:, 0, :], in0=BROWF[:, 0, :], in1=BROW[:, 0, :], op=ALU.subtract), [rCd, rCv], [rCv])
        op("dve", lambda e: e.tensor_tensor(out=BROWT[:, 1, :], in0=BROWF[:, 1, :], in1=BROW[:, 2, :], op=ALU.subtract), [rCd, rCv], [rCv])
        op("dve", lambda e: e.tensor_copy(out=BROW[:, 1, :], in_=BROWT[:, 0, :]), [rCd, rCv], [rCv])
        op("dve", lambda e: e.tensor_copy(out=BROW[:, 3, :], in_=BROWT[:, 1, :]), [rCd, rCv], [rCv])
    if 'wstp' not in skip:
        op("dve", lambda e: e.memset(WST[0][:], 0.0), [], [rCv])
        op("dve", lambda e: e.memset(WST[1][:], 0.0), [], [rCv])
        for g in range(8):
            bank = g % 2
            op("pe", lambda e, g=g, bank=bank: e.transpose(out=PS[bank][:, 0:128], in_=WSA[:, g, :], identity=identF[:]),
               [rWSA, rCp], [rPS[bank]])
            op("dve", lambda e, g=g, bank=bank: e.tensor_copy(out=WST[0][0:64, g, :], in_=PS[bank][0:64, 0:128]),
               [rPS[bank]], [rCv])
            op("dve", lambda e, g=g, bank=bank: e.tensor_copy(out=WST[0][64:128, g, 64:128], in_=PS[bank][64:128, 64:128]),
               [rPS[bank]], [rCv])
    if 'wsts' not in skip:
        op("dve", lambda e: e.memset(WSA[:], 0.0), [], [rWSA])
        dma("sp", WSA[0:64, :, 0:64], w_s[:, 0:64, 0:64].rearrange("g i j -> i g j"), [], [rWSA], "wsa")
        dma("sp", WSA[64:128, :, 64:128], w_s[:, 0:64, 0:64].rearrange("g i j -> i g j"), [], [rWSA], "wsa")
        for g in range(8):
            bank = g % 2
            op("pe", lambda e, g=g, bank=bank: e.transpose(out=PS[bank][:, 0:128], in_=WSA[:, g, :], identity=identF[:]),
               [rWSA, rCp], [rPS[bank]])
            op("dve", lambda e, g=g, bank=bank: e.tensor_copy(out=WST[1][:, g, :], in_=PS[bank][:, 0:128]),
               [rPS[bank]], [rCv])
    B.barrier(["pe", "act", "dve", "pool"])
    rC.w = None
    rC.r = {}
    rC.const = True

    ring_state = {"i": 0, "piece": 0, "first": True}
    NPIECE = 126
    WSCR = nc.dram_tensor("wscr", [NPIECE, 128, SLOT], BF16, kind="Internal").ap()
    rSCR = [Res() for _ in range(NPIECE)]

    def wload(src3, shape):
        i = ring_state["i"] % NSLOT
        ring_state["i"] += 1
        j = ring_state["piece"]
        ring_state["piece"] += 1
        a, b = shape
        flat = RING[i][:, 0:a * b]
        view = flat.rearrange("p (a b) -> p a b", a=a)
        if (ring_state["first"] and not ring_state.get("force_hw")) or dbg.get("noscr"):
            dma("pool", view, src3, [], [rRING[i]], "rgs%d" % i)
            if not dbg.get("noscr"):
                dma("sp", WSCR[j, :, 0:a * b], flat, [rRING[i]], [rSCR[j]], "wst%d" % i)
        else:
            dma("sp", flat, WSCR[j, :, 0:a * b], [rSCR[j]], [rRING[i]], "rgh%d" % i)
        return view, rRING[i]

    w_in_v = w_in.rearrange("(k p) n -> p k n", p=128)
    w_out_v = w_out.rearrange("(c p) n -> p c n", p=128)
    w_pw1_v = w_pw1.rearrange("(k p) n -> p k n", p=128)
    w_pw2_v = w_pw2.rearrange("(k p) n -> p k n", p=128)

    def layer_norm(zs, W, nch, srcs_bf=None, outs=None, fence=()):
        inv = 1.0 / (nch * 128)
        for m in range(nch):
            z, rz = zs[m]
            s = m % 2
            fz = list(fence) if m == 0 else []
            op("act", lambda e, z=z, s=s: e.activation(out=SQ[s][:, 0:W], in_=z, func=AF.Square), [rz], [rSQ[s]] + fz)
            if srcs_bf is None:
                op("dve", lambda e, z=z, s=s: e.tensor_copy(out=ZB[s][:, 0:W], in_=z), [rz], [rZB[s]] + fz)
                zb, rzb = ZB[s][:, 0:W], rZB[s]
            else:
                zb, rzb = srcs_bf[m]
            op("pe", lambda e, zb=zb, m=m: e.matmul(PS[6][:, 0:W], lhsT=onesB[:], rhs=zb, start=(m == 0), stop=(m == nch - 1)),
               [rzb, rC], [rPS[6]], signal=(m == nch - 1))
            op("pe", lambda e, s=s, m=m: e.matmul(PS[7][:, 0:W], lhsT=onesB[:], rhs=SQ[s][:, 0:W], start=(m == 0), stop=(m == nch - 1)),
               [rSQ[s], rC], [rPS[7]], signal=True)
        op("dve", lambda e: e.tensor_scalar(out=MEAN[:, 0:W], in0=PS[6][:, 0:W], scalar1=inv, scalar2=None, op0=ALU.mult),
           [rPS[6]], [rSTAT])
        op("dve", lambda e: e.tensor_tensor(out=VAR[:, 0:W], in0=MEAN[:, 0:W], in1=MEAN[:, 0:W], op=ALU.mult), [rSTAT], [rSTAT])
        op("dve", lambda e: e.scalar_tensor_tensor(out=VAR[:, 0:W], in0=PS[7][:, 0:W], scalar=inv, in1=VAR[:, 0:W],
                                                   op0=ALU.mult, op1=ALU.subtract), [rPS[7], rSTAT], [rSTAT])
        op("act", lambda e: e.activation(out=RSTD[:, 0:W], in_=VAR[:, 0:W], func=AF.Sqrt, bias=EPST[:], scale=1.0),
           [rSTAT, rC], [rSTAT])
        op("dve", lambda e: e.reciprocal(out=RSTD[:, 0:W], in_=RSTD[:, 0:W]), [rSTAT], [rSTAT])
        for m in range(nch):
            z, rz = zs[m]
            s = m % 2
            op("dve", lambda e, z=z, s=s: e.tensor_tensor(out=T1[s][:, 0:W], in0=z, in1=MEAN[:, 0:W], op=ALU.subtract),
               [rz, rSTAT], [rT1[s]])
            op(POOLENG, lambda e, s=s: e.tensor_tensor(out=T2[s][:, 0:W], in0=T1[s][:, 0:W], in1=RSTD[:, 0:W], op=ALU.mult),
               [rT1[s], rSTAT], [rT2[s]])
            outs(m, T2[s][:, 0:W], rT2[s])

    def deepnorm_ln(W, lnidx, scaled, make_xb=True):
        gsrc, bsrc = (VLNGA, VLNBA) if scaled else (VLNG, VLNB)

        def outs(m, t2, rt2):
            col = lnidx * 8 + m
            op("act", lambda e: e.activation(out=XF[:, m, 0:W], in_=t2, func=AF.Identity,
                                             bias=bsrc[:, col:col + 1], scale=gsrc[:, col:col + 1]), [rt2, rC], [rXFm[m]])
            if make_xb:
                if POOLENG == "pool":
                    op("pool", lambda e: e.tensor_scalar(out=XB[:, m, 0:W], in0=t2, scalar1=VLNG[:, col:col + 1],
                                                         scalar2=VLNB[:, col:col + 1], op0=ALU.mult, op1=ALU.add), [rt2, rC], [rXB])
                else:
                    op("act", lambda e: e.activation(out=XB[:, m, 0:W], in_=t2, func=AF.Identity,
                                                     bias=VLNB[:, col:col + 1], scale=VLNG[:, col:col + 1]), [rt2, rC], [rXB])
        layer_norm([(XF[:, m, 0:W], rXFm[m]) for m in range(KD)], W, KD, None, outs,
                   fence=(rACC + rACCB + [rPOSBC]) if lnidx == 3 else ())

    def load_tile(tok0, W):
        nb = W // 128
        LT = _os.environ.get("LT", "dpav")
        for b in range(nb):
            s = b % 2
            if "d" in LT:
                dma("sp", XS[s][:], xin[tok0 + b * 128: tok0 + (b + 1) * 128, :], [], [rXS[s]], "xs%d" % s)
            if "p" in LT:
                for m in range(KD):
                    bank = m // 4
                    op("pe", lambda e, m=m, s=s, bank=bank: e.transpose(out=PS[bank][:, (m % 4) * 128:(m % 4 + 1) * 128],
                                                                        in_=XS[s][:, m * 128:(m + 1) * 128], identity=identF[:]),
                       [rXS[s], rC], [rPS[bank]], signal=(m % 4 == 3))
            for h in range(2):
                src = PS[h][:].rearrange("p (a t) -> p a t", a=4)
                if "a" in LT:
                    op("act", lambda e, h=h, b=b, src=src: e.mul(out=XF[:, 4 * h:4 * h + 4, b * 128:(b + 1) * 128], in_=src, mul=ALPHA),
                       [rPS[h]], rXFm[4 * h:4 * h + 4])
                if "v" in LT:
                    op("dve", lambda e, h=h, b=b, src=src: e.tensor_copy(out=XB[:, 4 * h:4 * h + 4, b * 128:(b + 1) * 128], in_=src),
                       [rPS[h]] + ([rXFm[4 * h]] if _os.environ.get("SER") else []), [rXB])

    def store_tile(tok0, W):
        nb = W // 128
        for b in range(nb):
            s = b % 2
            for m in range(KD):
                bank = 2 + m // 4
                op("pe", lambda e, m=m, b=b, bank=bank: e.transpose(out=PS[bank][:, (m % 4) * 128:(m % 4 + 1) * 128],
                                                                    in_=XF[:, m, b * 128:(b + 1) * 128], identity=identF[:]),
                   [rXFm[m], rC], [rPS[bank]], signal=(m % 4 == 3))
            op("act", lambda e, s=s: e.copy(out=XS[s][:, 0:512], in_=PS[2][:]), [rPS[2]], [rXS[s]])
            op("dve", lambda e, s=s: e.tensor_copy(out=XS[s][:, 512:1024], in_=PS[3][:]), [rPS[3]], [rXS[s]])
            dma("sp", y_o[tok0 + b * 128: tok0 + (b + 1) * 128, :], XS[s][:], [rXS[s]], [], "ys%d" % s)

    def gmlp(W, sample, gv_rows):
        nb = W // 128
        wst = WST[1 if sample else 0]
        bro = 2 if sample else 0
        vps = 0
        for pc in range(4):
            wv, rw = wload(w_in_v[:, :, DV + pc * 768: DV + (pc + 1) * 768], (KD, 768))
            for cc in range(6):
                c = pc * 6 + cc
                bank = vps % 2
                vps += 1
                for k in range(KD):
                    op("pe", lambda e, k=k, cc=cc, bank=bank, wv=wv: e.matmul(PS[bank][:, 0:W], lhsT=wv[:, k, cc * 128:(cc + 1) * 128],
                                                                               rhs=XB[:, k, 0:W], start=(k == 0), stop=(k == KD - 1)),
                       [rw, rXB], [rPS[bank]], signal=(k == KD - 1))
                op("act", lambda e, c=c, bank=bank: e.activation(out=VNT[:, c, 0:W], in_=PS[bank][:, 0:W], func=AF.Gelu,
                                                                 bias=VB_IN[:, 24 + c:25 + c], scale=1.0),
                   [rPS[bank], rC], [rVNTc[c]])

        def outs(c, t2, rt2):
            op("act", lambda e: e.activation(out=VNT[:, c, 0:W], in_=t2, func=AF.Identity,
                                             bias=VLNVB[:, c:c + 1], scale=VLNVG[:, c:c + 1]), [rt2, rC], [rVNTc[c]])
        zs = [(VNT[:, c, 0:W], rVNTc[c]) for c in range(CV)]
        layer_norm(zs, W, CV, zs, outs)
        tb = 0
        for b in range(nb):
            for c8 in range(3):
                bank = 2 + tb % 2
                tb += 1
                psb = PS[bank][:].bitcast(BF16)
                for cc in range(8):
                    c = c8 * 8 + cc
                    op("pe", lambda e, c=c, cc=cc, b=b, psb=psb: e.transpose(out=psb[:, cc * 128:(cc + 1) * 128],
                                                                               in_=VNT[:, c, b * 128:(b + 1) * 128], identity=identB[:]),
                       [rVNTc[c], rC], [rPS[bank]], signal=(cc == 7))
                eng = "act" if c8 == 1 else "dve"
                if eng == "act":
                    op("act", lambda e, b=b, c8=c8, psb=psb: e.copy(out=VN[:, b, c8 * 1024:(c8 + 1) * 1024], in_=psb), [rPS[bank]], [rVN])
                else:
                    op("dve", lambda e, b=b, c8=c8, psb=psb: e.tensor_copy(out=VN[:, b, c8 * 1024:(c8 + 1) * 1024], in_=psb), [rPS[bank]], [rVN])
            if b in gv_rows:
                r0 = gv_rows[b]
                dma("pool", gv_o[r0:r0 + 128, :], VN[:, b, :], [rVN], [], "gv")
        ups = 0
        for pc in range(4):
            wu, rw = wload(w_in_v[:, :, pc * 768:(pc + 1) * 768], (KD, 768))
            for cc in range(6):
                c = pc * 6 + cc
                g = c // 3
                bank = ups % 2
                mb = 2 + ups % 2
                s = ups % 2
                ups += 1
                for k in range(KD):
                    op("pe", lambda e, k=k, cc=cc, bank=bank, wu=wu: e.matmul(PS[bank][:, 0:W], lhsT=wu[:, k, cc * 128:(cc + 1) * 128],
                                                                               rhs=XB[:, k, 0:W], start=(k == 0), stop=(k == KD - 1)),
                       [rw, rXB], [rPS[bank]], signal=(k == KD - 1))
                op("act", lambda e, c=c, bank=bank, s=s: e.activation(out=ZB[s][:, 0:W], in_=PS[bank][:, 0:W], func=AF.Gelu,
                                                                      bias=VB_IN[:, c:c + 1], scale=1.0),
                   [rPS[bank], rC], [rZB[s]])
                for b in range(nb):
                    cols = slice(b * 128, (b + 1) * 128)
                    op("pe", lambda e, b=b, c=c, g=g, mb=mb, cols=cols: e.matmul(PS[mb][:, cols], lhsT=VN[:, b, c * 128:(c + 1) * 128],
                                                                                  rhs=wst[:, g, :], start=True, stop=False),
                       [rVN, rC], [rPS[mb]], signal=False)
                    op("pe", lambda e, g=g, mb=mb, cols=cols: e.matmul(PS[mb][:, cols], lhsT=onesB[0:1, :],
                                                                       rhs=BROW[0:1, bro, g * 128:(g + 1) * 128], start=False, stop=False),
                       [rC], [rPS[mb]], signal=False)
                    op("pe", lambda e, g=g, mb=mb, cols=cols: e.matmul(PS[mb][:, cols], lhsT=onesB[0:1, :],
                                                                       rhs=BROW[0:1, bro + 1, g * 128:(g + 1) * 128], start=False, stop=True),
                       [rC], [rPS[mb]], signal=(b == nb - 1))
                op("dve", lambda e, c=c, mb=mb, s=s: e.tensor_tensor(out=OT[:, c, 0:W], in0=ZB[s][:, 0:W], in1=PS[mb][:, 0:W], op=ALU.mult),
                   [rZB[s], rPS[mb]], [rVNTc[c]])
        for pc in range(4):
            wo, rw = wload(w_out_v[:, :, pc * 256:(pc + 1) * 256], (CV, 256))
            for mm in range(2):
                m = pc * 2 + mm
                bank = 4 + m % 2
                for c in range(CV):
                    op("pe", lambda e, c=c, mm=mm, bank=bank, wo=wo: e.matmul(PS[bank][:, 0:W], lhsT=wo[:, c, mm * 128:(mm + 1) * 128],
                                                                               rhs=OT[:, c, 0:W], start=(c == 0), stop=(c == CV - 1)),
                       [rw, rVNTc[c]], [rPS[bank]], signal=(c == CV - 1))
                op("dve", lambda e, m=m, bank=bank: e.scalar_tensor_tensor(out=XF[:, m, 0:W], in0=PS[bank][:, 0:W], scalar=VBOUT[:, m:m + 1],
                                                                           in1=XF[:, m, 0:W], op0=ALU.add, op1=ALU.add),
                   [rPS[bank], rC, rXFm[m]], [rXFm[m]])

    def experts(W, wg_list, gated):
        cnt = 0
        for ei, (gv, uv, dv) in enumerate(wg_list):
            if gated:
                gs = ei % 2
                op("pe", lambda e, ei=ei: e.matmul(PS[6][:, 0:W], lhsT=SEL[:, ei, :], rhs=GTS[0:8, 0:W], start=True, stop=True),
                   [rC, rGTS], [rPS[6]])
                op("act", lambda e, gs=gs: e.copy(out=GBC[gs][:, 0:W], in_=PS[6][:, 0:W]), [rPS[6]], [rGBC[gs]])
            for gi in range(4):
                wgp, rwg = wload(gv[:, :, gi * 896:(gi + 1) * 896], (KD, 896))
                wup, rwu = wload(uv[:, :, gi * 896:(gi + 1) * 896], (KD, 896))
                wdp, rwd = wload(dv[:, gi * 7:(gi + 1) * 7, :], (7, D))
                hs = cnt % 2
                cnt += 1
                for fc in range(7):
                    s = fc % 2
                    gb, ub = fc % 2, 2 + fc % 2
                    for k in range(KD):
                        op("pe", lambda e, k=k, fc=fc, gb=gb, wgp=wgp: e.matmul(PS[gb][:, 0:W], lhsT=wgp[:, k, fc * 128:(fc + 1) * 128],
                                                                                 rhs=XB[:, k, 0:W], start=(k == 0), stop=(k == KD - 1)),
                           [rwg, rXB], [rPS[gb]], signal=(k == KD - 1))
                    for k in range(KD):
                        op("pe", lambda e, k=k, fc=fc, ub=ub, wup=wup: e.matmul(PS[ub][:, 0:W], lhsT=wup[:, k, fc * 128:(fc + 1) * 128],
                                                                                 rhs=XB[:, k, 0:W], start=(k == 0), stop=(k == KD - 1)),
                           [rwu, rXB], [rPS[ub]], signal=(k == KD - 1))
                    op("act", lambda e, gb=gb, s=s: e.activation(out=SS[s][:, 0:W], in_=PS[gb][:, 0:W], func=AF.Silu), [rPS[gb]], [rSS[s]])
                    if gated:
                        op("dve", lambda e, ub=ub, s=s: e.tensor_tensor(out=TT[s][:, 0:W], in0=SS[s][:, 0:W], in1=PS[ub][:, 0:W], op=ALU.mult),
                           [rSS[s], rPS[ub]], [rTT[s]])
                        op("dve", lambda e, s=s, hs=hs, fc=fc, gs=gs: e.tensor_tensor(out=HT[hs][:, fc, 0:W], in0=TT[s][:, 0:W],
                                                                                      in1=GBC[gs][:, 0:W], op=ALU.mult),
                           [rTT[s], rGBC[gs]], [rHT[hs]])
                    else:
                        op("dve", lambda e, ub=ub, s=s, hs=hs, fc=fc: e.tensor_tensor(out=HT[hs][:, fc, 0:W], in0=SS[s][:, 0:W],
                                                                                      in1=PS[ub][:, 0:W], op=ALU.mult),
                           [rSS[s], rPS[ub]], [rHT[hs]])
                for m in range(KD):
                    yb = 4 + m % 2
                    for fc in range(7):
                        op("pe", lambda e, fc=fc, m=m, yb=yb, hs=hs, wdp=wdp: e.matmul(PS[yb][:, 0:W], lhsT=wdp[:, fc, m * 128:(m + 1) * 128],
                                                                                        rhs=HT[hs][:, fc, 0:W], start=(fc == 0), stop=(fc == 6)),
                           [rwd, rHT[hs]], [rPS[yb]], signal=(fc == 6))
                    op("dve", lambda e, m=m, yb=yb: e.tensor_tensor(out=XF[:, m, 0:W], in0=XF[:, m, 0:W], in1=PS[yb][:, 0:W], op=ALU.add),
                       [rPS[yb], rXFm[m]], [rXFm[m]])

    def experts_sparse(wg_list, mode):
        IFE = ["pe", "act", "dve"]
        cnt = 0

        def gather(ei, xg, rxg, c0, width):
            kper = max(1, 512 // width)
            gi_ = 0
            for k0 in range(0, KD, kper):
                bank = 6 + gi_ % 2
                gi_ += 1
                ks = list(range(k0, min(KD, k0 + kper)))
                for kk, k in enumerate(ks):
                    for tb in range(4):
                        op("pe", lambda e, k=k, kk=kk, tb=tb, bank=bank: e.matmul(PS[bank][:, kk * width:(kk + 1) * width],
                                                                                  lhsT=XTOK[:, tb, k * 128:(k + 1) * 128],
                                                                                  rhs=PE_[tb][:, c0:c0 + width], start=(tb == 0), stop=(tb == 3)),
                           [rXTOK, rPE_], [rPS[bank]], signal=(tb == 3 and kk == len(ks) - 1))
                src = PS[bank][:, 0:len(ks) * width].rearrange("p (a t) -> p a t", a=len(ks))
                dst = xg[:, ks[0]:ks[-1] + 1, c0:c0 + width]
                if gi_ % 2 == 0:
                    op("act", lambda e, src=src, dst=dst: e.copy(out=dst, in_=src), [rPS[bank]], [rxg])
                else:
                    op("dve", lambda e, src=src, dst=dst: e.tensor_copy(out=dst, in_=src), [rPS[bank]], [rxg])

        def ffn_block(gi, xg, rxg, c0, width, ht, rht, sbs, wgp, rwg, wup, rwu, wdp, rwd):
            for fc in range(7):
                s_ = fc % 2
                gb, ub = fc % 2, 2 + fc % 2
                for k in range(KD):
                    op("pe", lambda e, k=k, fc=fc, gb=gb: e.matmul(PS[gb][:, 0:width], lhsT=wgp[:, k, fc * 128:(fc + 1) * 128],
                                                                    rhs=xg[:, k, c0:c0 + width], start=(k == 0), stop=(k == KD - 1)),
                       [rwg, rxg], [rPS[gb]], signal=(k == KD - 1))
                for k in range(KD):
                    op("pe", lambda e, k=k, fc=fc, ub=ub: e.matmul(PS[ub][:, 0:width], lhsT=wup[:, k, fc * 128:(fc + 1) * 128],
                                                                    rhs=xg[:, k, c0:c0 + width], start=(k == 0), stop=(k == KD - 1)),
                       [rwu, rxg], [rPS[ub]], signal=(k == KD - 1))
                op("act", lambda e, gb=gb, s_=s_: e.activation(out=SSA[s_][:, 0:width], in_=PS[gb][:, 0:width], func=AF.Silu),
                   [rPS[gb]], [rSSA[s_]])
                op("dve", lambda e, ub=ub, s_=s_, fc=fc: e.tensor_tensor(out=ht[:, fc, 0:width], in0=SSA[s_][:, 0:width], in1=PS[ub][:, 0:width],
                                                                         op=ALU.mult), [rSSA[s_], rPS[ub]], [rht])
            yi = 0
            for (sbk, hc0, mw, r0, nr) in sbs:
                for half in range(2):
                    yb = 4 + yi % 2
                    yi += 1
                    for fc in range(7):
                        op("pe", lambda e, fc=fc, hc0=hc0, mw=mw, half=half, yb=yb: e.matmul(PS[yb][0:mw, 0:512], lhsT=ht[:, fc, hc0:hc0 + mw],
                                                                                            rhs=wdp[:, fc, half * 512:(half + 1) * 512],
                                                                                            start=(fc == 0), stop=(fc == 6)),
                           [rwd, rht], [rPS[yb]], signal=(fc == 6))
                    dst = ACC[sbk][r0:r0 + nr, half * 512:(half + 1) * 512]
                    src = PS[yb][r0:r0 + nr, 0:512]
                    if gi == 0:
                        op("dve", lambda e, dst=dst, src=src: e.tensor_copy(out=dst, in_=src), [rPS[yb]], [rACC[sbk]])
                    else:
                        op("dve", lambda e, dst=dst, src=src: e.tensor_tensor(out=dst, in0=dst, in1=src, op=ALU.add),
                           [rPS[yb], rACC[sbk]], [rACC[sbk]])

        def scatter(sbs):
            for (sbk, hc0, mw, r0, nr) in sbs:
                op("act", lambda e, sbk=sbk, r0=r0, nr=nr: e.copy(out=ACCB[sbk][r0:r0 + nr, :], in_=ACC[sbk][r0:r0 + nr, :]),
                   [rACC[sbk]], [rACCB[sbk]])
            for m in range(KD):
                bank = 6 + m % 2
                for j, (sbk, hc0, mw, r0, nr) in enumerate(sbs):
                    op("pe", lambda e, m=m, j=j, sbk=sbk, r0=r0, nr=nr, bank=bank, n=len(sbs): e.matmul(
                        PS[bank][:, 0:512], lhsT=ACCB[sbk][r0:r0 + nr, m * 128:(m + 1) * 128], rhs=Q_[sbk][r0:r0 + nr, :],
                        start=(j == 0), stop=(j == n - 1)),
                       [rACCB[sbk], rQ[sbk]], [rPS[bank]], signal=(j == len(sbs) - 1))
                op("dve", lambda e, m=m, bank=bank: e.tensor_tensor(out=XF[:, m, 0:512], in0=XF[:, m, 0:512], in1=PS[bank][:, 0:512], op=ALU.add),
                   [rPS[bank], rXFm[m]], [rXFm[m]])

        def build_q(sbs):
            for sbk in sbs:
                op("dve", lambda e, sbk=sbk: e.scalar_tensor_tensor(out=Q_[sbk][:], in0=POSBC[:], scalar=SLOTID[:, sbk:sbk + 1], in1=GBC1[:],
                                                                    op0=ALU.is_equal, op1=ALU.mult), [rPOSBC, rGBC1, rC], [rQ[sbk]])

        for ei, (gv, uv, dv) in enumerate(wg_list):
            xg, rxg = XG[ei % 2], rXG[ei % 2]
            lnf = (rT1 + rT2 + rSQ + rZB + [rSTAT]) if (ei == 0 and mode == "A") else []
            for tb in range(4):
                op("dve", lambda e, tb=tb, ei=ei: e.tensor_scalar(out=PE_[tb][:], in0=IOTA_F[:], scalar1=POS[:, tb, ei:ei + 1],
                                                                  scalar2=MSKF[:, tb, ei:ei + 1], op0=ALU.is_equal, op1=ALU.mult),
                   [rPOS, rMSK, rC], [rPE_] + (lnf if tb == 0 else []))
            op("pe", lambda e, ei=ei: e.matmul(PS[6][:, 0:512], lhsT=SEL[:, ei, :], rhs=GTS[0:8, 0:512], start=True, stop=True),
               [rC, rGTS], [rPS[6]])
            op("pe", lambda e, ei=ei: e.matmul(PS[7][:, 0:512], lhsT=SEL[:, ei, :], rhs=POST[0:8, 0:512], start=True, stop=True),
               [rC, rPOST], [rPS[7]])
            op("act", lambda e: e.copy(out=GBC1[:], in_=PS[6][:, 0:512]), [rPS[6]], [rGBC1] + lnf)
            op("act", lambda e: e.copy(out=POSBC[:], in_=PS[7][:, 0:512]), [rPS[7]], [rPOSBC])
            build_q([0, 1, 2, 3])
            if mode == "A":
                gather(ei, xg, rxg, 0, WA)
            else:
                gather(ei, xg, rxg, B_C0, WT - B_C0)
            for gi in range(4):
                wgp, rwg = wload(gv[:, :, gi * 896:(gi + 1) * 896], (KD, 896))
                wup, rwu = wload(uv[:, :, gi * 896:(gi + 1) * 896], (KD, 896))
                wdp, rwd = wload(dv[:, gi * 7:(gi + 1) * 7, :], (7, D))
                hs = cnt % 2
                cnt += 1
                if mode == "A":
                    ffn_block(gi, xg, rxg, 0, WA, HTA[hs], rHTA[hs], A_BLK, wgp, rwg, wup, rwu, wdp, rwd)
                else:
                    ffn_block(gi, xg, rxg, B_C0, WB, HTBv, rHTB, B_BLK, wgp, rwg, wup, rwu, wdp, rwd)
            scatter(A_BLK if mode == "A" else B_BLK)

    def moe_sparse(wg_list):
        piece0 = ring_state["piece"]
        experts_sparse(wg_list, "A")
        IFE = ["pe", "act", "dve", "sp"]
        for eng in ("pe", "act", "dve"):
            op(eng, lambda e, eng=eng: e.reg_load(B.regs[eng], CNTM[0:1, 0:1]), [rCNT], [], signal=False)
        B.raw("sp", lambda e: e.reg_load(B.regs["sp"], CNTM[0:1, 0:1]), [rCNT])
        B.if_begin(IFE, 0, THR)
        ring_state["piece"] = piece0
        ring_state["force_hw"] = True
        experts_sparse(wg_list, "B")
        ring_state["force_hw"] = False
        B.if_end()

    def conv_module(W, nseq, L, sample, last_prompt):
        PADL = HW_ + L
        gtb = GTB[:, 0:KD * nseq * PADL].rearrange("p (m s t) -> p m s t", m=KD, s=nseq)
        if sample:
            for s in range(nseq):
                xs = s % 2
                dma("sp", XS[xs][0:HW_, :], cache[s], [], [rXS[xs]], "xs%d" % xs)
                bank = s % 2
                for m in range(KD):
                    op("pe", lambda e, m=m, xs=xs, bank=bank: e.transpose(out=PS[bank][:, m * HW_:(m + 1) * HW_],
                                                                          in_=XS[xs][0:HW_, m * 128:(m + 1) * 128], identity=identF[0:HW_, 0:HW_]),
                       [rXS[xs], rC], [rPS[bank]], signal=(m == KD - 1))
                op("dve", lambda e, s=s, bank=bank: e.tensor_copy(out=gtb[:, :, s, 0:HW_],
                                                                  in_=PS[bank][:, 0:KD * HW_].rearrange("p (m t) -> p m t", m=KD)),
                   [rPS[bank]], [rGTB])
        else:
            op("dve", lambda e: e.tensor_copy(out=gtb[:, :, 0, 0:HW_], in_=HIST[:]), [rHIST], [rGTB])
        pieces = {}
        for half in range(2):
            wa, rwa = wload(w_pw1_v[:, :, half * 512:(half + 1) * 512], (KD, 512))
            wgt, rwgt = wload(w_pw1_v[:, :, D + half * 512: D + (half + 1) * 512], (KD, 512))
            for mm in range(4):
                m = half * 4 + mm
                ab, gb = m % 2, 2 + m % 2
                s = m % 2
                for k in range(KD):
                    op("pe", lambda e, k=k, mm=mm, ab=ab, wa=wa: e.matmul(PS[ab][:, 0:W], lhsT=wa[:, k, mm * 128:(mm + 1) * 128],
                                                                           rhs=XB[:, k, 0:W], start=(k == 0), stop=(k == KD - 1)),
                       [rwa, rXB], [rPS[ab]], signal=(k == KD - 1))
                for k in range(KD):
                    op("pe", lambda e, k=k, mm=mm, gb=gb, wgt=wgt: e.matmul(PS[gb][:, 0:W], lhsT=wgt[:, k, mm * 128:(mm + 1) * 128],
                                                                             rhs=XB[:, k, 0:W], start=(k == 0), stop=(k == KD - 1)),
                       [rwgt, rXB], [rPS[gb]], signal=(k == KD - 1))
                op("act", lambda e, m=m, gb=gb, s=s: e.activation(out=T1[s][:, 0:W], in_=PS[gb][:, 0:W], func=AF.Sigmoid,
                                                                  bias=VBPW1[:, 8 + m:9 + m], scale=1.0), [rPS[gb], rC], [rT1[s]])
                a3 = PS[ab][:, 0:W].rearrange("p (s t) -> p s t", s=nseq)
                g3 = T1[s][:, 0:W].rearrange("p (s t) -> p s t", s=nseq)
                op("dve", lambda e, m=m, a3=a3, g3=g3: e.scalar_tensor_tensor(out=gtb[:, m, :, HW_:PADL], in0=a3, scalar=VBPW1[:, m:m + 1],
                                                                              in1=g3, op0=ALU.add, op1=ALU.mult),
                   [rPS[ab], rT1[s], rC], [rGTB])
                if sample or last_prompt:
                    op("dve", lambda e, m=m, a3=a3, g3=g3: e.scalar_tensor_tensor(out=GL[:, m, 0:nseq, :], in0=a3[:, :, L - HW_:L],
                                                                                  scalar=VBPW1[:, m:m + 1], in1=g3[:, :, L - HW_:L],
                                                                                  op0=ALU.add, op1=ALU.mult),
                       [rPS[ab], rT1[s], rC], [rGL])
        if not sample:
            op("dve", lambda e: e.tensor_copy(out=HIST[:], in_=gtb[:, :, 0, L:PADL]), [rGTB], [rHIST])
        if sample or last_prompt:
            for s in range(nseq):
                xs = s % 2
                for m in range(KD):
                    bank = 6 + m // 4
                    op("pe", lambda e, m=m, s=s, bank=bank: e.transpose(out=PS[bank][0:HW_, (m % 4) * 128:(m % 4 + 1) * 128],
                                                                        in_=GL[:, m, s, :], identity=identF[:]),
                       [rGL, rC], [rPS[bank]], signal=(m % 4 == 3))
                op("act", lambda e, xs=xs: e.copy(out=XS[xs][0:HW_, 0:512], in_=PS[6][0:HW_, :]), [rPS[6]], [rXS[xs]])
                op("dve", lambda e, xs=xs: e.tensor_copy(out=XS[xs][0:HW_, 512:1024], in_=PS[7][0:HW_, :]), [rPS[7]], [rXS[xs]])
                r0 = (HW_ * (1 + s)) if sample else 0
                dma("sp", cs_o[r0:r0 + HW_, :], XS[xs][0:HW_, :], [rXS[xs]], [], "ys%d" % xs)
        for m in range(KD):
            ds = m % 2
            for k in range(NTAP):
                op(POOLENG, lambda e, m=m, k=k, ds=ds: e.tensor_scalar(out=DG[ds][:, k, :], in0=identB[:], scalar1=WDW[:, m, k:k + 1],
                                                                       scalar2=None, op0=ALU.mult), [rC], [rDG[ds]])
            bank = 4 + m % 2
            for s in range(nseq):
                for k in range(NTAP):
                    op("pe", lambda e, m=m, k=k, s=s, ds=ds, bank=bank: e.matmul(PS[bank][:, s * L:(s + 1) * L], lhsT=DG[ds][:, k, :],
                                                                                  rhs=gtb[:, m, s, k:k + L], start=(k == 0), stop=(k == NTAP - 1)),
                       [rDG[ds], rGTB], [rPS[bank]], signal=(k == NTAP - 1 and s == nseq - 1))
            op("act", lambda e, m=m, bank=bank: e.activation(out=YC[:, m, 0:W], in_=PS[bank][:, 0:W], func=AF.Identity,
                                                             bias=VBDW[:, m:m + 1], scale=1.0), [rPS[bank], rC], [rYC])

        def outs(m, t2, rt2):
            op("act", lambda e: e.activation(out=ST[:, m, 0:W], in_=t2, func=AF.Silu, bias=VCLB[:, m:m + 1], scale=VCLG[:, m:m + 1]),
               [rt2, rC], [rST])
        layer_norm([(YC[:, m, 0:W], rYC) for m in range(KD)], W, KD, None, outs)
        for half in range(2):
            wp, rwp = wload(w_pw2_v[:, :, half * 512:(half + 1) * 512], (KD, 512))
            for mm in range(4):
                m = half * 4 + mm
                bank = 4 + m % 2
                for k in range(KD):
                    op("pe", lambda e, k=k, mm=mm, bank=bank, wp=wp: e.matmul(PS[bank][:, 0:W], lhsT=wp[:, k, mm * 128:(mm + 1) * 128],
                                                                               rhs=ST[:, k, 0:W], start=(k == 0), stop=(k == KD - 1)),
                       [rwp, rST], [rPS[bank]], signal=(k == KD - 1))
                op("dve", lambda e, m=m, bank=bank: e.scalar_tensor_tensor(out=XF[:, m, 0:W], in0=PS[bank][:, 0:W], scalar=VBPW2[:, m:m + 1],
                                                                           in1=XF[:, m, 0:W], op0=ALU.add, op1=ALU.add),
                   [rPS[bank], rC, rXFm[m]], [rXFm[m]])

    def router(W, sparse=False):
        nb = W // 128
        L_, M1, L2, M2, G = RT["L"], RT["M1"], RT["L2"], RT["M2"], RT["G"]
        for b in range(nb):
            for k in range(KD):
                op("pe", lambda e, k=k, b=b: e.matmul(PS[7][:, 0:NE], lhsT=XF[:, k, b * 128:(b + 1) * 128], rhs=WR[:, k, :],
                                                       start=(k == 0), stop=(k == KD - 1)),
                   [rXFm[k], rC], [rPS[7]], signal=(k == KD - 1))
            ops = [
                lambda e: e.tensor_tensor(out=L_[:], in0=PS[7][:, 0:NE], in1=BR[:], op=ALU.add),
                lambda e: e.tensor_reduce(out=RS["m1"][:], in_=L_[:], axis=AX.X, op=ALU.max),
                lambda e: e.tensor_scalar(out=M1[:], in0=L_[:], scalar1=RS["m1"][:], scalar2=None, op0=ALU.is_equal),
                lambda e: e.scalar_tensor_tensor(out=L2[:], in0=M1[:], scalar=-1e30, in1=L_[:], op0=ALU.mult, op1=ALU.add),
                lambda e: e.tensor_reduce(out=RS["m2"][:], in_=L2[:], axis=AX.X, op=ALU.max),
                lambda e: e.tensor_scalar(out=M2[:], in0=L2[:], scalar1=RS["m2"][:], scalar2=None, op0=ALU.is_equal),
                lambda e: e.tensor_tensor(out=RS["d"][:], in0=RS["m2"][:], in1=RS["m1"][:], op=ALU.subtract),
            ]
            for f in ops:
                op("dve", f, [rPS[7], rC, rRT], [rRT])
            op("act", lambda e: e.activation(out=RS["e"][:], in_=RS["d"][:], func=AF.Exp), [rRT], [rRT])
            ops = [
                lambda e: e.tensor_scalar(out=RS["den"][:], in0=RS["e"][:], scalar1=1.0, scalar2=None, op0=ALU.add),
                lambda e: e.reciprocal(out=RS["w1"][:], in_=RS["den"][:]),
                lambda e: e.tensor_tensor(out=RS["w2"][:], in0=RS["e"][:], in1=RS["w1"][:], op=ALU.mult),
                lambda e: e.tensor_scalar(out=G[:], in0=M1[:], scalar1=RS["w1"][:], scalar2=None, op0=ALU.mult),
                lambda e: e.scalar_tensor_tensor(out=G[:], in0=M2[:], scalar=RS["w2"][:], in1=G[:], op0=ALU.mult, op1=ALU.add),
            ]
            for f in ops:
                op("dve", f, [rRT], [rRT])
            op("pe", lambda e: e.transpose(out=PS[6][0:NE, 0:128], in_=G[:], identity=identF[:]), [rRT, rC], [rPS[6]])
            op("dve", lambda e, b=b: e.tensor_copy(out=GTS[0:8, b * 128:(b + 1) * 128], in_=PS[6][0:NE, 0:128]), [rPS[6]], [rGTS])
            if sparse:
                op("dve", lambda e, b=b: e.tensor_tensor(out=MSKF[:, b, :], in0=M1[:], in1=M2[:], op=ALU.add), [rRT], [rMSK])
                op("dve", lambda e, b=b: e.tensor_copy(out=MSK[:, b, :], in_=MSKF[:, b, :]), [rMSK], [rMSK])
        if sparse:
            for b in range(nb):
                terms = [(onesB, bb) for bb in range(b)] + [(UTRI, b)]
                for i, (lt, bb) in enumerate(terms):
                    op("pe", lambda e, lt=lt, bb=bb, b=b, i=i, n=len(terms): e.matmul(PS[6][:, b * NE:(b + 1) * NE], lhsT=lt[:], rhs=MSK[:, bb, :],
                                                                                      start=(i == 0), stop=(i == n - 1)),
                       [rMSK, rC], [rPS[6]], signal=False)
            for b in range(nb):
                op("pe", lambda e, b=b: e.matmul(PS[6][:, 4 * NE:5 * NE], lhsT=onesB[:], rhs=MSK[:, b, :], start=(b == 0), stop=(b == nb - 1)),
                   [rMSK, rC], [rPS[6]], signal=(b == nb - 1))
            op("dve", lambda e: e.tensor_copy(out=POS[:], in_=PS[6][:, 0:4 * NE].rearrange("p (b e) -> p b e", b=4)), [rPS[6]], [rPOS])
            op("dve", lambda e: e.tensor_copy(out=CNT[:], in_=PS[6][0:1, 4 * NE:5 * NE]), [rPS[6]], [rCNT])
            op("dve", lambda e: e.tensor_reduce(out=CNTMF[:], in_=PS[6][0:1, 4 * NE:5 * NE], axis=AX.X, op=ALU.max), [rPS[6]], [rCNT])
            op("dve", lambda e: e.tensor_copy(out=CNTM[:], in_=CNTMF[:]), [rCNT], [rCNT])
            for b in range(nb):
                op("pe", lambda e, b=b: e.transpose(out=PS[7][0:NE, b * 128:(b + 1) * 128], in_=POS[:, b, :], identity=identF[:]),
                   [rPOS, rC], [rPS[7]], signal=(b == nb - 1))
            op("dve", lambda e: e.tensor_copy(out=POST[:], in_=PS[7][0:NE, :]), [rPS[7]], [rPOST])
            for b in range(nb):
                bank = 4 + b % 2
                psb = PS[bank][:].bitcast(BF16)
                for k in range(KD):
                    op("pe", lambda e, k=k, b=b, psb=psb: e.transpose(out=psb[:, k * 128:(k + 1) * 128], in_=XB[:, k, b * 128:(b + 1) * 128],
                                                                       identity=identB[:]),
                       [rXB, rC], [rPS[bank]], signal=(k == KD - 1))
                if b % 2 == 0:
                    op("act", lambda e, b=b, psb=psb: e.copy(out=XTOK[:, b, :], in_=psb), [rPS[bank]], [rXTOK])
                else:
                    op("dve", lambda e, b=b, psb=psb: e.tensor_copy(out=XTOK[:, b, :], in_=psb), [rPS[bank]], [rXTOK])
        for m in range(KD):
            op("act", lambda e, m=m: e.mul(out=XF[:, m, 0:W], in_=XF[:, m, 0:W], mul=ALPHA), [rXFm[m]], [rXFm[m]])

    ff_views = [(ffg.rearrange("(k p) n -> p k n", p=128), ffu.rearrange("(k p) n -> p k n", p=128),
                 ffd.rearrange("(c p) n -> p c n", p=128))]
    moe_views = [(mog[e_].rearrange("(k p) n -> p k n", p=128), mou[e_].rearrange("(k p) n -> p k n", p=128),
                  mod[e_].rearrange("(c p) n -> p c n", p=128)) for e_ in range(NE)]

    tiles = [(i * WT, WT, 1, WT, False, i == NPT - 1) for i in range(NPT)]
    tiles.append((SEQ, NSAMP * LS, NSAMP, LS, True, False))
    stop = dbg.get("stop")
    if "tiles" in dbg:
        tiles = [tiles[i] for i in dbg["tiles"]]
    for ti_, (tok0, W, nseq, L, sample, last_prompt) in enumerate(tiles):
        ring_state["piece"] = 0
        ring_state["first"] = (ti_ == 0)
        if not dbg.get("noload"):
            load_tile(tok0, W)
        if sample:
            gv_rows = {0: 128, 1: 256}
        elif last_prompt:
            gv_rows = {3: 0}
        else:
            gv_rows = {}
        phases = [
            ("load", lambda: None),
            ("gmlp", lambda: gmlp(W, sample, gv_rows)),
            ("ln0", lambda: deepnorm_ln(W, 0, scaled=True)),
            ("ffn", lambda: experts(W, ff_views, gated=False)),
            ("ln1", lambda: deepnorm_ln(W, 1, scaled=True)),
            ("conv", lambda: conv_module(W, nseq, L, sample, last_prompt)),
            ("ln2", lambda: deepnorm_ln(W, 2, scaled=False)),
            ("router", lambda: router(W, sparse=(not sample) and not dbg.get("dense"))),
            ("moe", lambda: (experts(W, moe_views[:dbg.get("nexp", NE)], gated=True) if (sample or dbg.get("dense"))
                             else moe_sparse(moe_views[:dbg.get("nexp", NE)]))),
            ("ln3", lambda: deepnorm_ln(W, 3, scaled=False, make_xb=False)),
        ]
        for name, fn in phases:
            fn()
            if stop == name:
                break
        if not dbg.get("nostore"):
            store_tile(tok0, W)
    B.final_wait("sp")
    B.final_wait("pool")
    B.emit()


_NC_CACHE = {}


def kernel(x_prompt, x_sample, cache_conv, gm_w_in, gm_b_in, gm_lnv_g, gm_lnv_b, gm_w_s, gm_b_s, gm_w_out, gm_b_out,
           cv_w_pw1, cv_b_pw1, cv_w_dw, cv_b_dw, cv_ln_g, cv_ln_b, cv_w_pw2, cv_b_pw2,
           ff_w_gate, ff_w_up, ff_w_down, moe_w_router, moe_b_router, moe_w_gate, moe_w_up, moe_w_down, ln_g, ln_b):
    f = lambda a: np.ascontiguousarray(np.asarray(a, dtype=np.float32))
    shared = {
        "gm_w_in": f(gm_w_in[0]), "gm_b_in": f(gm_b_in).reshape(1, -1), "gm_lnv_g": f(gm_lnv_g).reshape(1, -1),
        "gm_lnv_b": f(gm_lnv_b).reshape(1, -1), "gm_w_s": f(gm_w_s[0]), "gm_b_s": f(gm_b_s[0]),
        "gm_w_out": f(gm_w_out[0]), "gm_b_out": f(gm_b_out).reshape(1, -1),
        "cv_w_pw1": f(cv_w_pw1[0]), "cv_b_pw1": f(cv_b_pw1).reshape(1, -1), "cv_w_dw": f(cv_w_dw[0]),
        "cv_b_dw": f(cv_b_dw).reshape(1, -1), "cv_ln_g": f(cv_ln_g).reshape(1, -1), "cv_ln_b": f(cv_ln_b).reshape(1, -1),
        "cv_w_pw2": f(cv_w_pw2[0]), "cv_b_pw2": f(cv_b_pw2).reshape(1, -1),
        "ff_w_gate": f(ff_w_gate[0]), "ff_w_up": f(ff_w_up[0]), "ff_w_down": f(ff_w_down[0]),
        "moe_w_router": f(moe_w_router[0]), "moe_b_router": f(moe_b_router).reshape(1, -1),
        "moe_w_gate": f(moe_w_gate[0]), "moe_w_up": f(moe_w_up[0]), "moe_w_down": f(moe_w_down[0]),
        "ln_g": f(ln_g).reshape(1, -1), "ln_b": f(ln_b).reshape(1, -1),
    }
    xp = f(x_prompt)
    xs = f(x_sample)
    cc = f(cache_conv)
    in_maps = []
    for c in range(NCORE):
        xin = np.concatenate([xp[c], xs[c * NSAMP:(c + 1) * NSAMP].reshape(NSAMP * LS, D)], axis=0)
        m = dict(shared)
        m["xin"] = np.ascontiguousarray(xin)
        m["cache"] = np.ascontiguousarray(cc[0, c * NSAMP:(c + 1) * NSAMP])
        in_maps.append(m)
    if "nc" not in _NC_CACHE:
        _NC_CACHE["nc"] = build_program()
    res = run_bass_kernel_spmd(_NC_CACHE["nc"], in_maps, core_ids=list(range(NCORE)))
    y_p = np.empty((8, SEQ, D), np.float32)
    y_s = np.empty((32, LS, D), np.float32)
    gv_p = np.empty((1, 8, 128, DV), np.float32)
    gv_s = np.empty((1, 32, LS, DV), np.float32)
    cs_p = np.empty((1, 8, HW_, D), np.float32)
    cs_s = np.empty((1, 32, HW_, D), np.float32)
    for c in range(NCORE):
        r = res.results[c]
        y_p[c] = r["y_o"][0:SEQ]
        y_s[c * NSAMP:(c + 1) * NSAMP] = r["y_o"][SEQ:].reshape(NSAMP, LS, D)
        gv_p[0, c] = r["gv_o"][0:128]
        gv_s[0, c * NSAMP:(c + 1) * NSAMP] = r["gv_o"][128:].reshape(NSAMP, LS, DV)
        cs_p[0, c] = r["cs_o"][0:HW_]
        cs_s[0, c * NSAMP:(c + 1) * NSAMP] = r["cs_o"][HW_:].reshape(NSAMP, HW_, D)
    return y_p, y_s, gv_p, gv_s, cs_p, cs_s
```

```python
import numpy as np
from contextlib import ExitStack
import concourse.bass as bass
import concourse.mybir as mybir
from concourse.bass_utils import run_bass_kernel_spmd

F32 = mybir.dt.float32
BF16 = mybir.dt.bfloat16
AF = mybir.ActivationFunctionType
ALU = mybir.AluOpType
AX = mybir.AxisListType

D = 1024
KD = 8
DV = 3072
CV = 24
FF = 3584
NE = 8
SEQ = 4096
NCORE = 8
NSAMP = 4
LS = 64
HW_ = 30
NTAP = 31
ALPHA = float((2.0 * 2) ** 0.25)
EPS = 1e-5
TOK = SEQ + NSAMP * LS
WT = 512
NPT = SEQ // WT
SLOT = 7168
NSLOT = 5
import os as _os
NOSELF = bool(_os.environ.get("NOSELF"))
POOLENG = "pool" if _os.environ.get("USEPOOL") else "dve"


class Res:
    __slots__ = ("w", "r", "const", "excl")

    def __init__(self, const=False, excl=False):
        self.w = None
        self.r = {}
        self.const = const
        self.excl = excl


class Builder:
    def __init__(self, nc, es):
        self.nc = nc
        self.es = es
        self.engs = {"pe": nc.tensor, "act": nc.scalar, "dve": nc.vector, "pool": nc.gpsimd, "sp": nc.sync}
        self.prog = {k: [] for k in self.engs}
        self.cnt = {k: 0 for k in self.engs}
        self.waited = {k: {} for k in self.engs}
        self.S = {k: es.enter_context(nc.semaphore("S_" + k)) for k in ("pe", "act", "dve", "pool")}
        self.dsem = {}
        self.regs = {"pe": es.enter_context(nc.tensor.register("cnt_pe")),
                     "act": es.enter_context(nc.scalar.register("cnt_act")),
                     "dve": es.enter_context(nc.vector.register("cnt_dve")),
                     "sp": es.enter_context(nc.sync.register("cnt_sp"))}
        self.semobj = {}
        for k, s in self.S.items():
            self.semobj[id(s)] = s

    def sb(self, name, shape, dt):
        return self.es.enter_context(self.nc.sbuf_tensor(name, shape, dt))

    def dma_sem(self, name):
        if name not in self.dsem:
            s = self.es.enter_context(self.nc.semaphore("D_" + name))
            self.dsem[name] = [s, 0]
            self.semobj[id(s)] = s
        return self.dsem[name]

    def _collect(self, eng, reads, writes):
        waits = {}
        own = id(self.S[eng]) if eng in self.S else None

        def need(ev):
            if ev is None:
                return
            sid, val = ev
            if sid == own and (eng == "pe" or NOSELF):
                return
            if self.waited[eng].get(sid, 0) >= val:
                return
            if waits.get(sid, 0) < val:
                waits[sid] = val

        for r in reads:
            need(r.w)
        for w in writes:
            need(w.w)
            for sid, val in w.r.items():
                need((sid, val))
        for sid, val in waits.items():
            self.waited[eng][sid] = val
        return [(self.semobj[sid], val) for sid, val in waits.items()]

    def _commit(self, ev, reads, writes):
        for r in reads:
            if not r.const:
                if r.r.get(ev[0], 0) < ev[1]:
                    r.r[ev[0]] = ev[1]
        for w in writes:
            w.w = ev
            w.r = {}

    def op(self, eng, fn, reads=(), writes=(), signal=True):
        if eng != "pe":
            ex = [r for r in reads if r.excl]
            if ex:
                reads = [r for r in reads if not r.excl]
                writes = list(writes) + ex
        waits = self._collect(eng, reads, writes)
        if signal:
            self.cnt[eng] += 1
            ev = (id(self.S[eng]), self.cnt[eng])
        else:
            ev = (id(self.S[eng]), self.cnt[eng] + 1)
        self._commit(ev, reads, writes)
        self.prog[eng].append((waits, fn, (self.S[eng], 1) if signal else None))

    def dma(self, q, out, in_, reads, writes, semname, **kw):
        waits = self._collect(q, reads, writes)
        ds = self.dma_sem(semname)
        ds[1] += 16
        ev = (id(ds[0]), ds[1])
        self._commit(ev, reads, writes)
        self.prog[q].append((waits, lambda e: e.dma_start(out=out, in_=in_, **kw), (ds[0], 16)))

    def raw(self, eng, fn, reads=()):
        waits = self._collect(eng, reads, [])
        self.prog[eng].append((waits, fn, None))

    def if_begin(self, engs, ei, thresh):
        self._if = {}
        self._if_ds = {name: v[1] for name, v in self.dsem.items()}
        for eng in engs:
            entry = ["IF", ei, thresh, 0, []]
            self.prog[eng].append(entry)
            self._if[eng] = (entry, self.cnt[eng], dict(self.waited[eng]))

    def if_end(self):
        for eng, (entry, c0, w0) in self._if.items():
            entry[3] = self.cnt[eng] - c0
            if eng == "sp":
                for name, (sem, tot) in self.dsem.items():
                    pre = self._if_ds.get(name, 0)
                    if tot > pre:
                        entry[4].append((sem, pre, tot - pre))
            self.prog[eng].append(["ENDIF"])
            self.waited[eng] = w0
        self._if = {}

    def barrier(self, engs):
        for eng in engs:
            waits = []
            for k, sem in self.S.items():
                if self.cnt[k] > 0 and not (eng == "pe" and k == "pe"):
                    if self.waited[eng].get(id(sem), 0) < self.cnt[k]:
                        self.waited[eng][id(sem)] = self.cnt[k]
                        waits.append((sem, self.cnt[k]))
            for name, (sem, tot) in self.dsem.items():
                if tot > 0 and self.waited[eng].get(id(sem), 0) < tot:
                    self.waited[eng][id(sem)] = tot
                    waits.append((sem, tot))
            self.prog[eng].append((waits, None, None))

    def final_wait(self, q):
        waits = []
        for name, (s, tot) in self.dsem.items():
            if tot > 0:
                waits.append((s, tot))
        self.prog[q].append((waits, None, None))

    def emit(self):
        nc = self.nc
        block = self.es.enter_context(nc.Block())

        def replay(key, e):
            prog = self.prog[key]
            i = 0
            n = len(prog)

            def run(entry):
                waits, fn, inc = entry
                for s_, v in waits:
                    e.wait_ge(s_, v)
                if fn is None:
                    return
                ins = fn(e)
                if inc is not None:
                    ins.then_inc(inc[0], inc[1])

            while i < n:
                entry = prog[i]
                if entry[0] == "IF":
                    _, ei, thresh, k, dcomp = entry
                    j = i + 1
                    while prog[j][0] != "ENDIF":
                        j += 1
                    reg = self.regs[key]
                    with e.If_lt(reg, thresh + 1):
                        if k > 0:
                            e.drain().then_inc(self.S[key], k)
                        for sem_, pre_, delta_ in dcomp:
                            e.wait_ge(sem_, pre_)
                            e.sem_inc(sem_, delta_)
                    with e.Else():
                        for ent in prog[i + 1:j]:
                            run(ent)
                    i = j + 1
                    continue
                run(entry)
                i += 1

        @block.tensor
        def _(e):
            replay("pe", e)

        @block.scalar
        def _(e):
            replay("act", e)

        @block.vector
        def _(e):
            replay("dve", e)

        @block.gpsimd
        def _(e):
            replay("pool", e)

        @block.sync
        def _(e):
            replay("sp", e)


def build_program(dbg=None):
    nc = bass.Bass("TRN2", target_bir_lowering=False)
    es = ExitStack()
    with es:
        _build(nc, es, dbg or {})
    return nc


def _build(nc, es, dbg):
    B = Builder(nc, es)
    op, dma, sb = B.op, B.dma, B.sb

    def din(name, shape):
        return nc.dram_tensor(name, shape, F32, kind="ExternalInput").ap()

    def dout(name, shape):
        return nc.dram_tensor(name, shape, F32, kind="ExternalOutput").ap()

    xin = din("xin", [TOK, D])
    cache = din("cache", [NSAMP, HW_, D])
    w_in = din("gm_w_in", [D, 2 * DV])
    b_in = din("gm_b_in", [1, 2 * DV])
    lnv_g = din("gm_lnv_g", [1, DV])
    lnv_b = din("gm_lnv_b", [1, DV])
    w_s = din("gm_w_s", [8, 128, 128])
    b_s = din("gm_b_s", [8, 128])
    w_out = din("gm_w_out", [DV, D])
    b_out = din("gm_b_out", [1, D])
    w_pw1 = din("cv_w_pw1", [D, 2 * D])
    b_pw1 = din("cv_b_pw1", [1, 2 * D])
    w_dw = din("cv_w_dw", [NTAP, D])
    b_dw = din("cv_b_dw", [1, D])
    cln_g = din("cv_ln_g", [1, D])
    cln_b = din("cv_ln_b", [1, D])
    w_pw2 = din("cv_w_pw2", [D, D])
    b_pw2 = din("cv_b_pw2", [1, D])
    ffg = din("ff_w_gate", [D, FF])
    ffu = din("ff_w_up", [D, FF])
    ffd = din("ff_w_down", [FF, D])
    w_r = din("moe_w_router", [D, NE])
    b_r = din("moe_b_router", [1, NE])
    mog = din("moe_w_gate", [NE, D, FF])
    mou = din("moe_w_up", [NE, D, FF])
    mod = din("moe_w_down", [NE, FF, D])
    ln_g = din("ln_g", [1, 4 * D])
    ln_b = din("ln_b", [1, 4 * D])
    y_o = dout("y_o", [TOK, D])
    gv_o = dout("gv_o", [128 + NSAMP * LS, DV])
    cs_o = dout("cs_o", [HW_ * (1 + NSAMP), D])

    XF = sb("XF", [128, KD, WT], F32)
    XB = sb("XB", [128, KD, WT], BF16)
    RING = [sb("ring%d" % i, [128, SLOT], BF16) for i in range(NSLOT)]
    PH = sb("PH", [128, 24576], BF16)
    XS0 = sb("XS0", [128, D], F32)
    XS = [XS0, XS0]
    BROWF = PH[0:1, 0:4096].bitcast(F32).rearrange("o (a n) -> o a n", a=2)
    BROWT = PH[0:1, 4096:8192].bitcast(F32).rearrange("o (a n) -> o a n", a=2)
    WSA = PH[:, 8192:10240].bitcast(F32).rearrange("p (g j) -> p g j", g=8)
    IOTA_I = PH[:, 10240:11264].bitcast(mybir.dt.int32)
    UTRI_F = PH[:, 11264:11520].bitcast(F32)
    LNS = sb("LNS", [128, 13312], BF16)

    def lnv(off, n, f32=True):
        v = LNS[:, off:off + n]
        return v.bitcast(F32) if f32 else v
    MEAN, VAR, RSTD = lnv(0, 1024), lnv(1024, 1024), lnv(2048, 1024)
    T1 = [lnv(3072 + i * 1024, 1024) for i in range(2)]
    T2 = [lnv(5120 + i * 1024, 1024) for i in range(2)]
    SQ = [lnv(7168 + i * 512, 512, False) for i in range(2)]
    ZB = [lnv(8192 + i * 512, 512, False) for i in range(2)]
    identF = sb("identF", [128, 128], F32)
    identB = sb("identB", [128, 128], BF16)
    onesB = sb("onesB", [128, 128], BF16)
    WST = [sb("WST%d" % i, [128, 8, 128], BF16) for i in range(2)]
    BROW = sb("BROW", [1, 4, 1024], BF16)
    SEL = sb("SEL", [8, NE, 128], F32)
    WR = sb("WR", [128, KD, NE], F32)
    BR = sb("BR", [128, NE], F32)
    GTS = sb("GTS", [8, WT], F32)
    VB_IN = sb("VB_IN", [128, 48], F32)
    VLNVG = sb("VLNVG", [128, CV], F32)
    VLNVB = sb("VLNVB", [128, CV], F32)
    VBOUT = sb("VBOUT", [128, KD], F32)
    VBPW1 = sb("VBPW1", [128, 16], F32)
    WDW = sb("WDW", [128, KD, NTAP], F32)
    VBDW = sb("VBDW", [128, KD], F32)
    VCLG = sb("VCLG", [128, KD], F32)
    VCLB = sb("VCLB", [128, KD], F32)
    VBPW2 = sb("VBPW2", [128, KD], F32)
    VLNG = sb("VLNG", [128, 32], F32)
    VLNB = sb("VLNB", [128, 32], F32)
    VLNGA = sb("VLNGA", [128, 32], F32)
    VLNBA = sb("VLNBA", [128, 32], F32)
    EPST = sb("EPST", [128, 1], F32)
    HIST = sb("HIST", [128, KD, HW_], BF16)
    GL = sb("GL", [128, KD, NSAMP, HW_], F32)
    RT = {n: sb("RT_" + n, [128, NE], F32) for n in ("L", "M1", "L2", "M2", "G")}
    RS = {n: sb("RS_" + n, [128, 1], F32) for n in ("m1", "m2", "d", "e", "den", "w1", "w2")}
    I32 = mybir.dt.int32
    CAP = int(dbg.get("cap", 192))
    PM = LNS
    IOTA_F = sb("IOTA_F", [128, WT], F32)
    SLOT_I = sb("SLOT_I", [128, 4], I32)
    SLOTID = sb("SLOTID", [128, 4], F32)
    UTRI = sb("UTRI", [128, 128], BF16)
    MSKF = sb("MSKF", [128, 4, NE], F32)
    MSK = sb("MSK", [128, 4, NE], BF16)
    POS = sb("POS", [128, 4, NE], F32)
    POST = sb("POST", [8, WT], F32)
    CNT = sb("CNT", [1, NE], I32)
    CNTM = sb("CNTM", [1, 1], I32)
    CNTMF = sb("CNTMF", [1, 1], F32)
    PS = [es.enter_context(nc.psum_tensor("ps%d" % i, [128, 512], F32)) for i in range(8)]

    R = {}

    def res(name, const=False):
        if name not in R:
            R[name] = Res(const)
        return R[name]

    rPS = [res("ps%d" % i) for i in range(8)]
    for r_ in rPS:
        r_.excl = True
    rRING = [res("ring%d" % i) for i in range(NSLOT)]
    rXFm = [res("XF%d" % m) for m in range(KD)]
    rXB = res("XB")
    rXS = [res("XS0"), res("XS0")]
    rC = res("consts")
    rCd, rWSA, rCp, rCv = res("Cd"), res("WSA"), res("Cp"), res("Cv")

    def phv(off, n):
        return PH[:, off:off + n]

    VNT = phv(0, 12288).rearrange("p (c t) -> p c t", c=CV)
    OT = VNT
    VN = phv(12288, 12288).rearrange("p (b c) -> p b c", b=4)
    GTB = phv(0, 4336)
    DG = [phv(4336 + i * 3968, 3968).rearrange("p (k c) -> p k c", k=NTAP) for i in range(2)]
    ST = phv(12272, 4096).rearrange("p (m t) -> p m t", m=KD)
    YC = phv(16384, 8192).bitcast(F32).rearrange("p (m t) -> p m t", m=KD)
    HT = [phv(i * 3584, 3584).rearrange("p (f t) -> p f t", f=7) for i in range(2)]
    SS = [phv(7168 + i * 512, 512) for i in range(2)]
    TT = [phv(8192 + i * 512, 512) for i in range(2)]
    GBC = [phv(9216 + i * 1024, 1024).bitcast(F32) for i in range(2)]
    rVNT, rVN, rGTB, rST, rYC = res("VNT"), res("VN"), res("GTB"), res("ST"), res("YC")
    rVNTc = [res("VNTc%d" % c) for c in range(CV)]
    rDG = [res("DG0"), res("DG1")]
    rDGk = [[res("DG%d_%d" % (i, k)) for k in range(NTAP)] for i in range(2)]
    rHT = [res("HT0"), res("HT1")]
    rSS = [res("SS0"), res("SS1")]
    rTT = [res("TT0"), res("TT1")]
    rGBC = [res("GBC0"), res("GBC1")]
    rT1 = [res("T1_0"), res("T1_1")]
    rT2 = [res("T2_0"), res("T2_1")]
    rSQ = [res("SQ0"), res("SQ1")]
    rZB = [res("ZB0"), res("ZB1")]
    rSTAT = res("STAT")
    rHIST, rGL, rGTS, rRT = res("HIST"), res("GL"), res("GTS"), res("RT")
    WA = CAP
    THR = int(dbg.get("thr", CAP))
    if CAP == 256:
        A_BLK = [(0, 0, 128, 0, 128), (1, 128, 128, 0, 128)]
        B_BLK = [(2, 0, 128, 0, 128), (3, 128, 128, 0, 128)]
        B_C0 = 256
    else:
        assert CAP == 192
        A_BLK = [(0, 0, 128, 0, 128), (1, 128, 64, 0, 64)]
        B_BLK = [(1, 0, 128, 64, 64), (2, 128, 128, 0, 128), (3, 256, 128, 0, 128)]
        B_C0 = 128
    WB = WT - B_C0
    B_SB = [1]
    HTA = [phv(i * 1792, 7 * WA).rearrange("p (f t) -> p f t", f=7) for i in range(2)]
    HTBv = phv(3584, 7 * WB).rearrange("p (f t) -> p f t", f=7)
    SSA = [phv(6272 + i * 384, 384) for i in range(2)]
    XTOK = phv(7040, 4096).rearrange("p (b c) -> p b c", b=4)
    PE_ = [phv(11136 + i * 512, 512) for i in range(4)]
    Q_ = [phv(13184 + i * 512, 512) for i in range(4)]
    XG = [phv(15232 + i * 4096, 4096).rearrange("p (k t) -> p k t", k=KD) for i in range(2)]
    GBC1 = phv(23424, 1024).bitcast(F32)
    ACC = [PM[:, i * 2048:(i + 1) * 2048].bitcast(F32) for i in range(4)]
    ACCB = [PM[:, 8192 + i * 1024: 8192 + (i + 1) * 1024] for i in range(4)]
    POSBC = PM[:, 12288:13312].bitcast(F32)
    rXTOK, rPE_, rGBC1, rPOSBC, rHTB = res("XTOK"), res("PE_"), res("GBC1"), res("POSBC"), res("HTB")
    rQ = [res("Q%d" % i) for i in range(4)]
    rXG = [res("XG0"), res("XG1")]
    rHTA = [res("HTA0"), res("HTA1")]
    rSSA = [res("SSA0"), res("SSA1")]
    rACC = [res("ACC%d" % i) for i in range(4)]
    rACCB = [res("ACCB%d" % i) for i in range(4)]
    rMSK, rPOS, rCNT, rPOST = res("MSK"), res("POS"), res("CNT"), res("POST")

    def vec_load(dst, src_row, n):
        dma("sp", dst, src_row.rearrange("o (c p) -> p (o c)", p=128), [], [rCd], "const",
            allow_slow_non_contiguous=True)

    skip = dbg.get("skip", ())
    if 'vec' not in skip:
        vec_load(VB_IN[:], b_in, 48)
        vec_load(VLNVG[:], lnv_g, CV)
        vec_load(VLNVB[:], lnv_b, CV)
        vec_load(VBOUT[:], b_out, KD)
        vec_load(VBPW1[:], b_pw1, 16)
        vec_load(VBDW[:], b_dw, KD)
        vec_load(VCLG[:], cln_g, KD)
        vec_load(VCLB[:], cln_b, KD)
        vec_load(VBPW2[:], b_pw2, KD)
        vec_load(VLNG[:], ln_g, 32)
        vec_load(VLNB[:], ln_b, 32)
    if 'wdw' not in skip:
        for k in range(NTAP):
            dma("sp", WDW[:, :, k], w_dw[k:k + 1, :].rearrange("o (c p) -> p (o c)", p=128), [], [rCd], "const",
                allow_slow_non_contiguous=True)
    if 'wr' not in skip:
        dma("sp", WR[:], w_r.rearrange("(k p) e -> p k e", p=128), [], [rCd], "const")
    if 'br' not in skip:
        dma("sp", BR[:], b_r.partition_broadcast(128), [], [rCd], "const")
    if 'wsa' not in skip:
        dma("sp", WSA[:], w_s.rearrange("g i j -> i g j"), [], [rWSA], "wsa")
    if 'browf' not in skip:
        dma("sp", BROWF[:, 0, :], b_s.rearrange("(o g) i -> o (g i)", o=1), [], [rCd], "const")
        for h in range(2):
            dma("sp", BROWF[:, 1, :].rearrange("o (g i) -> o g i", g=8)[:, :, h * 64:(h + 1) * 64],
                b_s[:, 0:64].rearrange("(o g) i -> o g i", o=1), [], [rCd], "const")
    if 'pool' not in skip:
        op("pool", lambda e: e.memset(identF[:], 0.0), [], [rCp])
        op("pool", lambda e: e.affine_select(out=identF[:], in_=identF[:], compare_op=ALU.not_equal, fill=1.0,
                                             base=0, pattern=[[-1, 128]], channel_multiplier=1), [], [rCp])
    if 'sel' not in skip:
        op("pool", lambda e: e.memset(SEL[:], 0.0), [], [rCp])
        for ee in range(NE):
            op("pool", lambda e, ee=ee: e.affine_select(out=SEL[:, ee, :], in_=SEL[:, ee, :], compare_op=ALU.not_equal,
                                                        fill=1.0, base=-ee, pattern=[[0, 128]], channel_multiplier=1),
               [], [rCp])
    if 'pool2' not in skip:
        op("pool", lambda e: e.memset(EPST[:], EPS), [], [rCp])
        op("pool", lambda e: e.memset(onesB[:], 1.0), [], [rCp])
        op("pool", lambda e: e.memset(HIST[:], 0.0), [], [rHIST])
        op("pool", lambda e: e.iota(out=IOTA_I[:], pattern=[[1, WT]], base=0, channel_multiplier=0), [], [rCp])
        op("pool", lambda e: e.iota(out=SLOT_I[:], pattern=[[128, 4]], base=0, channel_multiplier=1), [], [rCp])
        op("pool", lambda e: e.memset(UTRI_F[:], 1.0), [], [rCp])
        op("pool", lambda e: e.affine_select(out=UTRI_F[:], in_=UTRI_F[:], compare_op=ALU.is_gt, fill=0.0, base=0,
                                             pattern=[[1, 128]], channel_multiplier=-1), [], [rCp])
        op("dve", lambda e: e.tensor_copy(out=IOTA_F[:], in_=IOTA_I[:]), [rCp], [rCv])
        op("dve", lambda e: e.tensor_copy(out=SLOTID[:], in_=SLOT_I[:]), [rCp], [rCv])
        op("dve", lambda e: e.tensor_copy(out=UTRI[:], in_=UTRI_F[:]), [rCp], [rCv])
    if 'identb' not in skip:
        op("dve", lambda e: e.tensor_copy(out=identB[:], in_=identF[:]), [rCp], [rCv])
    if 'lnga' not in skip:
        op("dve", lambda e: e.tensor_scalar(out=VLNGA[:], in0=VLNG[:], scalar1=ALPHA, scalar2=None, op0=ALU.mult), [rCd], [rCv])
        op("dve", lambda e: e.tensor_scalar(out=VLNBA[:], in0=VLNB[:], scalar1=ALPHA, scalar2=None, op0=ALU.mult), [rCd], [rCv])
    if 'brow' not in skip:
        op("dve", lambda e: e.tensor_copy(out=BROW[:, 0, :], in_=BROWF[:, 0, :]), [rCd, rCv], [rCv])
        op("dve", lambda e: e.tensor_copy(out=BROW[:, 2, :], in_=BROWF[:, 1, :]), [rCd, rCv], [rCv])
        op("dve", lambda e: e.tensor_tensor(out=BROWT[:, 0, :], in0=BROWF[:, 0, :], in1=BROW[:, 0, :], op=ALU.subtract), [rCd, rCv], [rCv])
        op("dve", lambda e: e.tensor_tensor(out=BROWT[:, 1, :], in0=BROWF[:, 1, :], in1=BROW[:, 2, :], op=ALU.subtract), [rCd, rCv], [rCv])
        op("dve", lambda e: e.tensor_copy(out=BROW[:, 1, :], in_=BROWT[:, 0, :]), [rCd, rCv], [rCv])
        op("dve", lambda e: e.tensor_copy(out=BROW[:, 3, :], in_=BROWT[:, 1, :]), [rCd, rCv], [rCv])
    if 'wstp' not in skip:
        op("dve", lambda e: e.memset(WST[0][:], 0.0), [], [rCv])
        op("dve", lambda e: e.memset(WST[1][:], 0.0), [], [rCv])
        for g in range(8):
            bank = g % 2
            op("pe", lambda e, g=g, bank=bank: e.transpose(out=PS[bank][:, 0:128], in_=WSA[:, g, :], identity=identF[:]),
               [rWSA, rCp], [rPS[bank]])
            op("dve", lambda e, g=g, bank=bank: e.tensor_copy(out=WST[0][0:64, g, :], in_=PS[bank][0:64, 0:128]),
               [rPS[bank]], [rCv])
            op("dve", lambda e, g=g, bank=bank: e.tensor_copy(out=WST[0][64:128, g, 64:128], in_=PS[bank][64:128, 64:128]),
               [rPS[bank]], [rCv])
    if 'wsts' not in skip:
        op("dve", lambda e: e.memset(WSA[:], 0.0), [], [rWSA])
        dma("sp", WSA[0:64, :, 0:64], w_s[:, 0:64, 0:64].rearrange("g i j -> i g j"), [], [rWSA], "wsa")
        dma("sp", WSA[64:128, :, 64:128], w_s[:, 0:64, 0:64].rearrange("g i j -> i g j"), [], [rWSA], "wsa")
        for g in range(8):
            bank = g % 2
            op("pe", lambda e, g=g, bank=bank: e.transpose(out=PS[bank][:, 0:128], in_=WSA[:, g, :], identity=identF[:]),
               [rWSA, rCp], [rPS[bank]])
            op("dve", lambda e, g=g, bank=bank: e.tensor_copy(out=WST[1][:, g, :], in_=PS[bank][:, 0:128]),
               [rPS[bank]], [rCv])
    B.barrier(["pe", "act", "dve", "pool"])
    rC.w = None
    rC.r = {}
    rC.const = True

    ring_state = {"i": 0, "piece": 0, "first": True}
    NPIECE = 126
    WSCR = nc.dram_tensor("wscr", [NPIECE, 128, SLOT], BF16, kind="Internal").ap()
    rSCR = [Res() for _ in range(NPIECE)]

    def wload(src3, shape):
        i = ring_state["i"] % NSLOT
        ring_state["i"] += 1
        j = ring_state["piece"]
        ring_state["piece"] += 1
        a, b = shape
        flat = RING[i][:, 0:a * b]
        view = flat.rearrange("p (a b) -> p a b", a=a)
        if (ring_state["first"] and not ring_state.get("force_hw")) or dbg.get("noscr"):
            dma("pool", view, src3, [], [rRING[i]], "rgs%d" % i)
            if not dbg.get("noscr"):
                dma("sp", WSCR[j, :, 0:a * b], flat, [rRING[i]], [rSCR[j]], "wst%d" % i)
        else:
            dma("sp", flat, WSCR[j, :, 0:a * b], [rSCR[j]], [rRING[i]], "rgh%d" % i)
        return view, rRING[i]

    w_in_v = w_in.rearrange("(k p) n -> p k n", p=128)
    w_out_v = w_out.rearrange("(c p) n -> p c n", p=128)
    w_pw1_v = w_pw1.rearrange("(k p) n -> p k n", p=128)
    w_pw2_v = w_pw2.rearrange("(k p) n -> p k n", p=128)

    def layer_norm(zs, W, nch, srcs_bf=None, outs=None, fence=()):
        inv = 1.0 / (nch * 128)
        for m in range(nch):
            z, rz = zs[m]
            s = m % 2
            fz = list(fence) if m == 0 else []
            op("act", lambda e, z=z, s=s: e.activation(out=SQ[s][:, 0:W], in_=z, func=AF.Square), [rz], [rSQ[s]] + fz)
            if srcs_bf is None:
                op("dve", lambda e, z=z, s=s: e.tensor_copy(out=ZB[s][:, 0:W], in_=z), [rz], [rZB[s]] + fz)
                zb, rzb = ZB[s][:, 0:W], rZB[s]
            else:
                zb, rzb = srcs_bf[m]
            op("pe", lambda e, zb=zb, m=m: e.matmul(PS[6][:, 0:W], lhsT=onesB[:], rhs=zb, start=(m == 0), stop=(m == nch - 1)),
               [rzb, rC], [rPS[6]], signal=(m == nch - 1))
            op("pe", lambda e, s=s, m=m: e.matmul(PS[7][:, 0:W], lhsT=onesB[:], rhs=SQ[s][:, 0:W], start=(m == 0), stop=(m == nch - 1)),
               [rSQ[s], rC], [rPS[7]], signal=True)
        op("dve", lambda e: e.tensor_scalar(out=MEAN[:, 0:W], in0=PS[6][:, 0:W], scalar1=inv, scalar2=None, op0=ALU.mult),
           [rPS[6]], [rSTAT])
        op("dve", lambda e: e.tensor_tensor(out=VAR[:, 0:W], in0=MEAN[:, 0:W], in1=MEAN[:, 0:W], op=ALU.mult), [rSTAT], [rSTAT])
        op("dve", lambda e: e.scalar_tensor_tensor(out=VAR[:, 0:W], in0=PS[7][:, 0:W], scalar=inv, in1=VAR[:, 0:W],
                                                   op0=ALU.mult, op1=ALU.subtract), [rPS[7], rSTAT], [rSTAT])
        op("act", lambda e: e.activation(out=RSTD[:, 0:W], in_=VAR[:, 0:W], func=AF.Sqrt, bias=EPST[:], scale=1.0),
           [rSTAT, rC], [rSTAT])
        op("dve", lambda e: e.reciprocal(out=RSTD[:, 0:W], in_=RSTD[:, 0:W]), [rSTAT], [rSTAT])
        for m in range(nch):
            z, rz = zs[m]
            s = m % 2
            op("dve", lambda e, z=z, s=s: e.tensor_tensor(out=T1[s][:, 0:W], in0=z, in1=MEAN[:, 0:W], op=ALU.subtract),
               [rz, rSTAT], [rT1[s]])
            op(POOLENG, lambda e, s=s: e.tensor_tensor(out=T2[s][:, 0:W], in0=T1[s][:, 0:W], in1=RSTD[:, 0:W], op=ALU.mult),
               [rT1[s], rSTAT], [rT2[s]])
            outs(m, T2[s][:, 0:W], rT2[s])

    def deepnorm_ln(W, lnidx, scaled, make_xb=True):
        gsrc, bsrc = (VLNGA, VLNBA) if scaled else (VLNG, VLNB)

        def outs(m, t2, rt2):
            col = lnidx * 8 + m
            op("act", lambda e: e.activation(out=XF[:, m, 0:W], in_=t2, func=AF.Identity,
                                             bias=bsrc[:, col:col + 1], scale=gsrc[:, col:col + 1]), [rt2, rC], [rXFm[m]])
            if make_xb:
                if POOLENG == "pool":
                    op("pool", lambda e: e.tensor_scalar(out=XB[:, m, 0:W], in0=t2, scalar1=VLNG[:, col:col + 1],
                                                         scalar2=VLNB[:, col:col + 1], op0=ALU.mult, op1=ALU.add), [rt2, rC], [rXB])
                else:
                    op("act", lambda e: e.activation(out=XB[:, m, 0:W], in_=t2, func=AF.Identity,
                                                     bias=VLNB[:, col:col + 1], scale=VLNG[:, col:col + 1]), [rt2, rC], [rXB])
        layer_norm([(XF[:, m, 0:W], rXFm[m]) for m in range(KD)], W, KD, None, outs,
                   fence=(rACC + rACCB + [rPOSBC]) if lnidx == 3 else ())

    def load_tile(tok0, W):
        nb = W // 128
        LT = _os.environ.get("LT", "dpav")
        for b in range(nb):
            s = b % 2
            if "d" in LT:
                dma("sp", XS[s][:], xin[tok0 + b * 128: tok0 + (b + 1) * 128, :], [], [rXS[s]], "xs%d" % s)
            if "p" in LT:
                for m in range(KD):
                    bank = m // 4
                    op("pe", lambda e, m=m, s=s, bank=bank: e.transpose(out=PS[bank][:, (m % 4) * 128:(m % 4 + 1) * 128],
                                                                        in_=XS[s][:, m * 128:(m + 1) * 128], identity=identF[:]),
                       [rXS[s], rC], [rPS[bank]], signal=(m % 4 == 3))
            for h in range(2):
                src = PS[h][:].rearrange("p (a t) -> p a t", a=4)
                if "a" in LT:
                    op("act", lambda e, h=h, b=b, src=src: e.mul(out=XF[:, 4 * h:4 * h + 4, b * 128:(b + 1) * 128], in_=src, mul=ALPHA),
                       [rPS[h]], rXFm[4 * h:4 * h + 4])
                if "v" in LT:
                    op("dve", lambda e, h=h, b=b, src=src: e.tensor_copy(out=XB[:, 4 * h:4 * h + 4, b * 128:(b + 1) * 128], in_=src),
                       [rPS[h]] + ([rXFm[4 * h]] if _os.environ.get("SER") else []), [rXB])

    def store_tile(tok0, W):
        nb = W // 128
        for b in range(nb):
            s = b % 2
            for m in range(KD):
                bank = 2 + m // 4
                op("pe", lambda e, m=m, b=b, bank=bank: e.transpose(out=PS[bank][:, (m % 4) * 128:(m % 4 + 1) * 128],
                                                                    in_=XF[:, m, b * 128:(b + 1) * 128], identity=identF[:]),
                   [rXFm[m], rC], [rPS[bank]], signal=(m % 4 == 3))
            op("act", lambda e, s=s: e.copy(out=XS[s][:, 0:512], in_=PS[2][:]), [rPS[2]], [rXS[s]])
            op("dve", lambda e, s=s: e.tensor_copy(out=XS[s][:, 512:1024], in_=PS[3][:]), [rPS[3]], [rXS[s]])
            dma("sp", y_o[tok0 + b * 128: tok0 + (b + 1) * 128, :], XS[s][:], [rXS[s]], [], "ys%d" % s)

    def gmlp(W, sample, gv_rows):
        nb = W // 128
        wst = WST[1 if sample else 0]
        bro = 2 if sample else 0
        vps = 0
        for pc in range(4):
            wv, rw = wload(w_in_v[:, :, DV + pc * 768: DV + (pc + 1) * 768], (KD, 768))
            for cc in range(6):
                c = pc * 6 + cc
                bank = vps % 2
                vps += 1
                for k in range(KD):
                    op("pe", lambda e, k=k, cc=cc, bank=bank, wv=wv: e.matmul(PS[bank][:, 0:W], lhsT=wv[:, k, cc * 128:(cc + 1) * 128],
                                                                               rhs=XB[:, k, 0:W], start=(k == 0), stop=(k == KD - 1)),
                       [rw, rXB], [rPS[bank]], signal=(k == KD - 1))
                op("act", lambda e, c=c, bank=bank: e.activation(out=VNT[:, c, 0:W], in_=PS[bank][:, 0:W], func=AF.Gelu,
                                                                 bias=VB_IN[:, 24 + c:25 + c], scale=1.0),
                   [rPS[bank], rC], [rVNTc[c]])

        def outs(c, t2, rt2):
            op("act", lambda e: e.activation(out=VNT[:, c, 0:W], in_=t2, func=AF.Identity,
                                             bias=VLNVB[:, c:c + 1], scale=VLNVG[:, c:c + 1]), [rt2, rC], [rVNTc[c]])
        zs = [(VNT[:, c, 0:W], rVNTc[c]) for c in range(CV)]
        layer_norm(zs, W, CV, zs, outs)
        tb = 0
        for b in range(nb):
            for c8 in range(3):
                bank = 2 + tb % 2
                tb += 1
                psb = PS[bank][:].bitcast(BF16)
                for cc in range(8):
                    c = c8 * 8 + cc
                    op("pe", lambda e, c=c, cc=cc, b=b, psb=psb: e.transpose(out=psb[:, cc * 128:(cc + 1) * 128],
                                                                               in_=VNT[:, c, b * 128:(b + 1) * 128], identity=identB[:]),
                       [rVNTc[c], rC], [rPS[bank]], signal=(cc == 7))
                eng = "act" if c8 == 1 else "dve"
                if eng == "act":
                    op("act", lambda e, b=b, c8=c8, psb=psb: e.copy(out=VN[:, b, c8 * 1024:(c8 + 1) * 1024], in_=psb), [rPS[bank]], [rVN])
                else:
                    op("dve", lambda e, b=b, c8=c8, psb=psb: e.tensor_copy(out=VN[:, b, c8 * 1024:(c8 + 1) * 1024], in_=psb), [rPS[bank]], [rVN])
            if b in gv_rows:
                r0 = gv_rows[b]
                dma("pool", gv_o[r0:r0 + 128, :], VN[:, b, :], [rVN], [], "gv")
        ups = 0
        for pc in range(4):
            wu, rw = wload(w_in_v[:, :, pc * 768:(pc + 1) * 768], (KD, 768))
            for cc in range(6):
                c = pc * 6 + cc
                g = c // 3
                bank = ups % 2
                mb = 2 + ups % 2
                s = ups % 2
                ups += 1
                for k in range(KD):
                    op("pe", lambda e, k=k, cc=cc, bank=bank, wu=wu: e.matmul(PS[bank][:, 0:W], lhsT=wu[:, k, cc * 128:(cc + 1) * 128],
                                                                               rhs=XB[:, k, 0:W], start=(k == 0), stop=(k == KD - 1)),
                       [rw, rXB], [rPS[bank]], signal=(k == KD - 1))
                op("act", lambda e, c=c, bank=bank, s=s: e.activation(out=ZB[s][:, 0:W], in_=PS[bank][:, 0:W], func=AF.Gelu,
                                                                      bias=VB_IN[:, c:c + 1], scale=1.0),
                   [rPS[bank], rC], [rZB[s]])
                for b in range(nb):
                    cols = slice(b * 128, (b + 1) * 128)
                    op("pe", lambda e, b=b, c=c, g=g, mb=mb, cols=cols: e.matmul(PS[mb][:, cols], lhsT=VN[:, b, c * 128:(c + 1) * 128],
                                                                                  rhs=wst[:, g, :], start=True, stop=False),
                       [rVN, rC], [rPS[mb]], signal=False)
                    op("pe", lambda e, g=g, mb=mb, cols=cols: e.matmul(PS[mb][:, cols], lhsT=onesB[0:1, :],
                                                                       rhs=BROW[0:1, bro, g * 128:(g + 1) * 128], start=False, stop=False),
                       [rC], [rPS[mb]], signal=False)
                    op("pe", lambda e, g=g, mb=mb, cols=cols: e.matmul(PS[mb][:, cols], lhsT=onesB[0:1, :],
                                                                       rhs=BROW[0:1, bro + 1, g * 128:(g + 1) * 128], start=False, stop=True),
                       [rC], [rPS[mb]], signal=(b == nb - 1))
                op("dve", lambda e, c=c, mb=mb, s=s: e.tensor_tensor(out=OT[:, c, 0:W], in0=ZB[s][:, 0:W], in1=PS[mb][:, 0:W], op=ALU.mult),
                   [rZB[s], rPS[mb]], [rVNTc[c]])
        for pc in range(4):
            wo, rw = wload(w_out_v[:, :, pc * 256:(pc + 1) * 256], (CV, 256))
            for mm in range(2):
                m = pc * 2 + mm
                bank = 4 + m % 2
                for c in range(CV):
                    op("pe", lambda e, c=c, mm=mm, bank=bank, wo=wo: e.matmul(PS[bank][:, 0:W], lhsT=wo[:, c, mm * 128:(mm + 1) * 128],
                                                                               rhs=OT[:, c, 0:W], start=(c == 0), stop=(c == CV - 1)),
                       [rw, rVNTc[c]], [rPS[bank]], signal=(c == CV - 1))
                op("dve", lambda e, m=m, bank=bank: e.scalar_tensor_tensor(out=XF[:, m, 0:W], in0=PS[bank][:, 0:W], scalar=VBOUT[:, m:m + 1],
                                                                           in1=XF[:, m, 0:W], op0=ALU.add, op1=ALU.add),
                   [rPS[bank], rC, rXFm[m]], [rXFm[m]])

    def experts(W, wg_list, gated):
        cnt = 0
        for ei, (gv, uv, dv) in enumerate(wg_list):
            if gated:
                gs = ei % 2
                op("pe", lambda e, ei=ei: e.matmul(PS[6][:, 0:W], lhsT=SEL[:, ei, :], rhs=GTS[0:8, 0:W], start=True, stop=True),
                   [rC, rGTS], [rPS[6]])
                op("act", lambda e, gs=gs: e.copy(out=GBC[gs][:, 0:W], in_=PS[6][:, 0:W]), [rPS[6]], [rGBC[gs]])
            for gi in range(4):
                wgp, rwg = wload(gv[:, :, gi * 896:(gi + 1) * 896], (KD, 896))
                wup, rwu = wload(uv[:, :, gi * 896:(gi + 1) * 896], (KD, 896))
                wdp, rwd = wload(dv[:, gi * 7:(gi + 1) * 7, :], (7, D))
                hs = cnt % 2
                cnt += 1
                for fc in range(7):
                    s = fc % 2
                    gb, ub = fc % 2, 2 + fc % 2
                    for k in range(KD):
                        op("pe", lambda e, k=k, fc=fc, gb=gb, wgp=wgp: e.matmul(PS[gb][:, 0:W], lhsT=wgp[:, k, fc * 128:(fc + 1) * 128],
                                                                                 rhs=XB[:, k, 0:W], start=(k == 0), stop=(k == KD - 1)),
                           [rwg, rXB], [rPS[gb]], signal=(k == KD - 1))
                    for k in range(KD):
                        op("pe", lambda e, k=k, fc=fc, ub=ub, wup=wup: e.matmul(PS[ub][:, 0:W], lhsT=wup[:, k, fc * 128:(fc + 1) * 128],
                                                                                 rhs=XB[:, k, 0:W], start=(k == 0), stop=(k == KD - 1)),
                           [rwu, rXB], [rPS[ub]], signal=(k == KD - 1))
                    op("act", lambda e, gb=gb, s=s: e.activation(out=SS[s][:, 0:W], in_=PS[gb][:, 0:W], func=AF.Silu), [rPS[gb]], [rSS[s]])
                    if gated:
                        op("dve", lambda e, ub=ub, s=s: e.tensor_tensor(out=TT[s][:, 0:W], in0=SS[s][:, 0:W], in1=PS[ub][:, 0:W], op=ALU.mult),
                           [rSS[s], rPS[ub]], [rTT[s]])
                        op("dve", lambda e, s=s, hs=hs, fc=fc, gs=gs: e.tensor_tensor(out=HT[hs][:, fc, 0:W], in0=TT[s][:, 0:W],
                                                                                      in1=GBC[gs][:, 0:W], op=ALU.mult),
                           [rTT[s], rGBC[gs]], [rHT[hs]])
                    else:
                        op("dve", lambda e, ub=ub, s=s, hs=hs, fc=fc: e.tensor_tensor(out=HT[hs][:, fc, 0:W], in0=SS[s][:, 0:W],
                                                                                      in1=PS[ub][:, 0:W], op=ALU.mult),
                           [rSS[s], rPS[ub]], [rHT[hs]])
                for m in range(KD):
                    yb = 4 + m % 2
                    for fc in range(7):
                        op("pe", lambda e, fc=fc, m=m, yb=yb, hs=hs, wdp=wdp: e.matmul(PS[yb][:, 0:W], lhsT=wdp[:, fc, m * 128:(m + 1) * 128],
                                                                                        rhs=HT[hs][:, fc, 0:W], start=(fc == 0), stop=(fc == 6)),
                           [rwd, rHT[hs]], [rPS[yb]], signal=(fc == 6))
                    op("dve", lambda e, m=m, yb=yb: e.tensor_tensor(out=XF[:, m, 0:W], in0=XF[:, m, 0:W], in1=PS[yb][:, 0:W], op=ALU.add),
                       [rPS[yb], rXFm[m]], [rXFm[m]])

    def experts_sparse(wg_list, mode):
        IFE = ["pe", "act", "dve"]
        cnt = 0

        def gather(ei, xg, rxg, c0, width):
            kper = max(1, 512 // width)
            gi_ = 0
            for k0 in range(0, KD, kper):
                bank = 6 + gi_ % 2
                gi_ += 1
                ks = list(range(k0, min(KD, k0 + kper)))
                for kk, k in enumerate(ks):
                    for tb in range(4):
                        op("pe", lambda e, k=k, kk=kk, tb=tb, bank=bank: e.matmul(PS[bank][:, kk * width:(kk + 1) * width],
                                                                                  lhsT=XTOK[:, tb, k * 128:(k + 1) * 128],
                                                                                  rhs=PE_[tb][:, c0:c0 + width], start=(tb == 0), stop=(tb == 3)),
                           [rXTOK, rPE_], [rPS[bank]], signal=(tb == 3 and kk == len(ks) - 1))
                src = PS[bank][:, 0:len(ks) * width].rearrange("p (a t) -> p a t", a=len(ks))
                dst = xg[:, ks[0]:ks[-1] + 1, c0:c0 + width]
                if gi_ % 2 == 0:
                    op("act", lambda e, src=src, dst=dst: e.copy(out=dst, in_=src), [rPS[bank]], [rxg])
                else:
                    op("dve", lambda e, src=src, dst=dst: e.tensor_copy(out=dst, in_=src), [rPS[bank]], [rxg])

        def ffn_block(gi, xg, rxg, c0, width, ht, rht, sbs, wgp, rwg, wup, rwu, wdp, rwd):
            for fc in range(7):
                s_ = fc % 2
                gb, ub = fc % 2, 2 + fc % 2
                for k in range(KD):
                    op("pe", lambda e, k=k, fc=fc, gb=gb: e.matmul(PS[gb][:, 0:width], lhsT=wgp[:, k, fc * 128:(fc + 1) * 128],
                                                                    rhs=xg[:, k, c0:c0 + width], start=(k == 0), stop=(k == KD - 1)),
                       [rwg, rxg], [rPS[gb]], signal=(k == KD - 1))
                for k in range(KD):
                    op("pe", lambda e, k=k, fc=fc, ub=ub: e.matmul(PS[ub][:, 0:width], lhsT=wup[:, k, fc * 128:(fc + 1) * 128],
                                                                    rhs=xg[:, k, c0:c0 + width], start=(k == 0), stop=(k == KD - 1)),
                       [rwu, rxg], [rPS[ub]], signal=(k == KD - 1))
                op("act", lambda e, gb=gb, s_=s_: e.activation(out=SSA[s_][:, 0:width], in_=PS[gb][:, 0:width], func=AF.Silu),
                   [rPS[gb]], [rSSA[s_]])
                op("dve", lambda e, ub=ub, s_=s_, fc=fc: e.tensor_tensor(out=ht[:, fc, 0:width], in0=SSA[s_][:, 0:width], in1=PS[ub][:, 0:width],
                                                                         op=ALU.mult), [rSSA[s_], rPS[ub]], [rht])
            yi = 0
            for (sbk, hc0, mw, r0, nr) in sbs:
                for half in range(2):
                    yb = 4 + yi % 2
                    yi += 1
                    for fc in range(7):
                        op("pe", lambda e, fc=fc, hc0=hc0, mw=mw, half=half, yb=yb: e.matmul(PS[yb][0:mw, 0:512], lhsT=ht[:, fc, hc0:hc0 + mw],
                                                                                            rhs=wdp[:, fc, half * 512:(half + 1) * 512],
                                                                                            start=(fc == 0), stop=(fc == 6)),
                           [rwd, rht], [rPS[yb]], signal=(fc == 6))
                    dst = ACC[sbk][r0:r0 + nr, half * 512:(half + 1) * 512]
                    src = PS[yb][r0:r0 + nr, 0:512]
                    if gi == 0:
                        op("dve", lambda e, dst=dst, src=src: e.tensor_copy(out=dst, in_=src), [rPS[yb]], [rACC[sbk]])
                    else:
                        op("dve", lambda e, dst=dst, src=src: e.tensor_tensor(out=dst, in0=dst, in1=src, op=ALU.add),
                           [rPS[yb], rACC[sbk]], [rACC[sbk]])

        def scatter(sbs):
            for (sbk, hc0, mw, r0, nr) in sbs:
                op("act", lambda e, sbk=sbk, r0=r0, nr=nr: e.copy(out=ACCB[sbk][r0:r0 + nr, :], in_=ACC[sbk][r0:r0 + nr, :]),
                   [rACC[sbk]], [rACCB[sbk]])
            for m in range(KD):
                bank = 6 + m % 2
                for j, (sbk, hc0, mw, r0, nr) in enumerate(sbs):
                    op("pe", lambda e, m=m, j=j, sbk=sbk, r0=r0, nr=nr, bank=bank, n=len(sbs): e.matmul(
                        PS[bank][:, 0:512], lhsT=ACCB[sbk][r0:r0 + nr, m * 128:(m + 1) * 128], rhs=Q_[sbk][r0:r0 + nr, :],
                        start=(j == 0), stop=(j == n - 1)),
                       [rACCB[sbk], rQ[sbk]], [rPS[bank]], signal=(j == len(sbs) - 1))
                op("dve", lambda e, m=m, bank=bank: e.tensor_tensor(out=XF[:, m, 0:512], in0=XF[:, m, 0:512], in1=PS[bank][:, 0:512], op=ALU.add),
                   [rPS[bank], rXFm[m]], [rXFm[m]])

        def build_q(sbs):
            for sbk in sbs:
                op("dve", lambda e, sbk=sbk: e.scalar_tensor_tensor(out=Q_[sbk][:], in0=POSBC[:], scalar=SLOTID[:, sbk:sbk + 1], in1=GBC1[:],
                                                                    op0=ALU.is_equal, op1=ALU.mult), [rPOSBC, rGBC1, rC], [rQ[sbk]])

        for ei, (gv, uv, dv) in enumerate(wg_list):
            xg, rxg = XG[ei % 2], rXG[ei % 2]
            lnf = (rT1 + rT2 + rSQ + rZB + [rSTAT]) if (ei == 0 and mode == "A") else []
            for tb in range(4):
                op("dve", lambda e, tb=tb, ei=ei: e.tensor_scalar(out=PE_[tb][:], in0=IOTA_F[:], scalar1=POS[:, tb, ei:ei + 1],
                                                                  scalar2=MSKF[:, tb, ei:ei + 1], op0=ALU.is_equal, op1=ALU.mult),
                   [rPOS, rMSK, rC], [rPE_] + (lnf if tb == 0 else []))
            op("pe", lambda e, ei=ei: e.matmul(PS[6][:, 0:512], lhsT=SEL[:, ei, :], rhs=GTS[0:8, 0:512], start=True, stop=True),
               [rC, rGTS], [rPS[6]])
            op("pe", lambda e, ei=ei: e.matmul(PS[7][:, 0:512], lhsT=SEL[:, ei, :], rhs=POST[0:8, 0:512], start=True, stop=True),
               [rC, rPOST], [rPS[7]])
            op("act", lambda e: e.copy(out=GBC1[:], in_=PS[6][:, 0:512]), [rPS[6]], [rGBC1] + lnf)
            op("act", lambda e: e.copy(out=POSBC[:], in_=PS[7][:, 0:512]), [rPS[7]], [rPOSBC])
            build_q([0, 1, 2, 3])
            if mode == "A":
                gather(ei, xg, rxg, 0, WA)
            else:
                gather(ei, xg, rxg, B_C0, WT - B_C0)
            for gi in range(4):
                wgp, rwg = wload(gv[:, :, gi * 896:(gi + 1) * 896], (KD, 896))
                wup, rwu = wload(uv[:, :, gi * 896:(gi + 1) * 896], (KD, 896))
                wdp, rwd = wload(dv[:, gi * 7:(gi + 1) * 7, :], (7, D))
                hs = cnt % 2
                cnt += 1
                if mode == "A":
                    ffn_block(gi, xg, rxg, 0, WA, HTA[hs], rHTA[hs], A_BLK, wgp, rwg, wup, rwu, wdp, rwd)
                else:
                    ffn_block(gi, xg, rxg, B_C0, WB, HTBv, rHTB, B_BLK, wgp, rwg, wup, rwu, wdp, rwd)
            scatter(A_BLK if mode == "A" else B_BLK)

    def moe_sparse(wg_list):
        piece0 = ring_state["piece"]
        experts_sparse(wg_list, "A")
        IFE = ["pe", "act", "dve", "sp"]
        for eng in ("pe", "act", "dve"):
            op(eng, lambda e, eng=eng: e.reg_load(B.regs[eng], CNTM[0:1, 0:1]), [rCNT], [], signal=False)
        B.raw("sp", lambda e: e.reg_load(B.regs["sp"], CNTM[0:1, 0:1]), [rCNT])
        B.if_begin(IFE, 0, THR)
        ring_state["piece"] = piece0
        ring_state["force_hw"] = True
        experts_sparse(wg_list, "B")
        ring_state["force_hw"] = False
        B.if_end()

    def conv_module(W, nseq, L, sample, last_prompt):
        PADL = HW_ + L
        gtb = GTB[:, 0:KD * nseq * PADL].rearrange("p (m s t) -> p m s t", m=KD, s=nseq)
        if sample:
            for s in range(nseq):
                xs = s % 2
                dma("sp", XS[xs][0:HW_, :], cache[s], [], [rXS[xs]], "xs%d" % xs)
                bank = s % 2
                for m in range(KD):
                    op("pe", lambda e, m=m, xs=xs, bank=bank: e.transpose(out=PS[bank][:, m * HW_:(m + 1) * HW_],
                                                                          in_=XS[xs][0:HW_, m * 128:(m + 1) * 128], identity=identF[0:HW_, 0:HW_]),
                       [rXS[xs], rC], [rPS[bank]], signal=(m == KD - 1))
                op("dve", lambda e, s=s, bank=bank: e.tensor_copy(out=gtb[:, :, s, 0:HW_],
                                                                  in_=PS[bank][:, 0:KD * HW_].rearrange("p (m t) -> p m t", m=KD)),
                   [rPS[bank]], [rGTB])
        else:
            op("dve", lambda e: e.tensor_copy(out=gtb[:, :, 0, 0:HW_], in_=HIST[:]), [rHIST], [rGTB])
        def build_dg(m):
            ds = m % 2
            for k in range(NTAP):
                op("dve", lambda e, m=m, k=k, ds=ds: e.tensor_scalar(out=DG[ds][:, k, :], in0=identB[:], scalar1=WDW[:, m, k:k + 1],
                                                                     scalar2=None, op0=ALU.mult), [rC], [rDGk[ds][k]])
        build_dg(0)
        build_dg(1)
        pieces = {}
        for half in range(2):
            wa, rwa = wload(w_pw1_v[:, :, half * 512:(half + 1) * 512], (KD, 512))
            wgt, rwgt = wload(w_pw1_v[:, :, D + half * 512: D + (half + 1) * 512], (KD, 512))
            for mm in range(4):
                m = half * 4 + mm
                ab, gb = m % 2, 2 + m % 2
                s = m % 2
                for k in range(KD):
                    op("pe", lambda e, k=k, mm=mm, ab=ab, wa=wa: e.matmul(PS[ab][:, 0:W], lhsT=wa[:, k, mm * 128:(mm + 1) * 128],
                                                                           rhs=XB[:, k, 0:W], start=(k == 0), stop=(k == KD - 1)),
                       [rwa, rXB], [rPS[ab]], signal=(k == KD - 1))
                for k in range(KD):
                    op("pe", lambda e, k=k, mm=mm, gb=gb, wgt=wgt: e.matmul(PS[gb][:, 0:W], lhsT=wgt[:, k, mm * 128:(mm + 1) * 128],
                                                                             rhs=XB[:, k, 0:W], start=(k == 0), stop=(k == KD - 1)),
                       [rwgt, rXB], [rPS[gb]], signal=(k == KD - 1))
                op("act", lambda e, m=m, gb=gb, s=s: e.activation(out=T1[s][:, 0:W], in_=PS[gb][:, 0:W], func=AF.Sigmoid,
                                                                  bias=VBPW1[:, 8 + m:9 + m], scale=1.0), [rPS[gb], rC], [rT1[s]])
                a3 = PS[ab][:, 0:W].rearrange("p (s t) -> p s t", s=nseq)
                g3 = T1[s][:, 0:W].rearrange("p (s t) -> p s t", s=nseq)
                op("dve", lambda e, m=m, a3=a3, g3=g3: e.scalar_tensor_tensor(out=gtb[:, m, :, HW_:PADL], in0=a3, scalar=VBPW1[:, m:m + 1],
                                                                              in1=g3, op0=ALU.add, op1=ALU.mult),
                   [rPS[ab], rT1[s], rC], [rGTB])
                if sample or last_prompt:
                    op("dve", lambda e, m=m, a3=a3, g3=g3: e.scalar_tensor_tensor(out=GL[:, m, 0:nseq, :], in0=a3[:, :, L - HW_:L],
                                                                                  scalar=VBPW1[:, m:m + 1], in1=g3[:, :, L - HW_:L],
                                                                                  op0=ALU.add, op1=ALU.mult),
                       [rPS[ab], rT1[s], rC], [rGL])
        if not sample:
            op("dve", lambda e: e.tensor_copy(out=HIST[:], in_=gtb[:, :, 0, L:PADL]), [rGTB], [rHIST])
        if sample or last_prompt:
            for s in range(nseq):
                xs = s % 2
                for m in range(KD):
                    bank = 6 + m // 4
                    op("pe", lambda e, m=m, s=s, bank=bank: e.transpose(out=PS[bank][0:HW_, (m % 4) * 128:(m % 4 + 1) * 128],
                                                                        in_=GL[:, m, s, :], identity=identF[:]),
                       [rGL, rC], [rPS[bank]], signal=(m % 4 == 3))
                op("act", lambda e, xs=xs: e.copy(out=XS[xs][0:HW_, 0:512], in_=PS[6][0:HW_, :]), [rPS[6]], [rXS[xs]])
                op("dve", lambda e, xs=xs: e.tensor_copy(out=XS[xs][0:HW_, 512:1024], in_=PS[7][0:HW_, :]), [rPS[7]], [rXS[xs]])
                r0 = (HW_ * (1 + s)) if sample else 0
                dma("sp", cs_o[r0:r0 + HW_, :], XS[xs][0:HW_, :], [rXS[xs]], [], "ys%d" % xs)
        for m in range(KD):
            ds = m % 2
            if m >= 2:
                build_dg(m)
            bank = 4 + m % 2
            for s in range(nseq):
                for k in range(NTAP):
                    op("pe", lambda e, m=m, k=k, s=s, ds=ds, bank=bank: e.matmul(PS[bank][:, s * L:(s + 1) * L], lhsT=DG[ds][:, k, :],
                                                                                  rhs=gtb[:, m, s, k:k + L], start=(k == 0), stop=(k == NTAP - 1)),
                       [rDGk[ds][k], rGTB], [rPS[bank]], signal=(k == NTAP - 1 and s == nseq - 1))
            op("act", lambda e, m=m, bank=bank: e.activation(out=YC[:, m, 0:W], in_=PS[bank][:, 0:W], func=AF.Identity,
                                                             bias=VBDW[:, m:m + 1], scale=1.0), [rPS[bank], rC], [rYC])

        def outs(m, t2, rt2):
            op("act", lambda e: e.activation(out=ST[:, m, 0:W], in_=t2, func=AF.Silu, bias=VCLB[:, m:m + 1], scale=VCLG[:, m:m + 1]),
               [rt2, rC], [rST])
        layer_norm([(YC[:, m, 0:W], rYC) for m in range(KD)], W, KD, None, outs)
        for half in range(2):
            wp, rwp = wload(w_pw2_v[:, :, half * 512:(half + 1) * 512], (KD, 512))
            for mm in range(4):
                m = half * 4 + mm
                bank = 4 + m % 2
                for k in range(KD):
                    op("pe", lambda e, k=k, mm=mm, bank=bank, wp=wp: e.matmul(PS[bank][:, 0:W], lhsT=wp[:, k, mm * 128:(mm + 1) * 128],
                                                                               rhs=ST[:, k, 0:W], start=(k == 0), stop=(k == KD - 1)),
                       [rwp, rST], [rPS[bank]], signal=(k == KD - 1))
                op("dve", lambda e, m=m, bank=bank: e.scalar_tensor_tensor(out=XF[:, m, 0:W], in0=PS[bank][:, 0:W], scalar=VBPW2[:, m:m + 1],
                                                                           in1=XF[:, m, 0:W], op0=ALU.add, op1=ALU.add),
                   [rPS[bank], rC, rXFm[m]], [rXFm[m]])

    def router(W, sparse=False):
        nb = W // 128
        L_, M1, L2, M2, G = RT["L"], RT["M1"], RT["L2"], RT["M2"], RT["G"]
        for b in range(nb):
            for k in range(KD):
                op("pe", lambda e, k=k, b=b: e.matmul(PS[7][:, 0:NE], lhsT=XF[:, k, b * 128:(b + 1) * 128], rhs=WR[:, k, :],
                                                       start=(k == 0), stop=(k == KD - 1)),
                   [rXFm[k], rC], [rPS[7]], signal=(k == KD - 1))
            ops = [
                lambda e: e.tensor_tensor(out=L_[:], in0=PS[7][:, 0:NE], in1=BR[:], op=ALU.add),
                lambda e: e.tensor_reduce(out=RS["m1"][:], in_=L_[:], axis=AX.X, op=ALU.max),
                lambda e: e.tensor_scalar(out=M1[:], in0=L_[:], scalar1=RS["m1"][:], scalar2=None, op0=ALU.is_equal),
                lambda e: e.scalar_tensor_tensor(out=L2[:], in0=M1[:], scalar=-1e30, in1=L_[:], op0=ALU.mult, op1=ALU.add),
                lambda e: e.tensor_reduce(out=RS["m2"][:], in_=L2[:], axis=AX.X, op=ALU.max),
                lambda e: e.tensor_scalar(out=M2[:], in0=L2[:], scalar1=RS["m2"][:], scalar2=None, op0=ALU.is_equal),
                lambda e: e.tensor_tensor(out=RS["d"][:], in0=RS["m2"][:], in1=RS["m1"][:], op=ALU.subtract),
            ]
            for f in ops:
                op("dve", f, [rPS[7], rC, rRT], [rRT])
            op("act", lambda e: e.activation(out=RS["e"][:], in_=RS["d"][:], func=AF.Exp), [rRT], [rRT])
            ops = [
                lambda e: e.tensor_scalar(out=RS["den"][:], in0=RS["e"][:], scalar1=1.0, scalar2=None, op0=ALU.add),
                lambda e: e.reciprocal(out=RS["w1"][:], in_=RS["den"][:]),
                lambda e: e.tensor_tensor(out=RS["w2"][:], in0=RS["e"][:], in1=RS["w1"][:], op=ALU.mult),
                lambda e: e.tensor_scalar(out=G[:], in0=M1[:], scalar1=RS["w1"][:], scalar2=None, op0=ALU.mult),
                lambda e: e.scalar_tensor_tensor(out=G[:], in0=M2[:], scalar=RS["w2"][:], in1=G[:], op0=ALU.mult, op1=ALU.add),
            ]
            for f in ops:
                op("dve", f, [rRT], [rRT])
            op("pe", lambda e: e.transpose(out=PS[6][0:NE, 0:128], in_=G[:], identity=identF[:]), [rRT, rC], [rPS[6]])
            op("dve", lambda e, b=b: e.tensor_copy(out=GTS[0:8, b * 128:(b + 1) * 128], in_=PS[6][0:NE, 0:128]), [rPS[6]], [rGTS])
            if sparse:
                op("dve", lambda e, b=b: e.tensor_tensor(out=MSKF[:, b, :], in0=M1[:], in1=M2[:], op=ALU.add), [rRT], [rMSK])
                op("dve", lambda e, b=b: e.tensor_copy(out=MSK[:, b, :], in_=MSKF[:, b, :]), [rMSK], [rMSK])
        if sparse:
            for b in range(nb):
                terms = [(onesB, bb) for bb in range(b)] + [(UTRI, b)]
                for i, (lt, bb) in enumerate(terms):
                    op("pe", lambda e, lt=lt, bb=bb, b=b, i=i, n=len(terms): e.matmul(PS[6][:, b * NE:(b + 1) * NE], lhsT=lt[:], rhs=MSK[:, bb, :],
                                                                                      start=(i == 0), stop=(i == n - 1)),
                       [rMSK, rC], [rPS[6]], signal=False)
            for b in range(nb):
                op("pe", lambda e, b=b: e.matmul(PS[6][:, 4 * NE:5 * NE], lhsT=onesB[:], rhs=MSK[:, b, :], start=(b == 0), stop=(b == nb - 1)),
                   [rMSK, rC], [rPS[6]], signal=(b == nb - 1))
            op("dve", lambda e: e.tensor_copy(out=POS[:], in_=PS[6][:, 0:4 * NE].rearrange("p (b e) -> p b e", b=4)), [rPS[6]], [rPOS])
            op("dve", lambda e: e.tensor_copy(out=CNT[:], in_=PS[6][0:1, 4 * NE:5 * NE]), [rPS[6]], [rCNT])
            op("dve", lambda e: e.tensor_reduce(out=CNTMF[:], in_=PS[6][0:1, 4 * NE:5 * NE], axis=AX.X, op=ALU.max), [rPS[6]], [rCNT])
            op("dve", lambda e: e.tensor_copy(out=CNTM[:], in_=CNTMF[:]), [rCNT], [rCNT])
            for b in range(nb):
                op("pe", lambda e, b=b: e.transpose(out=PS[7][0:NE, b * 128:(b + 1) * 128], in_=POS[:, b, :], identity=identF[:]),
                   [rPOS, rC], [rPS[7]], signal=(b == nb - 1))
            op("dve", lambda e: e.tensor_copy(out=POST[:], in_=PS[7][0:NE, :]), [rPS[7]], [rPOST])
            for b in range(nb):
                bank = 4 + b % 2
                psb = PS[bank][:].bitcast(BF16)
                for k in range(KD):
                    op("pe", lambda e, k=k, b=b, psb=psb: e.transpose(out=psb[:, k * 128:(k + 1) * 128], in_=XB[:, k, b * 128:(b + 1) * 128],
                                                                       identity=identB[:]),
                       [rXB, rC], [rPS[bank]], signal=(k == KD - 1))
                if b % 2 == 0:
                    op("act", lambda e, b=b, psb=psb: e.copy(out=XTOK[:, b, :], in_=psb), [rPS[bank]], [rXTOK])
                else:
                    op("dve", lambda e, b=b, psb=psb: e.tensor_copy(out=XTOK[:, b, :], in_=psb), [rPS[bank]], [rXTOK])
        for m in range(KD):
            op("act", lambda e, m=m: e.mul(out=XF[:, m, 0:W], in_=XF[:, m, 0:W], mul=ALPHA), [rXFm[m]], [rXFm[m]])

    ff_views = [(ffg.rearrange("(k p) n -> p k n", p=128), ffu.rearrange("(k p) n -> p k n", p=128),
                 ffd.rearrange("(c p) n -> p c n", p=128))]
    moe_views = [(mog[e_].rearrange("(k p) n -> p k n", p=128), mou[e_].rearrange("(k p) n -> p k n", p=128),
                  mod[e_].rearrange("(c p) n -> p c n", p=128)) for e_ in range(NE)]

    tiles = [(i * WT, WT, 1, WT, False, i == NPT - 1) for i in range(NPT)]
    tiles.append((SEQ, NSAMP * LS, NSAMP, LS, True, False))
    stop = dbg.get("stop")
    if "tiles" in dbg:
        tiles = [tiles[i] for i in dbg["tiles"]]
    for ti_, (tok0, W, nseq, L, sample, last_prompt) in enumerate(tiles):
        ring_state["piece"] = 0
        ring_state["first"] = (ti_ == 0)
        if not dbg.get("noload"):
            load_tile(tok0, W)
        if sample:
            gv_rows = {0: 128, 1: 256}
        elif last_prompt:
            gv_rows = {3: 0}
        else:
            gv_rows = {}
        phases = [
            ("load", lambda: None),
            ("gmlp", lambda: gmlp(W, sample, gv_rows)),
            ("ln0", lambda: deepnorm_ln(W, 0, scaled=True)),
            ("ffn", lambda: experts(W, ff_views, gated=False)),
            ("ln1", lambda: deepnorm_ln(W, 1, scaled=True)),
            ("conv", lambda: conv_module(W, nseq, L, sample, last_prompt)),
            ("ln2", lambda: deepnorm_ln(W, 2, scaled=False)),
            ("router", lambda: router(W, sparse=(not sample) and not dbg.get("dense"))),
            ("moe", lambda: (experts(W, moe_views[:dbg.get("nexp", NE)], gated=True) if (sample or dbg.get("dense"))
                             else moe_sparse(moe_views[:dbg.get("nexp", NE)]))),
            ("ln3", lambda: deepnorm_ln(W, 3, scaled=False, make_xb=False)),
        ]
        for name, fn in phases:
            fn()
            if stop == name:
                break
        if not dbg.get("nostore"):
            store_tile(tok0, W)
    B.final_wait("sp")
    B.final_wait("pool")
    B.emit()


_NC_CACHE = {}


def kernel(x_prompt, x_sample, cache_conv, gm_w_in, gm_b_in, gm_lnv_g, gm_lnv_b, gm_w_s, gm_b_s, gm_w_out, gm_b_out,
           cv_w_pw1, cv_b_pw1, cv_w_dw, cv_b_dw, cv_ln_g, cv_ln_b, cv_w_pw2, cv_b_pw2,
           ff_w_gate, ff_w_up, ff_w_down, moe_w_router, moe_b_router, moe_w_gate, moe_w_up, moe_w_down, ln_g, ln_b):
    f = lambda a: np.ascontiguousarray(np.asarray(a, dtype=np.float32))
    shared = {
        "gm_w_in": f(gm_w_in[0]), "gm_b_in": f(gm_b_in).reshape(1, -1), "gm_lnv_g": f(gm_lnv_g).reshape(1, -1),
        "gm_lnv_b": f(gm_lnv_b).reshape(1, -1), "gm_w_s": f(gm_w_s[0]), "gm_b_s": f(gm_b_s[0]),
        "gm_w_out": f(gm_w_out[0]), "gm_b_out": f(gm_b_out).reshape(1, -1),
        "cv_w_pw1": f(cv_w_pw1[0]), "cv_b_pw1": f(cv_b_pw1).reshape(1, -1), "cv_w_dw": f(cv_w_dw[0]),
        "cv_b_dw": f(cv_b_dw).reshape(1, -1), "cv_ln_g": f(cv_ln_g).reshape(1, -1), "cv_ln_b": f(cv_ln_b).reshape(1, -1),
        "cv_w_pw2": f(cv_w_pw2[0]), "cv_b_pw2": f(cv_b_pw2).reshape(1, -1),
        "ff_w_gate": f(ff_w_gate[0]), "ff_w_up": f(ff_w_up[0]), "ff_w_down": f(ff_w_down[0]),
        "moe_w_router": f(moe_w_router[0]), "moe_b_router": f(moe_b_router).reshape(1, -1),
        "moe_w_gate": f(moe_w_gate[0]), "moe_w_up": f(moe_w_up[0]), "moe_w_down": f(moe_w_down[0]),
        "ln_g": f(ln_g).reshape(1, -1), "ln_b": f(ln_b).reshape(1, -1),
    }
    xp = f(x_prompt)
    xs = f(x_sample)
    cc = f(cache_conv)
    in_maps = []
    for c in range(NCORE):
        xin = np.concatenate([xp[c], xs[c * NSAMP:(c + 1) * NSAMP].reshape(NSAMP * LS, D)], axis=0)
        m = dict(shared)
        m["xin"] = np.ascontiguousarray(xin)
        m["cache"] = np.ascontiguousarray(cc[0, c * NSAMP:(c + 1) * NSAMP])
        in_maps.append(m)
    if "nc" not in _NC_CACHE:
        _NC_CACHE["nc"] = build_program()
    res = run_bass_kernel_spmd(_NC_CACHE["nc"], in_maps, core_ids=list(range(NCORE)))
    y_p = np.empty((8, SEQ, D), np.float32)
    y_s = np.empty((32, LS, D), np.float32)
    gv_p = np.empty((1, 8, 128, DV), np.float32)
    gv_s = np.empty((1, 32, LS, DV), np.float32)
    cs_p = np.empty((1, 8, HW_, D), np.float32)
    cs_s = np.empty((1, 32, HW_, D), np.float32)
    for c in range(NCORE):
        r = res.results[c]
        y_p[c] = r["y_o"][0:SEQ]
        y_s[c * NSAMP:(c + 1) * NSAMP] = r["y_o"][SEQ:].reshape(NSAMP, LS, D)
        gv_p[0, c] = r["gv_o"][0:128]
        gv_s[0, c * NSAMP:(c + 1) * NSAMP] = r["gv_o"][128:].reshape(NSAMP, LS, DV)
        cs_p[0, c] = r["cs_o"][0:HW_]
        cs_s[0, c * NSAMP:(c + 1) * NSAMP] = r["cs_o"][HW_:].reshape(NSAMP, HW_, D)
    return y_p, y_s, gv_p, gv_s, cs_p, cs_s
```
